# Optimizing a Trainium2 kernel written in Bass

```python
import jax
import jax.numpy as jnp
from jax import lax
import numpy as np

D_MODEL = 4096
BATCH = 1
SEQ = 8192
DEPTH = 1

HEAD_DIM = 128
MIX_WIDTH = D_MODEL
FOX_WIDTH = MIX_WIDTH // 2
HGRN_WIDTH = MIX_WIDTH - FOX_WIDTH
FOX_HEADS = FOX_WIDTH // HEAD_DIM
HGRN_HEADS = HGRN_WIDTH // HEAD_DIM
FOX_BLOCK = 128
HGRN_CHUNK = 64
IN_COLS = 3 * FOX_WIDTH + FOX_HEADS + 4 * HGRN_WIDTH
PEER_HEADS = 8
PEER_KEYS = 128
PEER_EXPERTS = PEER_KEYS * PEER_KEYS
PEER_TOPK = 16
PEER_QDIM = 256
PEER_HALF = PEER_QDIM // 2
PEER_TOKEN_BLOCK = 64
N_MOD = 6
EPS = 1e-6

kernel_name = "hymba_fox_hgrn2_peer_adaln"


def rms_norm(x, gain):
    xf = x.astype(jnp.float32)
    y = xf * lax.rsqrt(jnp.mean(xf * xf, axis=-1, keepdims=True) + EPS)
    return (y * gain.astype(jnp.float32)).astype(x.dtype)


def fox_attention(q, k, v, log_f):
    B, S, H, Dh = q.shape
    scale = Dh ** -0.5
    cum_f = jnp.cumsum(log_f, axis=1)
    qh = jnp.transpose(q, (0, 2, 1, 3))
    kh = jnp.transpose(k, (0, 2, 1, 3))
    vh = jnp.transpose(v, (0, 2, 1, 3))
    fh = jnp.transpose(cum_f, (0, 2, 1))
    nb = S // FOX_BLOCK
    q_blocks = jnp.transpose(qh.reshape(B, H, nb, FOX_BLOCK, Dh), (2, 0, 1, 3, 4))
    f_blocks = jnp.transpose(fh.reshape(B, H, nb, FOX_BLOCK), (2, 0, 1, 3))
    key_pos = jnp.arange(S)

    def one_block(args):
        qb, fb, bi = args
        s = jnp.einsum('bhqd,bhkd->bhqk', qb, kh, preferred_element_type=jnp.float32) * scale
        s = s + fb[..., None] - fh[:, :, None, :]
        q_pos = bi * FOX_BLOCK + jnp.arange(FOX_BLOCK)
        causal = key_pos[None, :] <= q_pos[:, None]
        s = jnp.where(causal, s, -jnp.inf)
        p = jax.nn.softmax(s, axis=-1)
        return jnp.einsum('bhqk,bhkd->bhqd', p.astype(vh.dtype), vh)

    out = lax.map(one_block, (q_blocks, f_blocks, jnp.arange(nb)))
    return jnp.transpose(out, (1, 0, 3, 2, 4)).reshape(B, S, H, Dh)


def hgrn2_recurrence(q, k, v, log_f):
    B, S, H, Dk = q.shape
    Dv = v.shape[-1]
    C = HGRN_CHUNK
    nc = S // C

    def to_chunks(t):
        return jnp.transpose(t.reshape(B, nc, C, H, t.shape[-1]), (1, 0, 3, 2, 4))

    qc, kc, vc, gc = to_chunks(q), to_chunks(k), to_chunks(v), to_chunks(log_f)
    causal = jnp.tril(jnp.ones((C, C), dtype=bool))[:, :, None]

    def step(state, inp):
        qb, kb, vb, gb = inp
        b = jnp.cumsum(gb, axis=2)
        diff = b[:, :, :, None, :] - b[:, :, None, :, :]
        decay = jnp.exp(jnp.where(causal, diff, -jnp.inf))
        scores = jnp.einsum('bhtk,bhsk,bhtsk->bhts', qb, kb, decay)
        o_intra = jnp.einsum('bhts,bhsv->bhtv', scores, vb)
        o_inter = jnp.einsum('bhtk,bhkv->bhtv', qb * jnp.exp(b), state)
        b_last = b[:, :, -1:, :]
        k_dec = kb * jnp.exp(b_last - b)
        new_state = state * jnp.exp(b_last)[:, :, 0, :, None] + jnp.einsum('bhsk,bhsv->bhkv', k_dec, vb)
        return new_state, o_intra + o_inter

    state0 = jnp.zeros((B, H, Dk, Dv), jnp.float32)
    _, out = lax.scan(step, state0, (qc, kc, vc, gc))
    return jnp.transpose(out, (1, 0, 3, 2, 4)).reshape(B, S, H, Dv)


def hybrid_mixer(h, w_in, fox_f_bias, fox_q_gain, fox_k_gain, lower_bound, hgrn_out_gain, w_out):
    B, S, _ = h.shape
    proj = h @ w_in
    o1 = FOX_WIDTH
    o2 = 2 * FOX_WIDTH
    o3 = 3 * FOX_WIDTH
    o4 = o3 + FOX_HEADS
    o5 = o4 + HGRN_WIDTH
    o6 = o5 + HGRN_WIDTH
    o7 = o6 + HGRN_WIDTH
    fq = proj[..., :o1].reshape(B, S, FOX_HEADS, HEAD_DIM)
    fk = proj[..., o1:o2].reshape(B, S, FOX_HEADS, HEAD_DIM)
    fv = proj[..., o2:o3].reshape(B, S, FOX_HEADS, HEAD_DIM)
    f_logit = proj[..., o3:o4]
    hq = proj[..., o4:o5].reshape(B, S, HGRN_HEADS, HEAD_DIM)
    hf = proj[..., o5:o6].reshape(B, S, HGRN_HEADS, HEAD_DIM)
    hi = proj[..., o6:o7].reshape(B, S, HGRN_HEADS, HEAD_DIM)
    hg = proj[..., o7:].reshape(B, S, HGRN_HEADS, HEAD_DIM)

    fq = rms_norm(fq, fox_q_gain)
    fk = rms_norm(fk, fox_k_gain)
    fox_log_f = jax.nn.log_sigmoid(f_logit.astype(jnp.float32) + fox_f_bias.astype(jnp.float32))
    fox_out = fox_attention(fq, fk, fv, fox_log_f)

    lb = lower_bound.reshape(HGRN_HEADS, HEAD_DIM)
    forget = lb + (1.0 - lb) * jax.nn.sigmoid(hf.astype(jnp.float32))
    h_log_f = jnp.log(forget)
    h_key = 1.0 - forget
    h_query = jax.nn.silu(hq.astype(jnp.float32)) * (HEAD_DIM ** -0.5)
    h_rec = hgrn2_recurrence(h_query, h_key, hi.astype(jnp.float32), h_log_f)
    hgrn_out = rms_norm(h_rec, hgrn_out_gain) * jax.nn.silu(hg.astype(jnp.float32))

    merged = jnp.concatenate([fox_out.reshape(B, S, FOX_WIDTH).astype(h.dtype),
                              hgrn_out.reshape(B, S, HGRN_WIDTH).astype(h.dtype)], axis=-1)
    return merged @ w_out


def peer_ffn(h, w_query, sub_keys, u_experts, v_experts):
    B, S, D = h.shape
    T = B * S
    ht = h.reshape(T, D)
    q = (ht @ w_query).reshape(T, PEER_HEADS, 2, PEER_HALF)
    scores = jnp.einsum('thpd,hpnd->thpn', q, sub_keys, preferred_element_type=jnp.float32)
    top_s, top_i = lax.top_k(scores, PEER_TOPK)
    cand_s = top_s[:, :, 0, :, None] + top_s[:, :, 1, None, :]
    cand_i = top_i[:, :, 0, :, None] * PEER_KEYS + top_i[:, :, 1, None, :]
    cand_s = cand_s.reshape(T, PEER_HEADS, PEER_TOPK * PEER_TOPK)
    cand_i = cand_i.reshape(T, PEER_HEADS, PEER_TOPK * PEER_TOPK)
    best_s, best_pos = lax.top_k(cand_s, PEER_TOPK)
    expert_idx = jnp.take_along_axis(cand_i, best_pos, axis=-1)
    gates = jax.nn.softmax(best_s.astype(jnp.float32), axis=-1)

    nb = T // PEER_TOKEN_BLOCK
    xb_all = ht.reshape(nb, PEER_TOKEN_BLOCK, D)
    idx_all = expert_idx.reshape(nb, PEER_TOKEN_BLOCK, PEER_HEADS, PEER_TOPK)
    g_all = gates.reshape(nb, PEER_TOKEN_BLOCK, PEER_HEADS, PEER_TOPK)

    def one_block(args):
        xb, idx, g = args
        u = u_experts[idx]
        act = jax.nn.gelu(jnp.einsum('td,thkd->thk', xb, u, preferred_element_type=jnp.float32), approximate=False)
        v = v_experts[idx]
        return jnp.einsum('thk,thkd->td', (g * act).astype(v.dtype), v)

    out = lax.map(one_block, (xb_all, idx_all, g_all))
    return out.reshape(B, S, D)


def setup_inputs(seed: int = 0) -> dict:
    key = jax.random.key(seed)
    ks = jax.random.split(key, 17)
    D = D_MODEL
    L = DEPTH
    f32 = jnp.float32

    def nrm(k, shape, scale):
        return jax.random.normal(k, shape, f32) * scale

    return {
        "x": nrm(ks[0], (BATCH, SEQ, D), 1.0),
        "c": nrm(ks[1], (BATCH, D), 1.0),
        "ada_w": nrm(ks[2], (L, D, N_MOD * D), 0.5 * D ** -0.5),
        "ada_b": nrm(ks[3], (L, N_MOD * D), 0.02),
        "norm1_gain": 1.0 + nrm(ks[4], (L, D), 0.02),
        "norm2_gain": 1.0 + nrm(ks[5], (L, D), 0.02),
        "w_in": nrm(ks[6], (L, D, IN_COLS), D ** -0.5),
        "fox_f_bias": 2.0 + nrm(ks[7], (L, FOX_HEADS), 0.5),
        "fox_q_gain": 1.0 + nrm(ks[8], (L, FOX_HEADS, HEAD_DIM), 0.02),
        "fox_k_gain": 1.0 + nrm(ks[9], (L, FOX_HEADS, HEAD_DIM), 0.02),
        "hgrn_lower_bounds": nrm(ks[10], (L + 1, HGRN_WIDTH), 0.1).at[-1].add(1.0),
        "hgrn_out_gain": 1.0 + nrm(ks[11], (L, HGRN_HEADS, HEAD_DIM), 0.02),
        "w_out": nrm(ks[12], (L, MIX_WIDTH, D), MIX_WIDTH ** -0.5),
        "peer_w_query": nrm(ks[13], (L, D, PEER_HEADS * PEER_QDIM), D ** -0.5),
        "peer_sub_keys": nrm(ks[14], (L, PEER_HEADS, 2, PEER_KEYS, PEER_HALF), PEER_HALF ** -0.5),
        "peer_u": nrm(ks[15], (L, PEER_EXPERTS, D), D ** -0.5),
        "peer_v": nrm(ks[16], (L, PEER_EXPERTS, D), PEER_HEADS ** -0.5),
    }


def reference(x, c, ada_w, ada_b, norm1_gain, norm2_gain, w_in, fox_f_bias, fox_q_gain, fox_k_gain,
              hgrn_lower_bounds, hgrn_out_gain, w_out, peer_w_query, peer_sub_keys, peer_u, peer_v):
    lower_bounds = jnp.cumsum(jax.nn.softmax(hgrn_lower_bounds.astype(jnp.float32), axis=0), axis=0)
    c_act = jax.nn.silu(c)
    for layer in range(DEPTH):
        mod = c_act @ ada_w[layer] + ada_b[layer]
        shift1, scale1, gate1, shift2, scale2, gate2 = jnp.split(mod[:, None, :], N_MOD, axis=-1)
        h = rms_norm(x, norm1_gain[layer]) * (1.0 + scale1) + shift1
        mix = hybrid_mixer(h, w_in[layer], fox_f_bias[layer], fox_q_gain[layer], fox_k_gain[layer],
                           lower_bounds[layer], hgrn_out_gain[layer], w_out[layer])
        x = x + gate1 * mix
        h = rms_norm(x, norm2_gain[layer]) * (1.0 + scale2) + shift2
        x = x + gate2 * peer_ffn(h, peer_w_query[layer], peer_sub_keys[layer], peer_u[layer], peer_v[layer])
    return x
```

```python
from contextlib import ExitStack

import numpy as np
import ml_dtypes

import concourse.bass as bass
import concourse.mybir as mybir
from concourse.bass_utils import run_bass_kernel_spmd

F32 = mybir.dt.float32
BF16 = mybir.dt.bfloat16
AF = mybir.ActivationFunctionType
ALU = mybir.AluOpType

NCORES = 8
D = 4096
SEQ = 8192
HD = 128
EPS = 1e-6
ENGS = ("pe", "act", "dve", "pool", "sp")


class _Op:
    __slots__ = ("eng", "fn", "deps", "dma", "sig", "sem", "val", "idx")

    def __init__(self, eng, fn, dma):
        self.eng = eng; self.fn = fn; self.deps = set(); self.dma = dma
        self.sig = False; self.sem = None; self.val = 0; self.idx = 0


class Sched:
    def __init__(self, nc, n_dsem=10):
        self.nc = nc
        self.ops = {e: [] for e in ENGS}
        self.lastw = {}
        self.readers = {}
        self.n_dsem = n_dsem

    def add(self, eng, fn, reads=(), writes=(), dma=False):
        op = _Op(eng, fn, dma)
        deps = set()
        for t in reads:
            w = self.lastw.get(t)
            if w is not None:
                deps.add(w)
        for t in writes:
            w = self.lastw.get(t)
            if w is not None:
                deps.add(w)
            for r in self.readers.get(t, ()):
                deps.add(r)
        for t in reads:
            self.readers.setdefault(t, []).append(op)
        for t in writes:
            self.lastw[t] = op
            self.readers[t] = []
        deps.discard(op)
        op.deps = deps
        op.idx = len(self.ops[eng])
        self.ops[eng].append(op)
        return op

    def dma(self, eng, out, in_, reads=(), writes=()):
        return self.add(eng, lambda e: e.dma_start(out=out, in_=in_), reads, writes, dma=True)

    def emit(self, stack):
        nc = self.nc

        def skip(d, op):
            return d.eng == "pe" and op.eng == "pe" and not d.dma and not op.dma

        for e in ENGS:
            for op in self.ops[e]:
                for d in op.deps:
                    if not skip(d, op):
                        d.sig = True
        csem = {e: stack.enter_context(nc.semaphore("c_" + e)) for e in ENGS}
        dsems = {e: [stack.enter_context(nc.semaphore("d_%s%d" % (e, i))) for i in range(self.n_dsem)]
                 for e in ("sp", "act", "pool")}
        for e in ENGS:
            cnt = 0
            dcnt = 0
            duse = [0] * self.n_dsem
            for op in self.ops[e]:
                if op.dma:
                    k = dcnt % self.n_dsem
                    dcnt += 1
                    duse[k] += 1
                    op.sem = dsems[e][k]
                    op.val = 16 * duse[k]
                    op.sig = True
                elif op.sig:
                    cnt += 1
                    op.sem = csem[e]
                    op.val = cnt
        blk = stack.enter_context(nc.Block())

        def run(e):
            def body(eng):
                waited = {}
                dprev = {}

                def wait(d):
                    key = id(d.sem)
                    if waited.get(key, 0) >= d.val:
                        return
                    eng.wait_ge(d.sem, d.val)
                    waited[key] = d.val

                for op in self.ops[e]:
                    for d in sorted(op.deps, key=lambda o: (o.eng, o.idx)):
                        if not skip(d, op):
                            wait(d)
                    if op.dma:
                        p = dprev.get(id(op.sem))
                        if p is not None:
                            wait(p)
                        dprev[id(op.sem)] = op
                    ins = op.fn(eng)
                    if op.sig:
                        ins.then_inc(op.sem, 16 if op.dma else 1)
            return body

        blk.tensor(run("pe"))
        blk.scalar(run("act"))
        blk.vector(run("dve"))
        blk.gpsimd(run("pool"))
        blk.sync(run("sp"))


def _finish(S, st, out_tokens):
    S.add("sp", lambda e: e.nop(), reads=list(out_tokens))
    S.emit(st)


MODC = 6 * D // NCORES


def build_mod():
    nc = bass.Bass("TRN2", target_bir_lowering=False)
    c_in = nc.dram_tensor("c", [128, 32], F32, kind="ExternalInput").ap()
    w_in = nc.dram_tensor("w", [D, MODC], F32, kind="ExternalInput").ap()
    b_in = nc.dram_tensor("b", [1, MODC], F32, kind="ExternalInput").ap()
    out = nc.dram_tensor("mod", [1, MODC], F32, kind="ExternalOutput").ap()
    wv = w_in.rearrange("(p k) n -> k p n", k=32)
    S = Sched(nc)
    with ExitStack() as st:
        ct = st.enter_context(nc.sbuf_tensor("ct", [128, 32], F32))
        cs = st.enter_context(nc.sbuf_tensor("cs", [128, 32], F32))
        acc = st.enter_context(nc.sbuf_tensor("acc", [128, MODC], F32))
        wts = [st.enter_context(nc.sbuf_tensor("wt%d" % i, [128, MODC], F32)) for i in range(3)]
        ones = st.enter_context(nc.sbuf_tensor("ones", [128, 1], F32))
        bt = st.enter_context(nc.sbuf_tensor("bt", [1, MODC], F32))
        res = st.enter_context(nc.sbuf_tensor("res", [1, MODC], F32))
        pss = [st.enter_context(nc.psum_tensor("ps%d" % i, [128, 512], F32)) for i in range(6)]
        S.dma("sp", ct[:], c_in[:, :], writes=["ct"])
        S.dma("sp", bt[:], b_in[:, :], writes=["bt"])
        S.add("act", lambda e: e.activation(out=cs[:], in_=ct[:], func=AF.Silu), reads=["ct"], writes=["cs"])
        S.add("pool", lambda e: e.memset(ones[:], 1.0), writes=["ones"])
        for k in range(32):
            wt = wts[k % 3]
            tok = "wt%d" % (k % 3)
            S.dma("sp", wt[:], wv[k], writes=[tok])
            if k == 0:
                S.add("dve", lambda e, wt=wt: e.tensor_scalar(out=acc[:], in0=wt[:], scalar1=cs[:, 0:1], scalar2=None,
                                                               op0=ALU.mult), reads=[tok, "cs"], writes=["acc"])
            else:
                S.add("dve", lambda e, wt=wt, k=k: e.scalar_tensor_tensor(out=acc[:], in0=wt[:], scalar=cs[:, k:k + 1],
                                                                           in1=acc[:], op0=ALU.mult, op1=ALU.add),
                      reads=[tok, "cs", "acc"], writes=["acc"])
        for j in range(6):
            S.add("pe", lambda e, j=j: e.matmul(pss[j][0:1, :], lhsT=ones[:, 0:1], rhs=acc[:, j * 512:(j + 1) * 512],
                                                start=True, stop=True), reads=["ones", "acc"], writes=["ps%d" % j])
            S.add("dve", lambda e, j=j: e.tensor_tensor(out=res[0:1, j * 512:(j + 1) * 512], in0=pss[j][0:1, :],
                                                        in1=bt[0:1, j * 512:(j + 1) * 512], op=ALU.add),
                  reads=["ps%d" % j, "bt"], writes=["res%d" % j])
        S.dma("sp", out[:, :], res[:], reads=["res%d" % j for j in range(6)], writes=["out"])
        _finish(S, st, ["out"])
    return nc


def run_mod(c, ada_w, ada_b):
    nc = build_mod()
    c2 = np.ascontiguousarray(c.reshape(128, 32))
    maps = []
    for i in range(NCORES):
        sl = slice(i * MODC, (i + 1) * MODC)
        maps.append({"c": c2, "w": np.ascontiguousarray(ada_w[0][:, sl]),
                     "b": np.ascontiguousarray(ada_b[0][sl].reshape(1, MODC))})
    res = run_bass_kernel_spmd(nc, maps, core_ids=list(range(NCORES)))
    return np.concatenate([np.asarray(r["mod"]).reshape(-1) for r in res.results])


NT = SEQ // 128
FC = 770


def _consts():
    ident = np.eye(128, dtype=np.float32)
    ut = np.triu(np.ones((128, 128), np.float32))
    return ident.astype(ml_dtypes.bfloat16), ut, ut.astype(ml_dtypes.bfloat16)


def build_fox(nt=NT):
    nc = bass.Bass("TRN2", target_bir_lowering=False)
    seq = nt * 128
    x_in = nc.dram_tensor("x", [seq, D], F32, kind="ExternalInput").ap()
    g1_in = nc.dram_tensor("g1", [128, 32], F32, kind="ExternalInput").ap()
    sc_in = nc.dram_tensor("sc1", [128, 32], F32, kind="ExternalInput").ap()
    sh_in = nc.dram_tensor("sh1", [128, 32], F32, kind="ExternalInput").ap()
    w_in = nc.dram_tensor("w", [D, FC], F32, kind="ExternalInput").ap()
    fb_in = nc.dram_tensor("fb", [128, 2], F32, kind="ExternalInput").ap()
    gq_in = nc.dram_tensor("gq", [128, 2], F32, kind="ExternalInput").ap()
    gk_in = nc.dram_tensor("gk", [128, 2], F32, kind="ExternalInput").ap()
    id_in = nc.dram_tensor("ident", [128, 128], BF16, kind="ExternalInput").ap()
    ut_in = nc.dram_tensor("ut", [128, 128], F32, kind="ExternalInput").ap()
    utb_in = nc.dram_tensor("utb", [128, 128], BF16, kind="ExternalInput").ap()
    neg_in = nc.dram_tensor("negm", [128, 128], BF16, kind="ExternalInput").ap()
    idf_in = nc.dram_tensor("identf", [128, 128], F32, kind="ExternalInput").ap()
    out = nc.dram_tensor("mT", [256, seq], BF16, kind="ExternalOutput").ap()
    wv = w_in.rearrange("(k p) n -> k p n", p=128)
    S = Sched(nc)
    with ExitStack() as st:
        sb = lambda n, s, d: st.enter_context(nc.sbuf_tensor(n, s, d))
        wF = sb("wF", [128, 32, FC], BF16)
        wst = [sb("wst%d" % i, [128, FC], F32) for i in range(2)]
        qT = [sb("qT%d" % h, [128, seq], BF16) for h in range(2)]
        kT = [sb("kT%d" % h, [128, seq], BF16) for h in range(2)]
        Vx = [sb("Vx%d" % h, [128, nt, 129], BF16) for h in range(2)]
        fl = sb("fl", [128, 2, nt], F32)
        xb = sb("xb", [128, D], F32)
        xs = sb("xs", [128, D], BF16)
        hT = [sb("hT%d" % i, [128, 32, 128], BF16) for i in range(2)]
        ss = sb("ss", [128, nt], F32)
        sq4 = sb("sq4", [128, 4], F32)
        qn = [sb("qn%d" % i, [128, 128], BF16) for i in range(4)]
        a1 = sb("a1_s", [128, 32], F32); sh1 = sb("sh1_s", [128, 32], F32); g1 = sb("g1_s", [128, 32], F32)
        fb = sb("fb_s", [128, 2], F32); gq = sb("gq_s", [128, 2], F32); gk = sb("gk_s", [128, 2], F32)
        ident = sb("ident_s", [128, 128], BF16); ut = sb("ut_s", [128, 128], F32); utb = sb("utb_s", [128, 128], BF16)
        onesf = sb("onesf", [128, 128], F32)
        negm = sb("negm_s", [128, 128], BF16); identf = sb("identf_s", [128, 128], F32)
        cum2 = sb("cum2", [128, 128], F32); off2 = sb("off2", [128, 128], F32)
        ctf = sb("ctf", [128, 128], F32); cthi = sb("cthi", [128, 128], BF16); ctr = sb("ctr", [128, 128], F32)
        ctlo = sb("ctlo", [128, 128], BF16)
        CT2 = [sb("CT2_%d" % h, [128, 128], BF16) for h in range(2)]
        E2 = [sb("E2_%d" % i, [128, 128], BF16) for i in range(2)]
        junk = sb("junk", [128, 128], BF16)
        cum = sb("cum", [128, 2, nt], F32); off = sb("off", [128, 2, nt], F32); tot = sb("tot", [128, 2, nt], F32)
        lf = sb("lf", [128, 2, nt], F32)
        negb = sb("negb", [128, nt], F32)
        pT = [sb("pT%d" % i, [128, 128], BF16) for i in range(3)]
        rden = sb("rden", [128, 1], F32)
        on = sb("on", [128, 128], BF16)
        ost = [sb("ost%d" % i, [128, 128], BF16) for i in range(2)]
        B = [st.enter_context(nc.psum_tensor("B%d" % i, [128, 512], F32)) for i in range(8)]
        Bb = [b[:].bitcast(BF16) for b in B]

        for (t, src, name) in ((g1, g1_in, "g1"), (a1, sc_in, "a1"), (sh1, sh_in, "sh1"), (fb, fb_in, "fb"),
                               (gq, gq_in, "gq"), (gk, gk_in, "gk"), (ident, id_in, "ident"), (ut, ut_in, "ut"),
                               (utb, utb_in, "utb"), (negm, neg_in, "negm"), (identf, idf_in, "identf")):
            S.dma("sp", t[:], src[:, :], writes=[name])
        S.add("dve", lambda e: e.scalar_tensor_tensor(out=a1[:], in0=a1[:], scalar=1.0, in1=g1[:], op0=ALU.add,
                                                      op1=ALU.mult), reads=["a1", "g1"], writes=["a1"])
        S.add("dve", lambda e: e.tensor_scalar(out=fb[:], in0=fb[:], scalar1=-1.0, scalar2=None, op0=ALU.mult),
              reads=["fb"], writes=["fb"])
        S.add("pool", lambda e: e.memset(onesf[:], 1.0), writes=["onesf"])
        for h in range(2):
            S.add("pool", lambda e, h=h: e.memset(Vx[h][:, :, 128:129], 1.0), writes=["Vx%d" % h])
        for k in range(32):
            S.dma("sp", wst[k % 2][:], wv[k], writes=["wst%d" % (k % 2)])
            eng = ("act", "dve", "pool")[k % 3]
            if eng == "act":
                S.add("act", lambda e, k=k: e.activation(out=wF[:, k, :], in_=wst[k % 2][:], func=AF.Copy),
                      reads=["wst%d" % (k % 2)], writes=["wF"])
            else:
                S.add(eng, lambda e, k=k: e.tensor_copy(out=wF[:, k, :], in_=wst[k % 2][:]),
                      reads=["wst%d" % (k % 2)], writes=["wF"])

        for tt in range(nt):
            ts = slice(tt * 128, (tt + 1) * 128)
            S.dma("sp", xb[:], x_in[ts, :], writes=["xb"])
            S.add("act", lambda e, tt=tt: e.activation(out=xs[:], in_=xb[:], func=AF.Square,
                                                        accum_out=ss[:, tt:tt + 1]), reads=["xb"], writes=["xs", "ss"])
            S.add("act", lambda e, tt=tt: e.activation(out=ss[:, tt:tt + 1], in_=ss[:, tt:tt + 1], func=AF.Sqrt,
                                                        scale=1.0 / D, bias=EPS), reads=["ss"], writes=["ss"])
            S.add("dve", lambda e, tt=tt: e.reciprocal(out=ss[:, tt:tt + 1], in_=ss[:, tt:tt + 1]),
                  reads=["ss"], writes=["ss"])
            S.add("dve", lambda e, tt=tt: e.tensor_scalar(out=xs[:], in0=xb[:], scalar1=ss[:, tt:tt + 1], scalar2=None,
                                                           op0=ALU.mult), reads=["xb", "ss"], writes=["xs"])
            hb = hT[tt % 2]
            htok = "hT%d" % (tt % 2)
            for k4 in range(8):
                bk = k4 % 2
                for j in range(4):
                    k = k4 * 4 + j
                    S.add("pe", lambda e, k=k, j=j, bk=bk: e.transpose(Bb[bk][:, j * 128:(j + 1) * 128],
                                                                       xs[:, k * 128:(k + 1) * 128], ident[:]),
                          reads=["xs", "ident"], writes=["B%d" % bk])
                for j in range(4):
                    k = k4 * 4 + j
                    S.add("act", lambda e, k=k, j=j, bk=bk, hb=hb: e.activation(
                        out=hb[:, k, :], in_=Bb[bk][:, j * 128:(j + 1) * 128], func=AF.Identity,
                        scale=a1[:, k:k + 1], bias=sh1[:, k:k + 1]),
                        reads=["B%d" % bk, "a1", "sh1"], writes=[htok])
            for cg, (c0, c1) in enumerate(((0, 512), (512, FC))):
                for k in range(32):
                    S.add("pe", lambda e, k=k, cg=cg, c0=c0, c1=c1, hb=hb: e.matmul(
                        B[2 + cg][:, 0:c1 - c0], lhsT=hb[:, k, :], rhs=wF[:, k, c0:c1], start=(k == 0), stop=(k == 31)),
                        reads=[htok, "wF"], writes=["B%d" % (2 + cg)])
            for i in range(4):
                S.add("act", lambda e, i=i: e.activation(out=junk[:, 0:128], in_=B[2][:, i * 128:(i + 1) * 128],
                                                          func=AF.Square, accum_out=sq4[:, i:i + 1]),
                      reads=["B2"], writes=["junk", "sq4"])
            S.add("act", lambda e: e.activation(out=sq4[:], in_=sq4[:], func=AF.Sqrt, scale=1.0 / HD, bias=EPS),
                  reads=["sq4"], writes=["sq4"])
            S.add("dve", lambda e: e.reciprocal(out=sq4[:], in_=sq4[:]), reads=["sq4"], writes=["sq4"])
            for i in range(4):
                S.add("dve", lambda e, i=i: e.tensor_scalar(out=qn[i][:], in0=B[2][:, i * 128:(i + 1) * 128],
                                                             scalar1=sq4[:, i:i + 1], scalar2=None, op0=ALU.mult),
                      reads=["B2", "sq4"], writes=["qn%d" % i])
                S.add("pe", lambda e, i=i: e.transpose(Bb[4][:, i * 128:(i + 1) * 128], qn[i][:], ident[:]),
                      reads=["qn%d" % i, "ident"], writes=["B4"])
                h = i % 2
                dst, gg, gname = (qT[h], gq, "gq") if i < 2 else (kT[h], gk, "gk")
                S.add("act", lambda e, i=i, dst=dst, gg=gg, h=h, ts=ts: e.activation(
                    out=dst[:, ts], in_=Bb[4][:, i * 128:(i + 1) * 128], func=AF.Copy, scale=gg[:, h:h + 1]),
                    reads=["B4", gname], writes=["qk%d" % i])
            for h in range(2):
                S.add("dve", lambda e, h=h, tt=tt: e.tensor_copy(out=Vx[h][:, tt, 0:128], in_=B[3][:, h * 128:(h + 1) * 128]),
                      reads=["B3"], writes=["Vx%d" % h])
            S.add("dve", lambda e, tt=tt: e.tensor_copy(out=fl[:, :, tt], in_=B[3][:, 256:258]),
                  reads=["B3"], writes=["fl"])

        for h in range(2):
            S.add("act", lambda e, h=h: e.activation(out=lf[:, h, :], in_=fl[:, h, :], func=AF.Exp, scale=-1.0,
                                                      bias=fb[:, h:h + 1]), reads=["fl", "fb"], writes=["lf"])
        S.add("act", lambda e: e.activation(out=lf[:], in_=lf[:], func=AF.Ln, scale=1.0, bias=1.0),
              reads=["lf"], writes=["lf"])
        S.add("dve", lambda e: e.tensor_scalar(out=lf[:], in0=lf[:], scalar1=-1.0, scalar2=None, op0=ALU.mult),
              reads=["lf"], writes=["lf"])
        lf2 = lf[:].rearrange("p h t -> p (h t)")
        n2 = 2 * nt
        S.add("pe", lambda e: e.matmul(B[0][:, 0:n2], lhsT=ut[:], rhs=lf2, start=True, stop=True),
              reads=["ut", "lf"], writes=["B0"])
        S.add("pe", lambda e: e.matmul(B[1][:, 0:n2], lhsT=onesf[:], rhs=lf2, start=True, stop=True),
              reads=["onesf", "lf"], writes=["B1"])
        S.add("dve", lambda e: e.tensor_copy(out=tot[:].rearrange("p h t -> p (h t)"), in_=B[1][:, 0:n2]),
              reads=["B1"], writes=["tot"])
        S.add("pool", lambda e: e.memset(off[:], 0.0), writes=["off"])
        for j in range(1, nt):
            S.add("dve", lambda e, j=j: e.tensor_tensor(out=off[:, :, j], in0=off[:, :, j - 1], in1=tot[:, :, j - 1],
                                                        op=ALU.add), reads=["off", "tot"], writes=["off"])
        S.add("dve", lambda e: e.tensor_tensor(out=cum[:].rearrange("p h t -> p (h t)"), in0=B[0][:, 0:n2],
                                               in1=off[:].rearrange("p h t -> p (h t)"), op=ALU.add),
              reads=["B0", "off"], writes=["cum"])

        scale = float(HD) ** -0.5
        for h in range(2):
            S.add("pool", lambda e: e.memset(cum2[:], 0.0), writes=["cum2"])
            S.add("pool", lambda e: e.memset(off2[:], 0.0), writes=["off2"])
            for r in range(2):
                S.add("dve", lambda e, h=h, r=r: e.tensor_copy(out=cum2[:, r * 64:r * 64 + nt], in_=cum[:, h, :]),
                      reads=["cum"], writes=["cum2"])
                S.add("dve", lambda e, h=h, r=r: e.tensor_copy(out=off2[:, r * 64:r * 64 + nt], in_=off[:, h, :]),
                      reads=["off"], writes=["off2"])
            S.add("pe", lambda e: e.transpose(B[0][:, 0:128], cum2[:], identf[:]), reads=["cum2", "identf"], writes=["B0"])
            S.add("pe", lambda e: e.transpose(B[1][:, 0:128], off2[:], identf[:]), reads=["off2", "identf"], writes=["B1"])
            S.add("dve", lambda e: e.tensor_copy(out=ctr[:], in_=B[1][:, 0:128]), reads=["B1"], writes=["ctr"])
            S.add("dve", lambda e: e.tensor_scalar(out=ctf[:], in0=B[0][:, 0:128], scalar1=ctr[:, 0:1], scalar2=1.0 / scale,
                                                   op0=ALU.subtract, op1=ALU.mult), reads=["B0", "ctr"], writes=["ctf"])
            S.add("dve", lambda e: e.tensor_copy(out=cthi[:], in_=ctf[:]), reads=["ctf"], writes=["cthi"])
            S.add("dve", lambda e: e.tensor_tensor(out=ctr[:], in0=ctf[:], in1=cthi[:], op=ALU.subtract),
                  reads=["ctf", "cthi"], writes=["ctr"])
            S.add("dve", lambda e: e.tensor_copy(out=ctlo[:], in_=ctr[:]), reads=["ctr"], writes=["ctlo"])
            S.add("dve", lambda e, h=h: e.tensor_copy(out=CT2[h][0:64, :], in_=cthi[0:64, :]), reads=["cthi"],
                  writes=["CT2_%d" % h])
            S.add("dve", lambda e, h=h: e.tensor_copy(out=CT2[h][64:128, :], in_=ctlo[64:128, :]), reads=["ctlo"],
                  writes=["CT2_%d" % h])
        cnt = 0
        for h in range(2):
            for qb in range(nt):
                qs_ = slice(qb * 128, (qb + 1) * 128)
                S.add("dve", lambda e, h=h, qb=qb: e.tensor_scalar(
                    out=negb[:, 0:qb + 1], in0=cum[:, h, 0:qb + 1], scalar1=-1.0, scalar2=off[:, h, qb:qb + 1],
                    op0=ALU.mult, op1=ALU.add), reads=["cum", "off"], writes=["negb"])
                e2 = E2[qb % 2]; e2tok = "E2_%d" % (qb % 2)
                S.add("dve", lambda e, qb=qb, e2=e2: e.tensor_tensor(
                    out=e2[:], in0=ident[:, qb:qb + 1].to_broadcast([128, 128]),
                    in1=ident[:, 64 + qb:65 + qb].to_broadcast([128, 128]), op=ALU.add),
                    reads=["ident"], writes=[e2tok])
                ab = 6 + (qb % 2)
                for kb in range(qb + 1):
                    sbk = 4 + (cnt % 2)
                    pt = pT[cnt % 3]
                    ptok = "pT%d" % (cnt % 3)
                    cnt += 1
                    S.add("pe", lambda e, h=h, kb=kb, qs_=qs_, sbk=sbk: e.matmul(
                        B[sbk][:, 0:128], lhsT=kT[h][:, kb * 128:(kb + 1) * 128], rhs=qT[h][:, qs_],
                        start=True, stop=False), reads=["qk%d" % h, "qk%d" % (2 + h)], writes=["B%d" % sbk])
                    S.add("pe", lambda e, h=h, sbk=sbk, e2=e2, kb=kb, qb=qb: e.matmul(
                        B[sbk][:, 0:128], lhsT=e2[:], rhs=CT2[h][:], start=False, stop=(kb != qb)),
                        reads=[e2tok, "CT2_%d" % h], writes=["B%d" % sbk])
                    if kb == qb:
                        S.add("pe", lambda e, sbk=sbk: e.matmul(B[sbk][:, 0:128], lhsT=ident[:], rhs=negm[:], start=False,
                                                                stop=True), reads=["ident", "negm"], writes=["B%d" % sbk])
                    S.add("act", lambda e, kb=kb, sbk=sbk, pt=pt: e.activation(
                        out=pt[:], in_=B[sbk][:, 0:128], func=AF.Exp, scale=scale, bias=negb[:, kb:kb + 1]),
                        reads=["B%d" % sbk, "negb"], writes=[ptok])
                    S.add("pe", lambda e, h=h, kb=kb, pt=pt, ab=ab, qb=qb: e.matmul(
                        B[ab][:, 0:129], lhsT=pt[:], rhs=Vx[h][:, kb, :], start=(kb == 0), stop=(kb == qb)),
                        reads=[ptok, "Vx%d" % h], writes=["B%d" % ab])
                S.add("dve", lambda e, ab=ab: e.reciprocal(out=rden[:], in_=B[ab][:, 128:129]),
                      reads=["B%d" % ab], writes=["rden"])
                S.add("dve", lambda e, ab=ab: e.tensor_scalar(out=on[:], in0=B[ab][:, 0:128], scalar1=rden[:, 0:1],
                                                              scalar2=None, op0=ALU.mult),
                      reads=["B%d" % ab, "rden"], writes=["on"])
                S.add("pe", lambda e: e.transpose(Bb[0][:, 0:128], on[:], ident[:]), reads=["on", "ident"], writes=["B0"])
                ob = ost[qb % 2]
                S.add("act", lambda e, ob=ob: e.activation(out=ob[:], in_=Bb[0][:, 0:128], func=AF.Copy),
                      reads=["B0"], writes=["ost%d" % (qb % 2)])
                S.dma("sp", out[h * 128:(h + 1) * 128, qs_], ob[:], reads=["ost%d" % (qb % 2)], writes=["out"])
        _finish(S, st, ["out"])
    return nc


def _pk(v):
    return np.ascontiguousarray(np.asarray(v, np.float32).reshape(32, 128).T)


def fox_maps(x2, mod, norm1_gain, w_in, fox_f_bias, fox_q_gain, fox_k_gain):
    idb, ut, utb = _consts()
    maps = []
    for c in range(NCORES):
        hs = [2 * c, 2 * c + 1]
        cols = np.concatenate([np.arange(256 * c, 256 * c + 256), 2048 + np.arange(256 * c, 256 * c + 256),
                               4096 + np.arange(256 * c, 256 * c + 256), 6144 + np.array(hs)])
        maps.append({
            "x": x2, "g1": _pk(norm1_gain[0]), "sc1": _pk(mod[D:2 * D]), "sh1": _pk(mod[0:D]),
            "w": np.ascontiguousarray(w_in[0][:, cols]),
            "fb": np.ascontiguousarray(np.broadcast_to(fox_f_bias[0][hs][None, :], (128, 2))).astype(np.float32),
            "gq": np.ascontiguousarray(fox_q_gain[0][hs].T), "gk": np.ascontiguousarray(fox_k_gain[0][hs].T),
            "ident": idb, "ut": ut, "utb": utb, "identf": np.eye(128, dtype=np.float32),
            "negm": (np.tril(np.ones((128, 128), np.float32), -1) * -30000.0).astype(ml_dtypes.bfloat16),
        })
    return maps


HC = 1024


def build_hgrn(nt=NT):
    nc = bass.Bass("TRN2", target_bir_lowering=False)
    seq = nt * 128
    din = lambda n, s, d: nc.dram_tensor(n, s, d, kind="ExternalInput").ap()
    x_in = din("x", [seq, D], F32)
    g1_in = din("g1", [128, 32], F32); sc_in = din("sc1", [128, 32], F32); sh_in = din("sh1", [128, 32], F32)
    w_in = din("w", [D, HC], F32)
    lb0_in = din("lb0", [128, 256], F32); lb1_in = din("lb1", [128, 256], F32); og_in = din("og", [128, 256], F32)
    id_in = din("ident", [128, 128], BF16)
    tri_in = din("tri", [128, 128], BF16); stri_in = din("stri", [128, 128], BF16); m_in = din("m128", [128, 128], F32)
    c01_in = din("c01", [128, 2], F32)
    out = nc.dram_tensor("mT", [256, seq], BF16, kind="ExternalOutput").ap()
    wv = w_in.rearrange("(k p) n -> k p n", p=128)
    S = Sched(nc)
    with ExitStack() as st:
        sb = lambda n, s, d: st.enter_context(nc.sbuf_tensor(n + "_s", s, d))
        wH = sb("wH", [128, 32, HC], BF16)
        wst = [sb("wst%d" % i, [128, HC], F32) for i in range(2)]
        xb = sb("xb", [128, D], F32); xs = sb("xs", [128, D], BF16)
        hT = [sb("hT%d" % i, [128, 32, 128], BF16) for i in range(2)]
        ss = sb("ss", [128, nt], F32)
        a1 = sb("a1", [128, 32], F32); sh1 = sb("sh1", [128, 32], F32); g1 = sb("g1", [128, 32], F32)
        lbb = sb("lbb", [128, 256], F32); omlb = sb("omlb", [128, 256], F32); ogb = sb("ogb", [128, 256], F32)
        ident = sb("ident", [128, 128], BF16)
        tri = sb("tri", [128, 128], BF16); stri = sb("stri", [128, 128], BF16); m128 = sb("m128", [128, 128], F32)
        c01 = sb("c01", [128, 2], F32)
        junk = sb("junk", [128, D], BF16)
        sg = sb("sg", [128, 256], F32); gl = sb("gl", [128, 256], F32)
        ghi = sb("ghi", [128, 256], BF16); glo = sb("glo", [128, 256], BF16); gr = sb("gr", [128, 256], F32)
        omfb = sb("omfb", [128, 256], BF16); qsb = sb("qsb", [128, 256], BF16)
        gs = sb("gs", [128, 256], F32); vHb = sb("vHb", [128, 256], BF16)
        eb = sb("eb", [128, 128], F32); enb = sb("enb", [128, 128], F32); er = sb("er", [128, 128], F32)
        QtT = sb("QtT", [128, 128], BF16); Qt0 = sb("Qt0", [128, 128], BF16); Qt1 = sb("Qt1", [128, 128], BF16)
        KtT = sb("KtT", [128, 128], BF16)
        Kh = sb("Kh", [128, 128], BF16); Kh0 = sb("Kh0", [128, 128], BF16); Kh1 = sb("Kh1", [128, 128], BF16)
        scm = sb("scm", [128, 128], BF16)
        Sst = [sb("S%d" % h, [128, 128], F32) for h in range(2)]
        Sbf = [sb("Sbf%d" % h, [128, 128], BF16) for h in range(2)]
        so = sb("so", [128, 1], F32); on1 = sb("on1", [128, 128], F32); on3 = sb("on3", [128, 128], BF16)
        ost = [sb("ost%d" % i, [128, 128], BF16) for i in range(2)]
        B = [st.enter_context(nc.psum_tensor("B%d" % i, [128, 512], F32)) for i in range(8)]
        Bb = [b[:].bitcast(BF16) for b in B]

        for (t, src, name) in ((g1, g1_in, "g1"), (a1, sc_in, "a1"), (sh1, sh_in, "sh1"), (lbb, lb0_in, "lbb"),
                               (omlb, lb1_in, "omlb"), (ogb, og_in, "ogb"), (ident, id_in, "ident"),
                               (tri, tri_in, "tri"), (stri, stri_in, "stri"), (m128, m_in, "m128"), (c01, c01_in, "c01")):
            S.dma("sp", t[:], src[:, :], writes=[name])
        S.add("dve", lambda e: e.scalar_tensor_tensor(out=a1[:], in0=a1[:], scalar=1.0, in1=g1[:], op0=ALU.add,
                                                      op1=ALU.mult), reads=["a1", "g1"], writes=["a1"])
        S.add("dve", lambda e: e.tensor_tensor(out=lbb[:], in0=lbb[:], in1=omlb[:], op=ALU.subtract),
              reads=["lbb", "omlb"], writes=["lbb"])
        S.add("act", lambda e: e.activation(out=lbb[:], in_=lbb[:], func=AF.Sigmoid), reads=["lbb"], writes=["lbb"])
        S.add("dve", lambda e: e.tensor_scalar(out=omlb[:], in0=lbb[:], scalar1=-1.0, scalar2=1.0, op0=ALU.mult,
                                               op1=ALU.add), reads=["lbb"], writes=["omlb"])
        for h in range(2):
            S.add("pool", lambda e, h=h: e.memset(Sst[h][:], 0.0), writes=["S%d" % h])
            S.add("pool", lambda e, h=h: e.memset(Sbf[h][:], 0.0), writes=["Sbf%d" % h])
        S.add("pool", lambda e: e.memset(Qt0[:], 0.0), writes=["Qt0"])
        S.add("pool", lambda e: e.memset(Qt1[:], 0.0), writes=["Qt1"])
        for k in range(32):
            S.dma("sp", wst[k % 2][:], wv[k], writes=["wst%d" % (k % 2)])
            eng = ("act", "dve", "pool")[k % 3]
            if eng == "act":
                S.add("act", lambda e, k=k: e.activation(out=wH[:, k, :], in_=wst[k % 2][:], func=AF.Copy),
                      reads=["wst%d" % (k % 2)], writes=["wH"])
            else:
                S.add(eng, lambda e, k=k: e.tensor_copy(out=wH[:, k, :], in_=wst[k % 2][:]),
                      reads=["wst%d" % (k % 2)], writes=["wH"])

        oc = 0
        for tt in range(nt):
            ts = slice(tt * 128, (tt + 1) * 128)
            S.dma("sp", xb[:], x_in[ts, :], writes=["xb"])
            S.add("act", lambda e, tt=tt: e.activation(out=junk[:], in_=xb[:], func=AF.Square,
                                                        accum_out=ss[:, tt:tt + 1]), reads=["xb"], writes=["junk", "ss"])
            S.add("act", lambda e, tt=tt: e.activation(out=ss[:, tt:tt + 1], in_=ss[:, tt:tt + 1], func=AF.Sqrt,
                                                        scale=1.0 / D, bias=EPS), reads=["ss"], writes=["ss"])
            S.add("dve", lambda e, tt=tt: e.reciprocal(out=ss[:, tt:tt + 1], in_=ss[:, tt:tt + 1]),
                  reads=["ss"], writes=["ss"])
            S.add("dve", lambda e, tt=tt: e.tensor_scalar(out=xs[:], in0=xb[:], scalar1=ss[:, tt:tt + 1], scalar2=None,
                                                           op0=ALU.mult), reads=["xb", "ss"], writes=["xs"])
            hb = hT[tt % 2]
            htok = "hT%d" % (tt % 2)
            for k4 in range(8):
                bk = k4 % 2
                for j in range(4):
                    k = k4 * 4 + j
                    S.add("pe", lambda e, k=k, j=j, bk=bk: e.transpose(Bb[bk][:, j * 128:(j + 1) * 128],
                                                                       xs[:, k * 128:(k + 1) * 128], ident[:]),
                          reads=["xs", "ident"], writes=["B%d" % bk])
                for j in range(4):
                    k = k4 * 4 + j
                    S.add("act", lambda e, k=k, j=j, bk=bk, hb=hb: e.activation(
                        out=hb[:, k, :], in_=Bb[bk][:, j * 128:(j + 1) * 128], func=AF.Identity,
                        scale=a1[:, k:k + 1], bias=sh1[:, k:k + 1]),
                        reads=["B%d" % bk, "a1", "sh1"], writes=[htok])
            for cg in range(2):
                for k in range(32):
                    S.add("pe", lambda e, k=k, cg=cg, hb=hb: e.matmul(
                        B[2 + cg][:, :], lhsT=hb[:, k, :], rhs=wH[:, k, cg * 512:(cg + 1) * 512],
                        start=(k == 0), stop=(k == 31)), reads=[htok, "wH"], writes=["B%d" % (2 + cg)])
            S.add("act", lambda e: e.activation(out=sg[:], in_=B[2][:, 256:512], func=AF.Sigmoid),
                  reads=["B2"], writes=["sg"])
            S.add("dve", lambda e: e.tensor_tensor(out=sg[:], in0=sg[:], in1=omlb[:], op=ALU.mult),
                  reads=["sg", "omlb"], writes=["sg"])
            S.add("dve", lambda e: e.tensor_tensor(out=sg[:], in0=sg[:], in1=lbb[:], op=ALU.add),
                  reads=["sg", "lbb"], writes=["sg"])
            S.add("act", lambda e: e.activation(out=gl[:], in_=sg[:], func=AF.Ln), reads=["sg"], writes=["gl"])
            S.add("dve", lambda e: e.tensor_copy(out=ghi[:], in_=gl[:]), reads=["gl"], writes=["ghi"])
            S.add("dve", lambda e: e.tensor_tensor(out=gr[:], in0=gl[:], in1=ghi[:], op=ALU.subtract),
                  reads=["gl", "ghi"], writes=["gr"])
            S.add("dve", lambda e: e.tensor_copy(out=glo[:], in_=gr[:]), reads=["gr"], writes=["glo"])
            S.add("dve", lambda e: e.tensor_scalar(out=omfb[:], in0=sg[:], scalar1=-1.0, scalar2=1.0, op0=ALU.mult,
                                                   op1=ALU.add), reads=["sg"], writes=["omfb"])
            S.add("act", lambda e: e.activation(out=qsb[:], in_=B[2][:, 0:256], func=AF.Silu),
                  reads=["B2"], writes=["qsb"])
            S.add("act", lambda e: e.activation(out=gs[:], in_=B[3][:, 256:512], func=AF.Silu),
                  reads=["B3"], writes=["gs"])
            S.add("act", lambda e: e.activation(out=vHb[:], in_=B[3][:, 0:256], func=AF.Copy),
                  reads=["B3"], writes=["vHb"])
            for h in range(2):
                hc = slice(h * 128, (h + 1) * 128)
                S.add("pe", lambda e, hc=hc: e.matmul(B[4][:, 0:128], lhsT=ghi[:, hc], rhs=tri[:], start=True, stop=False),
                      reads=["ghi", "tri"], writes=["B4"])
                S.add("pe", lambda e, hc=hc: e.matmul(B[4][:, 0:128], lhsT=glo[:, hc], rhs=tri[:], start=False, stop=True),
                      reads=["glo", "tri"], writes=["B4"])
                S.add("pe", lambda e, hc=hc: e.matmul(B[5][:, 0:128], lhsT=stri[:], rhs=ghi[:, hc], start=True,
                                                      stop=False), reads=["ghi", "stri"], writes=["B5"])
                S.add("pe", lambda e, hc=hc: e.matmul(B[5][:, 0:128], lhsT=stri[:], rhs=glo[:, hc], start=False,
                                                      stop=True), reads=["glo", "stri"], writes=["B5"])
                S.add("act", lambda e: e.activation(out=eb[:], in_=B[4][:, 0:128], func=AF.Exp), reads=["B4"], writes=["eb"])
                S.add("act", lambda e: e.activation(out=enb[:], in_=B[4][:, 0:128], func=AF.Exp, scale=-1.0),
                      reads=["B4"], writes=["enb"])
                S.add("act", lambda e: e.activation(out=er[:], in_=B[5][:, 0:128], func=AF.Exp),
                      reads=["B5"], writes=["er"])
                S.add("pe", lambda e, hc=hc: e.transpose(Bb[0][:, 0:128], qsb[:, hc], ident[:]),
                      reads=["qsb", "ident"], writes=["B0"])
                S.add("pe", lambda e, hc=hc: e.transpose(Bb[1][:, 0:128], omfb[:, hc], ident[:]),
                      reads=["omfb", "ident"], writes=["B1"])
                S.add("dve", lambda e: e.tensor_tensor(out=QtT[:], in0=Bb[0][:, 0:128], in1=eb[:], op=ALU.mult),
                      reads=["B0", "eb"], writes=["QtT"])
                S.add("pool", lambda e: e.tensor_copy(out=Qt0[:, 0:64], in_=QtT[:, 0:64]), reads=["QtT"], writes=["Qt0"])
                S.add("pool", lambda e: e.tensor_copy(out=Qt1[:, 64:128], in_=QtT[:, 64:128]), reads=["QtT"], writes=["Qt1"])
                S.add("dve", lambda e: e.tensor_tensor(out=KtT[:], in0=Bb[1][:, 0:128], in1=enb[:], op=ALU.mult),
                      reads=["B1", "enb"], writes=["KtT"])
                S.add("dve", lambda e, hc=hc: e.tensor_tensor(out=Kh[:], in0=omfb[:, hc], in1=er[:], op=ALU.mult),
                      reads=["omfb", "er"], writes=["Kh"])
                S.add("dve", lambda e: e.tensor_scalar(out=Kh0[:], in0=Kh[:], scalar1=c01[:, 0:1], scalar2=None,
                                                       op0=ALU.mult), reads=["Kh", "c01"], writes=["Kh0"])
                S.add("dve", lambda e: e.tensor_scalar(out=Kh1[:], in0=Kh[:], scalar1=c01[:, 1:2], scalar2=None,
                                                       op0=ALU.mult), reads=["Kh", "c01"], writes=["Kh1"])
                S.add("pe", lambda e: e.matmul(B[6][:, 0:128], lhsT=KtT[:], rhs=QtT[:], start=True, stop=True),
                      reads=["KtT", "QtT"], writes=["B6"])
                S.add("dve", lambda e: e.tensor_tensor(out=scm[:], in0=B[6][:, 0:128], in1=m128[:], op=ALU.mult),
                      reads=["B6", "m128"], writes=["scm"])
                S.add("pe", lambda e, hc=hc: e.matmul(B[7][:, 0:128], lhsT=scm[:], rhs=vHb[:, hc], start=True,
                                                      stop=False), reads=["scm", "vHb"], writes=["B7"])
                S.add("pe", lambda e, h=h: e.matmul(B[7][:, 0:128], lhsT=Qt0[:], rhs=Sbf[h][:], start=False,
                                                    stop=False), reads=["Qt0", "Sbf%d" % h], writes=["B7"])
                S.add("pe", lambda e, hc=hc: e.matmul(B[5][:, 256:384], lhsT=Kh0[:], rhs=vHb[:, hc], start=True,
                                                      stop=True), reads=["Kh0", "vHb"], writes=["B5"])
                S.add("dve", lambda e, h=h: e.scalar_tensor_tensor(out=Sst[h][:], in0=Sst[h][:], scalar=eb[:, 63:64],
                                                                   in1=B[5][:, 256:384], op0=ALU.mult, op1=ALU.add),
                      reads=["S%d" % h, "eb", "B5"], writes=["S%d" % h])
                S.add("act", lambda e, h=h: e.activation(out=Sbf[h][:], in_=Sst[h][:], func=AF.Copy),
                      reads=["S%d" % h], writes=["Sbf%d" % h])
                S.add("pe", lambda e, h=h: e.matmul(B[7][:, 0:128], lhsT=Qt1[:], rhs=Sbf[h][:], start=False,
                                                    stop=True), reads=["Qt1", "Sbf%d" % h], writes=["B7"])
                S.add("pe", lambda e, hc=hc: e.matmul(B[5][:, 384:512], lhsT=Kh1[:], rhs=vHb[:, hc], start=True,
                                                      stop=True), reads=["Kh1", "vHb"], writes=["B5"])
                S.add("dve", lambda e, h=h: e.scalar_tensor_tensor(out=Sst[h][:], in0=Sst[h][:], scalar=eb[:, 127:128],
                                                                   in1=B[5][:, 384:512], op0=ALU.mult, op1=ALU.add),
                      reads=["S%d" % h, "eb", "B5"], writes=["S%d" % h])
                S.add("act", lambda e, h=h: e.activation(out=Sbf[h][:], in_=Sst[h][:], func=AF.Copy),
                      reads=["S%d" % h], writes=["Sbf%d" % h])
                S.add("act", lambda e: e.activation(out=junk[:, 0:128], in_=B[7][:, 0:128], func=AF.Square,
                                                    accum_out=so[:, 0:1]), reads=["B7"], writes=["junk", "so"])
                S.add("act", lambda e: e.activation(out=so[:], in_=so[:], func=AF.Sqrt, scale=1.0 / HD,
                                                    bias=EPS * HD), reads=["so"], writes=["so"])
                S.add("dve", lambda e: e.reciprocal(out=so[:], in_=so[:]), reads=["so"], writes=["so"])
                S.add("dve", lambda e, hc=hc: e.scalar_tensor_tensor(out=on1[:], in0=B[7][:, 0:128], scalar=so[:, 0:1],
                                                                     in1=ogb[:, hc], op0=ALU.mult, op1=ALU.mult),
                      reads=["B7", "so", "ogb"], writes=["on1"])
                S.add("dve", lambda e, hc=hc: e.tensor_tensor(out=on3[:], in0=on1[:], in1=gs[:, hc], op=ALU.mult),
                      reads=["on1", "gs"], writes=["on3"])
                S.add("pe", lambda e: e.transpose(Bb[0][:, 128:256], on3[:], ident[:]),
                      reads=["on3", "ident"], writes=["B0"])
                ob = ost[oc % 2]
                otok = "ost%d" % (oc % 2)
                oc += 1
                S.add("act", lambda e, ob=ob: e.activation(out=ob[:], in_=Bb[0][:, 128:256], func=AF.Copy),
                      reads=["B0"], writes=[otok])
                S.dma("sp", out[h * 128:(h + 1) * 128, ts], ob[:], reads=[otok], writes=["out"])
        _finish(S, st, ["out"])
    return nc


def hgrn_maps(x2, mod, norm1_gain, w_in, hgrn_lower_bounds, hgrn_out_gain):
    idb, _, _ = _consts()
    t64 = np.triu(np.ones((64, 64), np.float32))
    s64 = np.tril(np.ones((64, 64), np.float32), -1)
    z = np.zeros((64, 64), np.float32)
    tri = np.block([[t64, z], [z, t64]]); stri = np.block([[s64, z], [z, s64]])
    c01 = np.zeros((128, 2), np.float32); c01[:64, 0] = 1.0; c01[64:, 1] = 1.0
    maps = []
    o4 = 3 * 2048 + 16
    for c in range(NCORES):
        cr = np.arange(256 * c, 256 * c + 256)
        cols = np.concatenate([o4 + cr, o4 + 2048 + cr, o4 + 4096 + cr, o4 + 6144 + cr])
        bc = lambda v: np.ascontiguousarray(np.broadcast_to(np.asarray(v, np.float32)[None, :], (128, 256)))
        maps.append({
            "x": x2, "g1": _pk(norm1_gain[0]), "sc1": _pk(mod[D:2 * D]), "sh1": _pk(mod[0:D]),
            "w": np.ascontiguousarray(w_in[0][:, cols]),
            "lb0": bc(hgrn_lower_bounds[0][cr]), "lb1": bc(hgrn_lower_bounds[1][cr]),
            "og": bc(hgrn_out_gain[0].reshape(-1)[cr]),
            "ident": idb, "tri": tri.astype(ml_dtypes.bfloat16), "stri": stri.astype(ml_dtypes.bfloat16),
            "m128": tri, "c01": c01,
        })
    return maps


TOK = SEQ // NCORES


def build_c1(ntl=TOK // 128):
    nc = bass.Bass("TRN2", target_bir_lowering=False)
    tok = ntl * 128
    din = lambda n, s, d: nc.dram_tensor(n, s, d, kind="ExternalInput").ap()
    x_in = din("x", [tok, D], F32)
    m_in = din("mT", [D, tok], BF16)
    w_in = din("w", [D, D], F32)
    gt_in = din("gate1", [128, D], F32)
    g2_in = din("g2", [128, 32], F32); sc_in = din("sc2", [128, 32], F32); sh_in = din("sh2", [128, 32], F32)
    id_in = din("ident", [128, 128], BF16)
    x1_out = nc.dram_tensor("x1", [tok, D], F32, kind="ExternalOutput").ap()
    h2_out = nc.dram_tensor("h2T", [D, tok], BF16, kind="ExternalOutput").ap()
    wv = w_in.rearrange("(k p) n -> k p n", p=128)
    mv = m_in.rearrange("(k p) t -> p k t", p=128)
    h2v = h2_out.rearrange("(k p) t -> p k t", p=128)
    S = Sched(nc)
    with ExitStack() as st:
        sb = lambda n, s, d: st.enter_context(nc.sbuf_tensor(n + "_s", s, d))
        wob = sb("wob", [128, 32, 512], BF16)
        wst = [sb("wst%d" % i, [128, 512], F32) for i in range(3)]
        mt = [sb("mt%d" % i, [128, 32, 128], BF16) for i in range(2)]
        xc = [sb("xc%d" % i, [128, 512], F32) for i in range(2)]
        oc_ = [sb("oc%d" % i, [128, 512], F32) for i in range(2)]
        gt = sb("gt", [128, D], F32)
        a2 = sb("a2", [128, 32], F32); sh2 = sb("sh2", [128, 32], F32); g2 = sb("g2", [128, 32], F32)
        ident = sb("ident", [128, 128], BF16)
        xb = sb("xb", [128, D], F32); xs = sb("xs", [128, D], BF16); junk = sb("junk", [128, D], BF16)
        ss = sb("ss", [128, ntl], F32)
        hT = [sb("hT%d" % i, [128, 32, 128], BF16) for i in range(2)]
        B = [st.enter_context(nc.psum_tensor("B%d" % i, [128, 512], F32)) for i in range(8)]
        Bb = [b[:].bitcast(BF16) for b in B]
        for (t, src, name) in ((g2, g2_in, "g2"), (a2, sc_in, "a2"), (sh2, sh_in, "sh2"), (ident, id_in, "ident"),
                               (gt, gt_in, "gt")):
            S.dma("sp", t[:], src[:, :], writes=[name])
        S.add("dve", lambda e: e.scalar_tensor_tensor(out=a2[:], in0=a2[:], scalar=1.0, in1=g2[:], op0=ALU.add,
                                                      op1=ALU.mult), reads=["a2", "g2"], writes=["a2"])
        n = 0
        for cg in range(8):
            cs = slice(cg * 512, (cg + 1) * 512)
            for k in range(32):
                S.dma("sp", wst[k % 3][:], wv[k][:, cs], writes=["wst%d" % (k % 3)])
                eng = ("act", "dve", "pool")[k % 3]
                if eng == "act":
                    S.add("act", lambda e, k=k: e.activation(out=wob[:, k, :], in_=wst[k % 3][:], func=AF.Copy),
                          reads=["wst%d" % (k % 3)], writes=["wob"])
                else:
                    S.add(eng, lambda e, k=k: e.tensor_copy(out=wob[:, k, :], in_=wst[k % 3][:]),
                          reads=["wst%d" % (k % 3)], writes=["wob"])
            for tt in range(ntl):
                ts = slice(tt * 128, (tt + 1) * 128)
                mb = mt[n % 2]; mtok = "mt%d" % (n % 2)
                xcb = xc[n % 2]; xtok = "xc%d" % (n % 2)
                ob = oc_[n % 2]; otok = "oc%d" % (n % 2)
                bk = 2 + (n % 2)
                n += 1
                S.dma("sp", mb[:], mv[:, :, ts], writes=[mtok])
                S.dma("sp", xcb[:], x_in[ts, cs], writes=[xtok])
                for k in range(32):
                    S.add("pe", lambda e, k=k, mb=mb, bk=bk: e.matmul(B[bk][:, :], lhsT=mb[:, k, :], rhs=wob[:, k, :],
                                                                     start=(k == 0), stop=(k == 31)),
                          reads=[mtok, "wob"], writes=["B%d" % bk])
                S.add("dve", lambda e, ob=ob, bk=bk, cs=cs: e.tensor_tensor(out=ob[:], in0=B[bk][:, :], in1=gt[:, cs],
                                                                           op=ALU.mult),
                      reads=["B%d" % bk, "gt"], writes=[otok])
                S.add("pool", lambda e, ob=ob, xcb=xcb: e.tensor_tensor(out=ob[:], in0=ob[:], in1=xcb[:], op=ALU.add),
                      reads=[otok, xtok], writes=[otok])
                S.dma("sp", x1_out[ts, cs], ob[:], reads=[otok], writes=["x1"])
        for tt in range(ntl):
            ts = slice(tt * 128, (tt + 1) * 128)
            S.dma("sp", xb[:], x1_out[ts, :], reads=["x1"], writes=["xb"])
            S.add("act", lambda e, tt=tt: e.activation(out=junk[:], in_=xb[:], func=AF.Square,
                                                        accum_out=ss[:, tt:tt + 1]), reads=["xb"], writes=["junk", "ss"])
            S.add("act", lambda e, tt=tt: e.activation(out=ss[:, tt:tt + 1], in_=ss[:, tt:tt + 1], func=AF.Sqrt,
                                                        scale=1.0 / D, bias=EPS), reads=["ss"], writes=["ss"])
            S.add("dve", lambda e, tt=tt: e.reciprocal(out=ss[:, tt:tt + 1], in_=ss[:, tt:tt + 1]),
                  reads=["ss"], writes=["ss"])
            S.add("dve", lambda e, tt=tt: e.tensor_scalar(out=xs[:], in0=xb[:], scalar1=ss[:, tt:tt + 1], scalar2=None,
                                                           op0=ALU.mult), reads=["xb", "ss"], writes=["xs"])
            hb = hT[tt % 2]
            htok = "hT%d" % (tt % 2)
            for k4 in range(8):
                bk = k4 % 2
                for j in range(4):
                    k = k4 * 4 + j
                    S.add("pe", lambda e, k=k, j=j, bk=bk: e.transpose(Bb[bk][:, j * 128:(j + 1) * 128],
                                                                       xs[:, k * 128:(k + 1) * 128], ident[:]),
                          reads=["xs", "ident"], writes=["B%d" % bk])
                for j in range(4):
                    k = k4 * 4 + j
                    S.add("act", lambda e, k=k, j=j, bk=bk, hb=hb: e.activation(
                        out=hb[:, k, :], in_=Bb[bk][:, j * 128:(j + 1) * 128], func=AF.Identity,
                        scale=a2[:, k:k + 1], bias=sh2[:, k:k + 1]),
                        reads=["B%d" % bk, "a2", "sh2"], writes=[htok])
            S.dma("sp", h2v[:, :, ts], hb[:], reads=[htok], writes=["h2"])
        _finish(S, st, ["x1", "h2"])
    return nc


def c1_maps(x2, mergedT, mod, norm2_gain, w_out):
    idb, _, _ = _consts()
    maps = []
    gate1 = np.ascontiguousarray(np.broadcast_to(mod[2 * D:3 * D][None, :], (128, D))).astype(np.float32)
    wo = np.ascontiguousarray(w_out[0])
    for c in range(NCORES):
        ts = slice(c * TOK, (c + 1) * TOK)
        maps.append({"x": np.ascontiguousarray(x2[ts]), "mT": np.ascontiguousarray(mergedT[:, ts]), "w": wo,
                     "gate1": gate1, "g2": _pk(norm2_gain[0]), "sc2": _pk(mod[4 * D:5 * D]), "sh2": _pk(mod[3 * D:4 * D]),
                     "ident": idb})
    return maps


NEXP_B = 128


def build_c2(nq=4, tq=2, nb=NEXP_B):
    nc = bass.Bass("TRN2", target_bir_lowering=False)
    tokq = tq * 128
    tok = nq * tokq
    din = lambda n, s, d: nc.dram_tensor(n, s, d, kind="ExternalInput").ap()
    h_in = din("h2T", [D, tok], BF16)
    x1_in = din("x1", [tok, D], F32)
    g2_in = din("gate2", [128, D], F32)
    wq_in = din("wq", [D, 2048], F32)
    kt_in = din("keysT", [128, 16, 128], F32)
    ut_in = din("UT", [D, nb * 128], F32)
    v_in = din("V", [nb * 128, D], F32)
    id_in = din("ident", [128, 128], BF16)
    y_out = nc.dram_tensor("y", [tok, D], F32, kind="ExternalOutput").ap()
    hv = h_in.rearrange("(k p) t -> p k t", p=128)
    wqv = wq_in.rearrange("(k p) n -> p k n", p=128)
    utv = ut_in.rearrange("(k p) e -> p k e", p=128)
    S = Sched(nc)
    with ExitStack() as st:
        sb = lambda n, s, d: st.enter_context(nc.sbuf_tensor(n + "_s", s, d))
        hh = sb("hh", [128, 32, tokq], BF16)
        st16 = sb("st16", [128, 32, 128], F32)
        st16f = st16[:].rearrange("p k e -> p (k e)")
        w8 = sb("w8", [128, 32, 128], BF16)
        qpT = sb("qpT", [128, 16, tokq], BF16)
        ktf = sb("ktf", [128, 16, 128], F32); kTb = sb("kTb", [128, 16, 128], BF16)
        sc = sb("sc", [128, 16, 128], F32)
        mx = sb("mx", [128, 16, 16], F32); tmpv = sb("tmpv", [128, 128], F32)
        cand = sb("cand", [128, 8, 256], F32); tmpc = sb("tmpc", [128, 256], F32)
        c16 = sb("c16", [128, 8, 16], F32)
        th = sb("th", [128, 8], F32); mm = sb("mm", [128, 8], F32); nm = sb("nm", [128, 8], F32)
        zz = sb("zz", [128, 8], F32); m2 = sb("m2", [128, 8], F32); dm = sb("dm", [128, 8], F32)
        ej = sb("ej", [128, 16], F32)
        L2 = [sb("L2_%d" % i, [128, 8, 128], F32) for i in range(tq)]
        D1 = [sb("D1_%d" % i, [128, 8, 128], F32) for i in range(tq)]
        e1 = [sb("e1_%d" % i, [128, 8], F32) for i in range(tq)]
        acc = sb("acc", [128, tq, D], F32)
        vst = sb("vst", [128, D], F32); vb = sb("vb", [128, D], BF16)
        ga = sb("ga", [128, tokq], F32)
        Xt = sb("Xt", [128, 8, 128], F32); X2 = sb("X2", [128, 8, 128], F32)
        Ee = sb("Ee", [128, 8, 128], F32); Gh = sb("Gh", [128, 8, 128], F32)
        Gt = sb("Gt", [128, 128], BF16); Gtf = sb("Gtf", [128, 128], F32)
        wT = sb("wT", [128, tokq], BF16)
        ident = sb("ident", [128, 128], BF16)
        B = [st.enter_context(nc.psum_tensor("B%d" % i, [128, 512], F32)) for i in range(8)]
        Bb = [b[:].bitcast(BF16) for b in B]

        S.dma("sp", ident[:], id_in[:, :], writes=["ident"])
        S.dma("sp", ktf[:], kt_in[:, :, :], writes=["ktf"])
        S.add("dve", lambda e: e.tensor_copy(out=kTb[:], in_=ktf[:]), reads=["ktf"], writes=["kTb"])
        for qi in range(nq):
            t0 = qi * tokq
            S.dma("sp", hh[:], hv[:, :, t0:t0 + tokq], writes=["hh"])
            for hp in range(16):
                S.dma("sp", st16[:], wqv[:, :, hp * 128:(hp + 1) * 128], writes=["st16"])
                S.add("act", lambda e: e.activation(out=w8[:], in_=st16[:], func=AF.Copy), reads=["st16"], writes=["w8"])
                bk = 2 + hp % 2
                for k in range(32):
                    S.add("pe", lambda e, k=k, bk=bk: e.matmul(B[bk][:, 0:tokq], lhsT=w8[:, k, :], rhs=hh[:, k, :],
                                                              start=(k == 0), stop=(k == 31)),
                          reads=["w8", "hh"], writes=["B%d" % bk])
                S.add("dve", lambda e, hp=hp, bk=bk: e.tensor_copy(out=qpT[:, hp, :], in_=B[bk][:, 0:tokq]),
                      reads=["B%d" % bk], writes=["qpT"])
            for tt in range(tq):
                tsl = slice(tt * 128, (tt + 1) * 128)
                for hp in range(16):
                    bk = 4 + hp // 4
                    S.add("pe", lambda e, hp=hp, bk=bk, tsl=tsl: e.matmul(
                        B[bk][:, (hp % 4) * 128:(hp % 4 + 1) * 128], lhsT=qpT[:, hp, tsl], rhs=kTb[:, hp, :],
                        start=True, stop=True), reads=["qpT", "kTb"], writes=["B%d" % bk])
                for g in range(4):
                    S.add("act", lambda e, g=g: e.activation(
                        out=sc[:, g * 4:(g + 1) * 4, :].rearrange("p a n -> p (a n)"), in_=B[4 + g][:, :], func=AF.Copy),
                        reads=["B%d" % (4 + g)], writes=["sc"])
                for hp in range(16):
                    S.add("dve", lambda e, hp=hp: e.max(out=mx[:, hp, 0:8], in_=sc[:, hp, :]), reads=["sc"], writes=["mx"])
                    S.add("dve", lambda e, hp=hp: e.match_replace(out=tmpv[:], in_to_replace=mx[:, hp, 0:8],
                                                                  in_values=sc[:, hp, :], imm_value=-1e30),
                          reads=["sc", "mx"], writes=["tmpv"])
                    S.add("dve", lambda e, hp=hp: e.max(out=mx[:, hp, 8:16], in_=tmpv[:]), reads=["tmpv"], writes=["mx"])
                for h in range(8):
                    S.add("dve", lambda e, h=h: e.tensor_tensor(
                        out=cand[:, h, :].rearrange("p (a b) -> p a b", a=16),
                        in0=mx[:, 2 * h, :].unsqueeze(2).to_broadcast([128, 16, 16]),
                        in1=mx[:, 2 * h + 1, :].unsqueeze(1).to_broadcast([128, 16, 16]), op=ALU.add),
                        reads=["mx"], writes=["cand"])
                    S.add("dve", lambda e, h=h: e.max(out=c16[:, h, 0:8], in_=cand[:, h, :]), reads=["cand"], writes=["c16"])
                    S.add("dve", lambda e, h=h: e.match_replace(out=tmpc[:], in_to_replace=c16[:, h, 0:8],
                                                                in_values=cand[:, h, :], imm_value=-1e30),
                          reads=["cand", "c16"], writes=["tmpc"])
                    S.add("dve", lambda e, h=h: e.max(out=c16[:, h, 8:16], in_=tmpc[:]), reads=["tmpc"], writes=["c16"])
                AX = mybir.AxisListType.X
                S.add("dve", lambda e: e.tensor_reduce(out=th[:], in_=c16[:], axis=AX, op=ALU.min), reads=["c16"], writes=["th"])
                S.add("dve", lambda e: e.tensor_reduce(out=mm[:], in_=c16[:], axis=AX, op=ALU.max), reads=["c16"], writes=["mm"])
                S.add("dve", lambda e: e.tensor_scalar(out=nm[:], in0=mm[:], scalar1=-1.0, scalar2=None, op0=ALU.mult),
                      reads=["mm"], writes=["nm"])
                for h in range(8):
                    S.add("act", lambda e, h=h: e.activation(out=ej[:], in_=c16[:, h, :], func=AF.Exp, bias=nm[:, h:h + 1],
                                                              accum_out=zz[:, h:h + 1]),
                          reads=["c16", "nm"], writes=["ej", "zz"])
                S.add("act", lambda e: e.activation(out=zz[:], in_=zz[:], func=AF.Ln), reads=["zz"], writes=["zz"])
                sc4 = sc[:].rearrange("p (h two) n -> p h two n", two=2)
                S.add("dve", lambda e: e.tensor_reduce(out=m2[:], in_=sc4[:, :, 1, :], axis=AX, op=ALU.max),
                      reads=["sc"], writes=["m2"])
                S.add("dve", lambda e, tt=tt: e.tensor_tensor(out=L2[tt][:], in0=sc4[:, :, 1, :],
                                                              in1=m2[:].unsqueeze(2).to_broadcast([128, 8, 128]),
                                                              op=ALU.subtract), reads=["sc", "m2"], writes=["L2_%d" % tt])
                S.add("dve", lambda e: e.tensor_tensor(out=dm[:], in0=m2[:], in1=th[:], op=ALU.subtract),
                      reads=["m2", "th"], writes=["dm"])
                S.add("dve", lambda e: e.tensor_scalar(out=dm[:], in0=dm[:], scalar1=1e-4, scalar2=None, op0=ALU.add),
                      reads=["dm"], writes=["dm"])
                S.add("dve", lambda e, tt=tt: e.tensor_tensor(out=D1[tt][:], in0=sc4[:, :, 0, :],
                                                              in1=dm[:].unsqueeze(2).to_broadcast([128, 8, 128]),
                                                              op=ALU.add), reads=["sc", "dm"], writes=["D1_%d" % tt])
                S.add("dve", lambda e, tt=tt: e.tensor_tensor(out=e1[tt][:], in0=th[:], in1=mm[:], op=ALU.subtract),
                      reads=["th", "mm"], writes=["e1_%d" % tt])
                S.add("dve", lambda e, tt=tt: e.tensor_tensor(out=e1[tt][:], in0=e1[tt][:], in1=zz[:], op=ALU.subtract),
                      reads=["e1_%d" % tt, "zz"], writes=["e1_%d" % tt])
            S.add("pool", lambda e: e.memset(acc[:], 0.0), writes=["acc"])
            pc = 0
            for b in range(nb):
                S.dma("sp", st16[:], utv[:, :, b * 128:(b + 1) * 128], writes=["st16"])
                S.add("act", lambda e: e.activation(out=w8[:], in_=st16[:], func=AF.Copy), reads=["st16"], writes=["w8"])
                S.dma("sp", vst[:], v_in[b * 128:(b + 1) * 128, :], writes=["vst"])
                S.add("pool", lambda e: e.tensor_copy(out=vb[:], in_=vst[:]), reads=["vst"], writes=["vb"])
                for k in range(32):
                    S.add("pe", lambda e, k=k: e.matmul(B[2][:, 0:tokq], lhsT=w8[:, k, :], rhs=hh[:, k, :],
                                                        start=(k == 0), stop=(k == 31)),
                          reads=["w8", "hh"], writes=["B2"])
                S.add("act", lambda e: e.activation(out=ga[:], in_=B[2][:, 0:tokq], func=AF.Gelu), reads=["B2"], writes=["ga"])
                for tt in range(tq):
                    S.add("dve", lambda e, tt=tt, b=b: e.tensor_tensor(
                        out=Xt[:], in0=L2[tt][:], in1=D1[tt][:, :, b:b + 1].to_broadcast([128, 8, 128]), op=ALU.add),
                        reads=["L2_%d" % tt, "D1_%d" % tt], writes=["Xt"])
                    S.add("pool", lambda e, tt=tt: e.tensor_tensor(
                        out=X2[:], in0=Xt[:], in1=e1[tt][:].unsqueeze(2).to_broadcast([128, 8, 128]), op=ALU.add),
                        reads=["Xt", "e1_%d" % tt], writes=["X2"])
                    S.add("act", lambda e: e.activation(out=Ee[:], in_=X2[:], func=AF.Exp), reads=["X2"], writes=["Ee"])
                    S.add("dve", lambda e: e.scalar_tensor_tensor(out=Gh[:], in0=Xt[:], scalar=0.0, in1=Ee[:],
                                                                  op0=ALU.is_ge, op1=ALU.mult),
                          reads=["Xt", "Ee"], writes=["Gh"])
                    S.add("dve", lambda e: e.tensor_reduce(out=Gtf[:], in_=Gh[:].rearrange("p h n -> p n h"),
                                                           axis=mybir.AxisListType.X, op=ALU.add),
                          reads=["Gh"], writes=["Gtf"])
                    S.add("pool", lambda e: e.tensor_copy(out=Gt[:], in_=Gtf[:]), reads=["Gtf"], writes=["Gt"])
                    S.add("pe", lambda e, tt=tt: e.transpose(Bb[3][:, tt * 128:(tt + 1) * 128], Gt[:], ident[:]),
                          reads=["Gt", "ident"], writes=["B3"])
                S.add("dve", lambda e: e.tensor_tensor(out=wT[:], in0=ga[:], in1=Bb[3][:, 0:tokq], op=ALU.mult),
                      reads=["ga", "B3"], writes=["wT"])
                for tt in range(tq):
                    for dc in range(8):
                        bk = 4 + pc % 4
                        pc += 1
                        S.add("pe", lambda e, tt=tt, dc=dc, bk=bk: e.matmul(
                            B[bk][:, :], lhsT=wT[:, tt * 128:(tt + 1) * 128], rhs=vb[:, dc * 512:(dc + 1) * 512],
                            start=True, stop=True), reads=["wT", "vb"], writes=["B%d" % bk])
                        S.add("dve", lambda e, tt=tt, dc=dc, bk=bk: e.tensor_tensor(
                            out=acc[:, tt, dc * 512:(dc + 1) * 512], in0=acc[:, tt, dc * 512:(dc + 1) * 512],
                            in1=B[bk][:, :], op=ALU.add), reads=["acc", "B%d" % bk], writes=["acc"])
            S.dma("sp", st16f, g2_in[:, :], writes=["st16"])
            for tt in range(tq):
                rs = slice(t0 + tt * 128, t0 + (tt + 1) * 128)
                S.dma("sp", vst[:], x1_in[rs, :], writes=["vst"])
                S.add("dve", lambda e, tt=tt: e.tensor_tensor(out=acc[:, tt, :], in0=acc[:, tt, :], in1=st16f, op=ALU.mult),
                      reads=["acc", "st16"], writes=["acc"])
                S.add("pool", lambda e, tt=tt: e.tensor_tensor(out=acc[:, tt, :], in0=acc[:, tt, :], in1=vst[:], op=ALU.add),
                      reads=["acc", "vst"], writes=["acc"])
                S.dma("sp", y_out[rs, :], acc[:, tt, :], reads=["acc"], writes=["y"])
        _finish(S, st, ["y"])
    return nc


def c2_maps(h2T, x1, mod, peer_w_query, peer_sub_keys, peer_u, peer_v, tok):
    idb, _, _ = _consts()
    gate2 = np.ascontiguousarray(np.broadcast_to(mod[5 * D:6 * D][None, :], (128, D))).astype(np.float32)
    wq = np.ascontiguousarray(peer_w_query[0])
    keysT = np.ascontiguousarray(np.transpose(peer_sub_keys[0].reshape(16, 128, 128), (2, 0, 1)))
    UT = np.ascontiguousarray(peer_u[0].T)
    V = np.ascontiguousarray(peer_v[0])
    maps = []
    for c in range(NCORES):
        ts = slice(c * tok, (c + 1) * tok)
        maps.append({"h2T": np.ascontiguousarray(h2T[:, ts]), "x1": np.ascontiguousarray(x1[ts]), "gate2": gate2,
                     "wq": wq, "keysT": keysT, "UT": UT, "V": V, "ident": idb})
    return maps


def _run(nc, maps):
    return run_bass_kernel_spmd(nc, maps, core_ids=list(range(NCORES))).results


def kernel(x, c, ada_w, ada_b, norm1_gain, norm2_gain, w_in, fox_f_bias, fox_q_gain, fox_k_gain,
           hgrn_lower_bounds, hgrn_out_gain, w_out, peer_w_query, peer_sub_keys, peer_u, peer_v):
    f = lambda a: np.asarray(a)
    x, w_in = f(x), f(w_in)
    mod = run_mod(f(c), f(ada_w), f(ada_b))
    x2 = np.ascontiguousarray(x[0])
    fox = _run(build_fox(), fox_maps(x2, mod, f(norm1_gain), w_in, f(fox_f_bias), f(fox_q_gain), f(fox_k_gain)))
    hg = _run(build_hgrn(), hgrn_maps(x2, mod, f(norm1_gain), w_in, f(hgrn_lower_bounds), f(hgrn_out_gain)))
    mergedT = np.concatenate([np.asarray(r["mT"]) for r in fox] + [np.asarray(r["mT"]) for r in hg], axis=0)
    del fox, hg
    c1 = _run(build_c1(), c1_maps(x2, mergedT, mod, f(norm2_gain), f(w_out)))
    x1 = np.concatenate([np.asarray(r["x1"]) for r in c1], axis=0)
    h2T = np.concatenate([np.asarray(r["h2T"]) for r in c1], axis=1)
    del c1, mergedT
    c2 = _run(build_c2(), c2_maps(h2T, x1, mod, f(peer_w_query), f(peer_sub_keys), f(peer_u), f(peer_v), TOK))
    y = np.concatenate([np.asarray(r["y"]) for r in c2], axis=0)
    return y.reshape(1, SEQ, D).astype(np.float32)
```

```python
from contextlib import ExitStack

import numpy as np
import ml_dtypes

import concourse.bass as bass
import concourse.mybir as mybir
from concourse.bass_utils import run_bass_kernel_spmd

F32 = mybir.dt.float32
BF16 = mybir.dt.bfloat16
AF = mybir.ActivationFunctionType
ALU = mybir.AluOpType

NCORES = 8
D = 4096
SEQ = 8192
HD = 128
EPS = 1e-6
ENGS = ("pe", "act", "dve", "pool", "sp")


class _Op:
    __slots__ = ("eng", "fn", "deps", "dma", "sig", "sem", "val", "idx")

    def __init__(self, eng, fn, dma):
        self.eng = eng; self.fn = fn; self.deps = set(); self.dma = dma
        self.sig = False; self.sem = None; self.val = 0; self.idx = 0


class Sched:
    def __init__(self, nc, n_dsem=10):
        self.nc = nc
        self.ops = {e: [] for e in ENGS}
        self.lastw = {}
        self.readers = {}
        self.n_dsem = n_dsem

    def add(self, eng, fn, reads=(), writes=(), dma=False):
        op = _Op(eng, fn, dma)
        deps = set()
        for t in reads:
            w = self.lastw.get(t)
            if w is not None:
                deps.add(w)
        for t in writes:
            w = self.lastw.get(t)
            if w is not None:
                deps.add(w)
            for r in self.readers.get(t, ()):
                deps.add(r)
        for t in reads:
            self.readers.setdefault(t, []).append(op)
        for t in writes:
            self.lastw[t] = op
            self.readers[t] = []
        deps.discard(op)
        op.deps = deps
        op.idx = len(self.ops[eng])
        self.ops[eng].append(op)
        return op

    def dma(self, eng, out, in_, reads=(), writes=()):
        return self.add(eng, lambda e: e.dma_start(out=out, in_=in_), reads, writes, dma=True)

    def emit(self, stack):
        nc = self.nc

        def skip(d, op):
            return d.eng == "pe" and op.eng == "pe" and not d.dma and not op.dma

        for e in ENGS:
            for op in self.ops[e]:
                for d in op.deps:
                    if not skip(d, op):
                        d.sig = True
        csem = {e: stack.enter_context(nc.semaphore("c_" + e)) for e in ENGS}
        dsems = {e: [stack.enter_context(nc.semaphore("d_%s%d" % (e, i))) for i in range(self.n_dsem)]
                 for e in ("sp", "act", "pool")}
        for e in ENGS:
            cnt = 0
            dcnt = 0
            duse = [0] * self.n_dsem
            for op in self.ops[e]:
                if op.dma:
                    k = dcnt % self.n_dsem
                    dcnt += 1
                    duse[k] += 1
                    op.sem = dsems[e][k]
                    op.val = 16 * duse[k]
                    op.sig = True
                elif op.sig:
                    cnt += 1
                    op.sem = csem[e]
                    op.val = cnt
        blk = stack.enter_context(nc.Block())

        def run(e):
            def body(eng):
                waited = {}
                dprev = {}

                def wait(d):
                    key = id(d.sem)
                    if waited.get(key, 0) >= d.val:
                        return
                    eng.wait_ge(d.sem, d.val)
                    waited[key] = d.val

                for op in self.ops[e]:
                    for d in sorted(op.deps, key=lambda o: (o.eng, o.idx)):
                        if not skip(d, op):
                            wait(d)
                    if op.dma:
                        p = dprev.get(id(op.sem))
                        if p is not None:
                            wait(p)
                        dprev[id(op.sem)] = op
                    ins = op.fn(eng)
                    if op.sig:
                        ins.then_inc(op.sem, 16 if op.dma else 1)
            return body

        blk.tensor(run("pe"))
        blk.scalar(run("act"))
        blk.vector(run("dve"))
        blk.gpsimd(run("pool"))
        blk.sync(run("sp"))


def _finish(S, st, out_tokens):
    S.add("sp", lambda e: e.nop(), reads=list(out_tokens))
    S.emit(st)


MODC = 6 * D // NCORES


def build_mod():
    nc = bass.Bass("TRN2", target_bir_lowering=False)
    c_in = nc.dram_tensor("c", [128, 32], F32, kind="ExternalInput").ap()
    w_in = nc.dram_tensor("w", [D, MODC], F32, kind="ExternalInput").ap()
    b_in = nc.dram_tensor("b", [1, MODC], F32, kind="ExternalInput").ap()
    out = nc.dram_tensor("mod", [1, MODC], F32, kind="ExternalOutput").ap()
    wv = w_in.rearrange("(p k) n -> k p n", k=32)
    S = Sched(nc)
    with ExitStack() as st:
        ct = st.enter_context(nc.sbuf_tensor("ct", [128, 32], F32))
        cs = st.enter_context(nc.sbuf_tensor("cs", [128, 32], F32))
        acc = st.enter_context(nc.sbuf_tensor("acc", [128, MODC], F32))
        wts = [st.enter_context(nc.sbuf_tensor("wt%d" % i, [128, MODC], F32)) for i in range(3)]
        ones = st.enter_context(nc.sbuf_tensor("ones", [128, 1], F32))
        bt = st.enter_context(nc.sbuf_tensor("bt", [1, MODC], F32))
        res = st.enter_context(nc.sbuf_tensor("res", [1, MODC], F32))
        pss = [st.enter_context(nc.psum_tensor("ps%d" % i, [128, 512], F32)) for i in range(6)]
        S.dma("sp", ct[:], c_in[:, :], writes=["ct"])
        S.dma("sp", bt[:], b_in[:, :], writes=["bt"])
        S.add("act", lambda e: e.activation(out=cs[:], in_=ct[:], func=AF.Silu), reads=["ct"], writes=["cs"])
        S.add("pool", lambda e: e.memset(ones[:], 1.0), writes=["ones"])
        for k in range(32):
            wt = wts[k % 3]
            tok = "wt%d" % (k % 3)
            S.dma("sp", wt[:], wv[k], writes=[tok])
            if k == 0:
                S.add("dve", lambda e, wt=wt: e.tensor_scalar(out=acc[:], in0=wt[:], scalar1=cs[:, 0:1], scalar2=None,
                                                               op0=ALU.mult), reads=[tok, "cs"], writes=["acc"])
            else:
                S.add("dve", lambda e, wt=wt, k=k: e.scalar_tensor_tensor(out=acc[:], in0=wt[:], scalar=cs[:, k:k + 1],
                                                                           in1=acc[:], op0=ALU.mult, op1=ALU.add),
                      reads=[tok, "cs", "acc"], writes=["acc"])
        for j in range(6):
            S.add("pe", lambda e, j=j: e.matmul(pss[j][0:1, :], lhsT=ones[:, 0:1], rhs=acc[:, j * 512:(j + 1) * 512],
                                                start=True, stop=True), reads=["ones", "acc"], writes=["ps%d" % j])
            S.add("dve", lambda e, j=j: e.tensor_tensor(out=res[0:1, j * 512:(j + 1) * 512], in0=pss[j][0:1, :],
                                                        in1=bt[0:1, j * 512:(j + 1) * 512], op=ALU.add),
                  reads=["ps%d" % j, "bt"], writes=["res%d" % j])
        S.dma("sp", out[:, :], res[:], reads=["res%d" % j for j in range(6)], writes=["out"])
        _finish(S, st, ["out"])
    return nc


def run_mod(c, ada_w, ada_b):
    nc = build_mod()
    c2 = np.ascontiguousarray(c.reshape(128, 32))
    maps = []
    for i in range(NCORES):
        sl = slice(i * MODC, (i + 1) * MODC)
        maps.append({"c": c2, "w": np.ascontiguousarray(ada_w[0][:, sl]),
                     "b": np.ascontiguousarray(ada_b[0][sl].reshape(1, MODC))})
    res = run_bass_kernel_spmd(nc, maps, core_ids=list(range(NCORES)))
    return np.concatenate([np.asarray(r["mod"]).reshape(-1) for r in res.results])


NT = SEQ // 128
FC = 770


def _consts():
    ident = np.eye(128, dtype=np.float32)
    ut = np.triu(np.ones((128, 128), np.float32))
    return ident.astype(ml_dtypes.bfloat16), ut, ut.astype(ml_dtypes.bfloat16)


def build_fox(nt=NT):
    nc = bass.Bass("TRN2", target_bir_lowering=False)
    seq = nt * 128
    x_in = nc.dram_tensor("x", [seq, D], F32, kind="ExternalInput").ap()
    g1_in = nc.dram_tensor("g1", [128, 32], F32, kind="ExternalInput").ap()
    sc_in = nc.dram_tensor("sc1", [128, 32], F32, kind="ExternalInput").ap()
    sh_in = nc.dram_tensor("sh1", [128, 32], F32, kind="ExternalInput").ap()
    w_in = nc.dram_tensor("w", [D, FC], F32, kind="ExternalInput").ap()
    fb_in = nc.dram_tensor("fb", [128, 2], F32, kind="ExternalInput").ap()
    gq_in = nc.dram_tensor("gq", [128, 2], F32, kind="ExternalInput").ap()
    gk_in = nc.dram_tensor("gk", [128, 2], F32, kind="ExternalInput").ap()
    id_in = nc.dram_tensor("ident", [128, 128], BF16, kind="ExternalInput").ap()
    ut_in = nc.dram_tensor("ut", [128, 128], F32, kind="ExternalInput").ap()
    utb_in = nc.dram_tensor("utb", [128, 128], BF16, kind="ExternalInput").ap()
    neg_in = nc.dram_tensor("negm", [128, 128], BF16, kind="ExternalInput").ap()
    idf_in = nc.dram_tensor("identf", [128, 128], F32, kind="ExternalInput").ap()
    out = nc.dram_tensor("mT", [256, seq], BF16, kind="ExternalOutput").ap()
    wv = w_in.rearrange("(k p) n -> k p n", p=128)
    S = Sched(nc)
    with ExitStack() as st:
        sb = lambda n, s, d: st.enter_context(nc.sbuf_tensor(n, s, d))
        wF = sb("wF", [128, 32, FC], BF16)
        wst = [sb("wst%d" % i, [128, FC], F32) for i in range(2)]
        qT = [sb("qT%d" % h, [128, seq], BF16) for h in range(2)]
        kT = [sb("kT%d" % h, [128, seq], BF16) for h in range(2)]
        Vx = [sb("Vx%d" % h, [128, nt, 129], BF16) for h in range(2)]
        fl = sb("fl", [128, 2, nt], F32)
        xb = sb("xb", [128, D], F32)
        xs = sb("xs", [128, D], BF16)
        hT = [sb("hT%d" % i, [128, 32, 128], BF16) for i in range(2)]
        ss = sb("ss", [128, nt], F32)
        sq4 = sb("sq4", [128, 4], F32)
        qn = [sb("qn%d" % i, [128, 128], BF16) for i in range(4)]
        a1 = sb("a1_s", [128, 32], F32); sh1 = sb("sh1_s", [128, 32], F32); g1 = sb("g1_s", [128, 32], F32)
        fb = sb("fb_s", [128, 2], F32); gq = sb("gq_s", [128, 2], F32); gk = sb("gk_s", [128, 2], F32)
        ident = sb("ident_s", [128, 128], BF16); ut = sb("ut_s", [128, 128], F32); utb = sb("utb_s", [128, 128], BF16)
        onesf = sb("onesf", [128, 128], F32)
        negm = sb("negm_s", [128, 128], BF16); identf = sb("identf_s", [128, 128], F32)
        cum2 = sb("cum2", [128, 128], F32); off2 = sb("off2", [128, 128], F32)
        ctf = sb("ctf", [128, 128], F32); cthi = sb("cthi", [128, 128], BF16); ctr = sb("ctr", [128, 128], F32)
        ctlo = sb("ctlo", [128, 128], BF16)
        CT2 = [sb("CT2_%d" % h, [128, 128], BF16) for h in range(2)]
        E2 = [sb("E2_%d" % i, [128, 128], BF16) for i in range(2)]
        junk = sb("junk", [128, 128], BF16)
        cum = sb("cum", [128, 2, nt], F32); off = sb("off", [128, 2, nt], F32); tot = sb("tot", [128, 2, nt], F32)
        lf = sb("lf", [128, 2, nt], F32)
        negb2 = [sb("negb%d" % i, [128, nt], F32) for i in range(2)]
        rden2 = [sb("rden%d" % i, [128, 1], F32) for i in range(2)]
        on2 = [sb("on%d" % i, [128, 128], BF16) for i in range(2)]
        pT = [sb("pT%d" % i, [128, 128], BF16) for i in range(3)]
        rden = sb("rden", [128, 1], F32)
        on = sb("on", [128, 128], BF16)
        ost = [sb("ost%d" % i, [128, 128], BF16) for i in range(2)]
        B = [st.enter_context(nc.psum_tensor("B%d" % i, [128, 512], F32)) for i in range(8)]
        Bb = [b[:].bitcast(BF16) for b in B]

        for (t, src, name) in ((g1, g1_in, "g1"), (a1, sc_in, "a1"), (sh1, sh_in, "sh1"), (fb, fb_in, "fb"),
                               (gq, gq_in, "gq"), (gk, gk_in, "gk"), (ident, id_in, "ident"), (ut, ut_in, "ut"),
                               (utb, utb_in, "utb"), (negm, neg_in, "negm"), (identf, idf_in, "identf")):
            S.dma("sp", t[:], src[:, :], writes=[name])
        S.add("dve", lambda e: e.scalar_tensor_tensor(out=a1[:], in0=a1[:], scalar=1.0, in1=g1[:], op0=ALU.add,
                                                      op1=ALU.mult), reads=["a1", "g1"], writes=["a1"])
        S.add("dve", lambda e: e.tensor_scalar(out=fb[:], in0=fb[:], scalar1=-1.0, scalar2=None, op0=ALU.mult),
              reads=["fb"], writes=["fb"])
        S.add("pool", lambda e: e.memset(onesf[:], 1.0), writes=["onesf"])
        for h in range(2):
            S.add("pool", lambda e, h=h: e.memset(Vx[h][:, :, 128:129], 1.0), writes=["Vx%d" % h])
        for k in range(32):
            S.dma("sp", wst[k % 2][:], wv[k], writes=["wst%d" % (k % 2)])
            eng = ("act", "dve", "pool")[k % 3]
            if eng == "act":
                S.add("act", lambda e, k=k: e.activation(out=wF[:, k, :], in_=wst[k % 2][:], func=AF.Copy),
                      reads=["wst%d" % (k % 2)], writes=["wF"])
            else:
                S.add(eng, lambda e, k=k: e.tensor_copy(out=wF[:, k, :], in_=wst[k % 2][:]),
                      reads=["wst%d" % (k % 2)], writes=["wF"])

        for tt in range(nt):
            ts = slice(tt * 128, (tt + 1) * 128)
            S.dma("sp", xb[:], x_in[ts, :], writes=["xb"])
            S.add("act", lambda e, tt=tt: e.activation(out=xs[:], in_=xb[:], func=AF.Square,
                                                        accum_out=ss[:, tt:tt + 1]), reads=["xb"], writes=["xs", "ss"])
            S.add("act", lambda e, tt=tt: e.activation(out=ss[:, tt:tt + 1], in_=ss[:, tt:tt + 1], func=AF.Sqrt,
                                                        scale=1.0 / D, bias=EPS), reads=["ss"], writes=["ss"])
            S.add("dve", lambda e, tt=tt: e.reciprocal(out=ss[:, tt:tt + 1], in_=ss[:, tt:tt + 1]),
                  reads=["ss"], writes=["ss"])
            S.add("dve", lambda e, tt=tt: e.tensor_scalar(out=xs[:], in0=xb[:], scalar1=ss[:, tt:tt + 1], scalar2=None,
                                                           op0=ALU.mult), reads=["xb", "ss"], writes=["xs"])
            hb = hT[tt % 2]
            htok = "hT%d" % (tt % 2)
            for k4 in range(8):
                bk = k4 % 2
                for j in range(4):
                    k = k4 * 4 + j
                    S.add("pe", lambda e, k=k, j=j, bk=bk: e.transpose(Bb[bk][:, j * 128:(j + 1) * 128],
                                                                       xs[:, k * 128:(k + 1) * 128], ident[:]),
                          reads=["xs", "ident"], writes=["B%d" % bk])
                for j in range(4):
                    k = k4 * 4 + j
                    S.add("act", lambda e, k=k, j=j, bk=bk, hb=hb: e.activation(
                        out=hb[:, k, :], in_=Bb[bk][:, j * 128:(j + 1) * 128], func=AF.Identity,
                        scale=a1[:, k:k + 1], bias=sh1[:, k:k + 1]),
                        reads=["B%d" % bk, "a1", "sh1"], writes=[htok])
            for cg, (c0, c1) in enumerate(((0, 512), (512, FC))):
                for k in range(32):
                    S.add("pe", lambda e, k=k, cg=cg, c0=c0, c1=c1, hb=hb: e.matmul(
                        B[2 + cg][:, 0:c1 - c0], lhsT=hb[:, k, :], rhs=wF[:, k, c0:c1], start=(k == 0), stop=(k == 31)),
                        reads=[htok, "wF"], writes=["B%d" % (2 + cg)])
            for i in range(4):
                S.add("act", lambda e, i=i: e.activation(out=junk[:, 0:128], in_=B[2][:, i * 128:(i + 1) * 128],
                                                          func=AF.Square, accum_out=sq4[:, i:i + 1]),
                      reads=["B2"], writes=["junk", "sq4"])
            S.add("act", lambda e: e.activation(out=sq4[:], in_=sq4[:], func=AF.Sqrt, scale=1.0 / HD, bias=EPS),
                  reads=["sq4"], writes=["sq4"])
            S.add("dve", lambda e: e.reciprocal(out=sq4[:], in_=sq4[:]), reads=["sq4"], writes=["sq4"])
            for i in range(4):
                S.add("dve", lambda e, i=i: e.tensor_scalar(out=qn[i][:], in0=B[2][:, i * 128:(i + 1) * 128],
                                                             scalar1=sq4[:, i:i + 1], scalar2=None, op0=ALU.mult),
                      reads=["B2", "sq4"], writes=["qn%d" % i])
                S.add("pe", lambda e, i=i: e.transpose(Bb[4][:, i * 128:(i + 1) * 128], qn[i][:], ident[:]),
                      reads=["qn%d" % i, "ident"], writes=["B4"])
                h = i % 2
                dst, gg, gname = (qT[h], gq, "gq") if i < 2 else (kT[h], gk, "gk")
                S.add("act", lambda e, i=i, dst=dst, gg=gg, h=h, ts=ts: e.activation(
                    out=dst[:, ts], in_=Bb[4][:, i * 128:(i + 1) * 128], func=AF.Copy, scale=gg[:, h:h + 1]),
                    reads=["B4", gname], writes=["qk%d" % i])
            for h in range(2):
                S.add("dve", lambda e, h=h, tt=tt: e.tensor_copy(out=Vx[h][:, tt, 0:128], in_=B[3][:, h * 128:(h + 1) * 128]),
                      reads=["B3"], writes=["Vx%d" % h])
            S.add("dve", lambda e, tt=tt: e.tensor_copy(out=fl[:, :, tt], in_=B[3][:, 256:258]),
                  reads=["B3"], writes=["fl"])

        for h in range(2):
            S.add("act", lambda e, h=h: e.activation(out=lf[:, h, :], in_=fl[:, h, :], func=AF.Exp, scale=-1.0,
                                                      bias=fb[:, h:h + 1]), reads=["fl", "fb"], writes=["lf"])
        S.add("act", lambda e: e.activation(out=lf[:], in_=lf[:], func=AF.Ln, scale=1.0, bias=1.0),
              reads=["lf"], writes=["lf"])
        S.add("dve", lambda e: e.tensor_scalar(out=lf[:], in0=lf[:], scalar1=-1.0, scalar2=None, op0=ALU.mult),
              reads=["lf"], writes=["lf"])
        lf2 = lf[:].rearrange("p h t -> p (h t)")
        n2 = 2 * nt
        S.add("pe", lambda e: e.matmul(B[0][:, 0:n2], lhsT=ut[:], rhs=lf2, start=True, stop=True),
              reads=["ut", "lf"], writes=["B0"])
        S.add("pe", lambda e: e.matmul(B[1][:, 0:n2], lhsT=onesf[:], rhs=lf2, start=True, stop=True),
              reads=["onesf", "lf"], writes=["B1"])
        S.add("dve", lambda e: e.tensor_copy(out=tot[:].rearrange("p h t -> p (h t)"), in_=B[1][:, 0:n2]),
              reads=["B1"], writes=["tot"])
        S.add("pool", lambda e: e.memset(off[:], 0.0), writes=["off"])
        for j in range(1, nt):
            S.add("dve", lambda e, j=j: e.tensor_tensor(out=off[:, :, j], in0=off[:, :, j - 1], in1=tot[:, :, j - 1],
                                                        op=ALU.add), reads=["off", "tot"], writes=["off"])
        S.add("dve", lambda e: e.tensor_tensor(out=cum[:].rearrange("p h t -> p (h t)"), in0=B[0][:, 0:n2],
                                               in1=off[:].rearrange("p h t -> p (h t)"), op=ALU.add),
              reads=["B0", "off"], writes=["cum"])

        scale = float(HD) ** -0.5
        for h in range(2):
            S.add("pool", lambda e: e.memset(cum2[:], 0.0), writes=["cum2"])
            S.add("pool", lambda e: e.memset(off2[:], 0.0), writes=["off2"])
            for r in range(2):
                S.add("dve", lambda e, h=h, r=r: e.tensor_copy(out=cum2[:, r * 64:r * 64 + nt], in_=cum[:, h, :]),
                      reads=["cum"], writes=["cum2"])
                S.add("dve", lambda e, h=h, r=r: e.tensor_copy(out=off2[:, r * 64:r * 64 + nt], in_=off[:, h, :]),
                      reads=["off"], writes=["off2"])
            S.add("pe", lambda e: e.transpose(B[0][:, 0:128], cum2[:], identf[:]), reads=["cum2", "identf"], writes=["B0"])
            S.add("pe", lambda e: e.transpose(B[1][:, 0:128], off2[:], identf[:]), reads=["off2", "identf"], writes=["B1"])
            S.add("dve", lambda e: e.tensor_copy(out=ctr[:], in_=B[1][:, 0:128]), reads=["B1"], writes=["ctr"])
            S.add("dve", lambda e: e.tensor_scalar(out=ctf[:], in0=B[0][:, 0:128], scalar1=ctr[:, 0:1], scalar2=1.0 / scale,
                                                   op0=ALU.subtract, op1=ALU.mult), reads=["B0", "ctr"], writes=["ctf"])
            S.add("dve", lambda e: e.tensor_copy(out=cthi[:], in_=ctf[:]), reads=["ctf"], writes=["cthi"])
            S.add("dve", lambda e: e.tensor_tensor(out=ctr[:], in0=ctf[:], in1=cthi[:], op=ALU.subtract),
                  reads=["ctf", "cthi"], writes=["ctr"])
            S.add("dve", lambda e: e.tensor_copy(out=ctlo[:], in_=ctr[:]), reads=["ctr"], writes=["ctlo"])
            S.add("dve", lambda e, h=h: e.tensor_copy(out=CT2[h][0:64, :], in_=cthi[0:64, :]), reads=["cthi"],
                  writes=["CT2_%d" % h])
            S.add("dve", lambda e, h=h: e.tensor_copy(out=CT2[h][64:128, :], in_=ctlo[64:128, :]), reads=["ctlo"],
                  writes=["CT2_%d" % h])
        for h in range(2):
            pairs = [(qb, kb) for qb in range(nt) for kb in range(qb + 1)]

            def emit_s(i, h=h):
                qb, kb = pairs[i]
                qs_ = slice(qb * 128, (qb + 1) * 128)
                sbk = 4 + (i % 2)
                e2 = E2[qb % 2]; e2tok = "E2_%d" % (qb % 2)
                if kb == 0:
                    S.add("dve", lambda e, qb=qb, e2=e2: e.tensor_tensor(
                        out=e2[:], in0=ident[:, qb:qb + 1].to_broadcast([128, 128]),
                        in1=ident[:, 64 + qb:65 + qb].to_broadcast([128, 128]), op=ALU.add),
                        reads=["ident"], writes=[e2tok])
                S.add("pe", lambda e: e.matmul(B[sbk][:, 0:128], lhsT=kT[h][:, kb * 128:(kb + 1) * 128], rhs=qT[h][:, qs_],
                                               start=True, stop=False),
                      reads=["qk%d" % h, "qk%d" % (2 + h)], writes=["B%d" % sbk])
                S.add("pe", lambda e: e.matmul(B[sbk][:, 0:128], lhsT=e2[:], rhs=CT2[h][:], start=False, stop=(kb != qb)),
                      reads=[e2tok, "CT2_%d" % h], writes=["B%d" % sbk])
                if kb == qb:
                    S.add("pe", lambda e: e.matmul(B[sbk][:, 0:128], lhsT=ident[:], rhs=negm[:], start=False, stop=True),
                          reads=["ident", "negm"], writes=["B%d" % sbk])

            def emit_rest(i, h=h):
                qb, kb = pairs[i]
                qs_ = slice(qb * 128, (qb + 1) * 128)
                sbk = 4 + (i % 2)
                pt = pT[i % 3]; ptok = "pT%d" % (i % 3)
                ab = 6 + (qb % 2)
                nb_ = negb2[qb % 2]; nbtok = "negb%d" % (qb % 2)
                if kb == 0:
                    S.add("dve", lambda e: e.tensor_scalar(
                        out=nb_[:, 0:qb + 1], in0=cum[:, h, 0:qb + 1], scalar1=-1.0, scalar2=off[:, h, qb:qb + 1],
                        op0=ALU.mult, op1=ALU.add), reads=["cum", "off"], writes=[nbtok])
                S.add("act", lambda e: e.activation(out=pt[:], in_=B[sbk][:, 0:128], func=AF.Exp, scale=scale,
                                                    bias=nb_[:, kb:kb + 1]),
                      reads=["B%d" % sbk, nbtok], writes=[ptok])
                S.add("pe", lambda e: e.matmul(B[ab][:, 0:129], lhsT=pt[:], rhs=Vx[h][:, kb, :], start=(kb == 0),
                                               stop=(kb == qb)), reads=[ptok, "Vx%d" % h], writes=["B%d" % ab])
                if kb == qb:
                    rd = rden2[qb % 2]; rdtok = "rden%d" % (qb % 2)
                    onb = on2[qb % 2]; ontok = "on%d" % (qb % 2)
                    S.add("dve", lambda e: e.reciprocal(out=rd[:], in_=B[ab][:, 128:129]), reads=["B%d" % ab], writes=[rdtok])
                    S.add("dve", lambda e: e.tensor_scalar(out=onb[:], in0=B[ab][:, 0:128], scalar1=rd[:, 0:1],
                                                           scalar2=None, op0=ALU.mult),
                          reads=["B%d" % ab, rdtok], writes=[ontok])
                    tb = qb % 2
                    S.add("pe", lambda e: e.transpose(Bb[tb][:, 0:128], onb[:], ident[:]), reads=[ontok, "ident"],
                          writes=["B%d" % tb])
                    ob = ost[qb % 2]
                    S.add("act", lambda e: e.activation(out=ob[:], in_=Bb[tb][:, 0:128], func=AF.Copy),
                          reads=["B%d" % tb], writes=["ost%d" % (qb % 2)])
                    S.dma("sp", out[h * 128:(h + 1) * 128, qs_], ob[:], reads=["ost%d" % (qb % 2)], writes=["out"])

            emit_s(0)
            for i in range(len(pairs)):
                if i + 1 < len(pairs):
                    emit_s(i + 1)
                emit_rest(i)
        _finish(S, st, ["out"])
    return nc


def _pk(v):
    return np.ascontiguousarray(np.asarray(v, np.float32).reshape(32, 128).T)


def fox_maps(x2, mod, norm1_gain, w_in, fox_f_bias, fox_q_gain, fox_k_gain):
    idb, ut, utb = _consts()
    maps = []
    for c in range(NCORES):
        hs = [2 * c, 2 * c + 1]
        cols = np.concatenate([np.arange(256 * c, 256 * c + 256), 2048 + np.arange(256 * c, 256 * c + 256),
                               4096 + np.arange(256 * c, 256 * c + 256), 6144 + np.array(hs)])
        maps.append({
            "x": x2, "g1": _pk(norm1_gain[0]), "sc1": _pk(mod[D:2 * D]), "sh1": _pk(mod[0:D]),
            "w": np.ascontiguousarray(w_in[0][:, cols]),
            "fb": np.ascontiguousarray(np.broadcast_to(fox_f_bias[0][hs][None, :], (128, 2))).astype(np.float32),
            "gq": np.ascontiguousarray(fox_q_gain[0][hs].T), "gk": np.ascontiguousarray(fox_k_gain[0][hs].T),
            "ident": idb, "ut": ut, "utb": utb, "identf": np.eye(128, dtype=np.float32),
            "negm": (np.tril(np.ones((128, 128), np.float32), -1) * -30000.0).astype(ml_dtypes.bfloat16),
        })
    return maps


HC = 1024


def build_hgrn(nt=NT):
    nc = bass.Bass("TRN2", target_bir_lowering=False)
    seq = nt * 128
    din = lambda n, s, d: nc.dram_tensor(n, s, d, kind="ExternalInput").ap()
    x_in = din("x", [seq, D], F32)
    g1_in = din("g1", [128, 32], F32); sc_in = din("sc1", [128, 32], F32); sh_in = din("sh1", [128, 32], F32)
    w_in = din("w", [D, HC], F32)
    lb0_in = din("lb0", [128, 256], F32); lb1_in = din("lb1", [128, 256], F32); og_in = din("og", [128, 256], F32)
    id_in = din("ident", [128, 128], BF16)
    tri_in = din("tri", [128, 128], BF16); stri_in = din("stri", [128, 128], BF16); m_in = din("m128", [128, 128], F32)
    c01_in = din("c01", [128, 2], F32)
    out = nc.dram_tensor("mT", [256, seq], BF16, kind="ExternalOutput").ap()
    wv = w_in.rearrange("(k p) n -> k p n", p=128)
    S = Sched(nc)
    with ExitStack() as st:
        sb = lambda n, s, d: st.enter_context(nc.sbuf_tensor(n + "_s", s, d))
        wH = sb("wH", [128, 32, HC], BF16)
        wst = [sb("wst%d" % i, [128, HC], F32) for i in range(2)]
        xb = sb("xb", [128, D], F32); xs = sb("xs", [128, D], BF16)
        hT = [sb("hT%d" % i, [128, 32, 128], BF16) for i in range(2)]
        ss = sb("ss", [128, nt], F32)
        a1 = sb("a1", [128, 32], F32); sh1 = sb("sh1", [128, 32], F32); g1 = sb("g1", [128, 32], F32)
        lbb = sb("lbb", [128, 256], F32); omlb = sb("omlb", [128, 256], F32); ogb = sb("ogb", [128, 256], F32)
        ident = sb("ident", [128, 128], BF16)
        tri = sb("tri", [128, 128], BF16); stri = sb("stri", [128, 128], BF16); m128 = sb("m128", [128, 128], F32)
        c01 = sb("c01", [128, 2], F32)
        junk = sb("junk", [128, D], BF16)
        sg = sb("sg", [128, 256], F32); gl = sb("gl", [128, 256], F32)
        ghi = sb("ghi", [128, 256], BF16); glo = sb("glo", [128, 256], BF16); gr = sb("gr", [128, 256], F32)
        omfb = sb("omfb", [128, 256], BF16); qsb = sb("qsb", [128, 256], BF16)
        gs = sb("gs", [128, 256], F32); vHb = sb("vHb", [128, 256], BF16)
        ebs = [sb("eb%d" % h, [128, 128], F32) for h in range(2)]
        enbs = [sb("enb%d" % h, [128, 128], F32) for h in range(2)]
        ers = [sb("er%d" % h, [128, 128], F32) for h in range(2)]
        QtTs = [sb("QtT%d" % h, [128, 128], BF16) for h in range(2)]
        Qt0s = [sb("Qt0%d" % h, [128, 128], BF16) for h in range(2)]
        Qt1s = [sb("Qt1%d" % h, [128, 128], BF16) for h in range(2)]
        KtTs = [sb("KtT%d" % h, [128, 128], BF16) for h in range(2)]
        Khs = [sb("Kh%d" % h, [128, 128], BF16) for h in range(2)]
        Kh0s = [sb("Kh0%d" % h, [128, 128], BF16) for h in range(2)]
        Kh1s = [sb("Kh1%d" % h, [128, 128], BF16) for h in range(2)]
        scms = [sb("scm%d" % h, [128, 128], BF16) for h in range(2)]
        junks = [sb("junkh%d" % h, [128, 128], BF16) for h in range(2)]
        Sst = [sb("S%d" % h, [128, 128], F32) for h in range(2)]
        Sbf = [sb("Sbf%d" % h, [128, 128], BF16) for h in range(2)]
        sos = [sb("so%d" % h, [128, 1], F32) for h in range(2)]
        on1s = [sb("on1%d" % h, [128, 128], F32) for h in range(2)]
        on3s = [sb("on3%d" % h, [128, 128], BF16) for h in range(2)]
        ost = [sb("ost%d" % i, [128, 128], BF16) for i in range(4)]
        B = [st.enter_context(nc.psum_tensor("B%d" % i, [128, 512], F32)) for i in range(8)]
        Bb = [b[:].bitcast(BF16) for b in B]

        for (t, src, name) in ((g1, g1_in, "g1"), (a1, sc_in, "a1"), (sh1, sh_in, "sh1"), (lbb, lb0_in, "lbb"),
                               (omlb, lb1_in, "omlb"), (ogb, og_in, "ogb"), (ident, id_in, "ident"),
                               (tri, tri_in, "tri"), (stri, stri_in, "stri"), (m128, m_in, "m128"), (c01, c01_in, "c01")):
            S.dma("sp", t[:], src[:, :], writes=[name])
        S.add("dve", lambda e: e.scalar_tensor_tensor(out=a1[:], in0=a1[:], scalar=1.0, in1=g1[:], op0=ALU.add,
                                                      op1=ALU.mult), reads=["a1", "g1"], writes=["a1"])
        S.add("dve", lambda e: e.tensor_tensor(out=lbb[:], in0=lbb[:], in1=omlb[:], op=ALU.subtract),
              reads=["lbb", "omlb"], writes=["lbb"])
        S.add("act", lambda e: e.activation(out=lbb[:], in_=lbb[:], func=AF.Sigmoid), reads=["lbb"], writes=["lbb"])
        S.add("dve", lambda e: e.tensor_scalar(out=omlb[:], in0=lbb[:], scalar1=-1.0, scalar2=1.0, op0=ALU.mult,
                                               op1=ALU.add), reads=["lbb"], writes=["omlb"])
        for h in range(2):
            S.add("pool", lambda e, h=h: e.memset(Sst[h][:], 0.0), writes=["S%d" % h])
            S.add("pool", lambda e, h=h: e.memset(Sbf[h][:], 0.0), writes=["Sbf%d" % h])
        for h in range(2):
            S.add("pool", lambda e, h=h: e.memset(Qt0s[h][:], 0.0), writes=["Qt0_h%d" % h])
            S.add("pool", lambda e, h=h: e.memset(Qt1s[h][:], 0.0), writes=["Qt1_h%d" % h])
        for k in range(32):
            S.dma("sp", wst[k % 2][:], wv[k], writes=["wst%d" % (k % 2)])
            eng = ("act", "dve", "pool")[k % 3]
            if eng == "act":
                S.add("act", lambda e, k=k: e.activation(out=wH[:, k, :], in_=wst[k % 2][:], func=AF.Copy),
                      reads=["wst%d" % (k % 2)], writes=["wH"])
            else:
                S.add(eng, lambda e, k=k: e.tensor_copy(out=wH[:, k, :], in_=wst[k % 2][:]),
                      reads=["wst%d" % (k % 2)], writes=["wH"])

        oc = 0
        for tt in range(nt):
            ts = slice(tt * 128, (tt + 1) * 128)
            S.dma("sp", xb[:], x_in[ts, :], writes=["xb"])
            S.add("act", lambda e, tt=tt: e.activation(out=junk[:], in_=xb[:], func=AF.Square,
                                                        accum_out=ss[:, tt:tt + 1]), reads=["xb"], writes=["junk", "ss"])
            S.add("act", lambda e, tt=tt: e.activation(out=ss[:, tt:tt + 1], in_=ss[:, tt:tt + 1], func=AF.Sqrt,
                                                        scale=1.0 / D, bias=EPS), reads=["ss"], writes=["ss"])
            S.add("dve", lambda e, tt=tt: e.reciprocal(out=ss[:, tt:tt + 1], in_=ss[:, tt:tt + 1]),
                  reads=["ss"], writes=["ss"])
            S.add("dve", lambda e, tt=tt: e.tensor_scalar(out=xs[:], in0=xb[:], scalar1=ss[:, tt:tt + 1], scalar2=None,
                                                           op0=ALU.mult), reads=["xb", "ss"], writes=["xs"])
            hb = hT[tt % 2]
            htok = "hT%d" % (tt % 2)
            for k4 in range(8):
                bk = k4 % 2
                for j in range(4):
                    k = k4 * 4 + j
                    S.add("pe", lambda e, k=k, j=j, bk=bk: e.transpose(Bb[bk][:, j * 128:(j + 1) * 128],
                                                                       xs[:, k * 128:(k + 1) * 128], ident[:]),
                          reads=["xs", "ident"], writes=["B%d" % bk])
                for j in range(4):
                    k = k4 * 4 + j
                    S.add("act", lambda e, k=k, j=j, bk=bk, hb=hb: e.activation(
                        out=hb[:, k, :], in_=Bb[bk][:, j * 128:(j + 1) * 128], func=AF.Identity,
                        scale=a1[:, k:k + 1], bias=sh1[:, k:k + 1]),
                        reads=["B%d" % bk, "a1", "sh1"], writes=[htok])
            for cg in range(2):
                for k in range(32):
                    S.add("pe", lambda e, k=k, cg=cg, hb=hb: e.matmul(
                        B[2 + cg][:, :], lhsT=hb[:, k, :], rhs=wH[:, k, cg * 512:(cg + 1) * 512],
                        start=(k == 0), stop=(k == 31)), reads=[htok, "wH"], writes=["B%d" % (2 + cg)])
            S.add("act", lambda e: e.activation(out=sg[:], in_=B[2][:, 256:512], func=AF.Sigmoid),
                  reads=["B2"], writes=["sg"])
            S.add("dve", lambda e: e.tensor_tensor(out=sg[:], in0=sg[:], in1=omlb[:], op=ALU.mult),
                  reads=["sg", "omlb"], writes=["sg"])
            S.add("dve", lambda e: e.tensor_tensor(out=sg[:], in0=sg[:], in1=lbb[:], op=ALU.add),
                  reads=["sg", "lbb"], writes=["sg"])
            S.add("act", lambda e: e.activation(out=gl[:], in_=sg[:], func=AF.Ln), reads=["sg"], writes=["gl"])
            S.add("dve", lambda e: e.tensor_copy(out=ghi[:], in_=gl[:]), reads=["gl"], writes=["ghi"])
            S.add("dve", lambda e: e.tensor_tensor(out=gr[:], in0=gl[:], in1=ghi[:], op=ALU.subtract),
                  reads=["gl", "ghi"], writes=["gr"])
            S.add("dve", lambda e: e.tensor_copy(out=glo[:], in_=gr[:]), reads=["gr"], writes=["glo"])
            S.add("dve", lambda e: e.tensor_scalar(out=omfb[:], in0=sg[:], scalar1=-1.0, scalar2=1.0, op0=ALU.mult,
                                                   op1=ALU.add), reads=["sg"], writes=["omfb"])
            S.add("act", lambda e: e.activation(out=qsb[:], in_=B[2][:, 0:256], func=AF.Silu),
                  reads=["B2"], writes=["qsb"])
            S.add("act", lambda e: e.activation(out=gs[:], in_=B[3][:, 256:512], func=AF.Silu),
                  reads=["B3"], writes=["gs"])
            S.add("act", lambda e: e.activation(out=vHb[:], in_=B[3][:, 0:256], func=AF.Copy),
                  reads=["B3"], writes=["vHb"])
            def head_ops(h, tt=tt, ts=ts):
                hc = slice(h * 128, (h + 1) * 128)
                X, Y = 4 + h, 6 + h
                BX, BY, BXb, BYb = B[X], B[Y], Bb[X], Bb[Y]
                tX, tY = "B%d" % X, "B%d" % Y
                tk = lambda n: "%s_h%d" % (n, h)
                eb, enb, er = ebs[h], enbs[h], ers[h]
                QtT, Qt0, Qt1, KtT = QtTs[h], Qt0s[h], Qt1s[h], KtTs[h]
                Kh, Kh0, Kh1, scm = Khs[h], Kh0s[h], Kh1s[h], scms[h]
                so, on1, on3 = sos[h], on1s[h], on3s[h]
                S.add("pe", lambda e: e.matmul(BX[:, 0:128], lhsT=ghi[:, hc], rhs=tri[:], start=True, stop=False),
                      reads=["ghi", "tri"], writes=[tX]); yield
                S.add("pe", lambda e: e.matmul(BX[:, 0:128], lhsT=glo[:, hc], rhs=tri[:], start=False, stop=True),
                      reads=["glo", "tri"], writes=[tX]); yield
                S.add("pe", lambda e: e.matmul(BX[:, 128:256], lhsT=stri[:], rhs=ghi[:, hc], start=True, stop=False),
                      reads=["ghi", "stri"], writes=[tX]); yield
                S.add("pe", lambda e: e.matmul(BX[:, 128:256], lhsT=stri[:], rhs=glo[:, hc], start=False, stop=True),
                      reads=["glo", "stri"], writes=[tX]); yield
                S.add("act", lambda e: e.activation(out=eb[:], in_=BX[:, 0:128], func=AF.Exp), reads=[tX],
                      writes=[tk("eb")]); yield
                S.add("act", lambda e: e.activation(out=enb[:], in_=BX[:, 0:128], func=AF.Exp, scale=-1.0),
                      reads=[tX], writes=[tk("enb")]); yield
                S.add("act", lambda e: e.activation(out=er[:], in_=BX[:, 128:256], func=AF.Exp), reads=[tX],
                      writes=[tk("er")]); yield
                S.add("pe", lambda e: e.transpose(BXb[:, 512:640], qsb[:, hc], ident[:]), reads=["qsb", "ident"],
                      writes=[tX]); yield
                S.add("pe", lambda e: e.transpose(BXb[:, 640:768], omfb[:, hc], ident[:]), reads=["omfb", "ident"],
                      writes=[tX]); yield
                S.add("dve", lambda e: e.tensor_tensor(out=QtT[:], in0=BXb[:, 512:640], in1=eb[:], op=ALU.mult),
                      reads=[tX, tk("eb")], writes=[tk("QtT")]); yield
                S.add("pool", lambda e: e.tensor_copy(out=Qt0[:, 0:64], in_=QtT[:, 0:64]), reads=[tk("QtT")],
                      writes=[tk("Qt0")]); yield
                S.add("pool", lambda e: e.tensor_copy(out=Qt1[:, 64:128], in_=QtT[:, 64:128]), reads=[tk("QtT")],
                      writes=[tk("Qt1")]); yield
                S.add("dve", lambda e: e.tensor_tensor(out=KtT[:], in0=BXb[:, 640:768], in1=enb[:], op=ALU.mult),
                      reads=[tX, tk("enb")], writes=[tk("KtT")]); yield
                S.add("dve", lambda e: e.tensor_tensor(out=Kh[:], in0=omfb[:, hc], in1=er[:], op=ALU.mult),
                      reads=["omfb", tk("er")], writes=[tk("Kh")]); yield
                S.add("pool", lambda e: e.tensor_scalar(out=Kh0[:], in0=Kh[:], scalar1=c01[:, 0:1], scalar2=None,
                                                        op0=ALU.mult), reads=[tk("Kh"), "c01"], writes=[tk("Kh0")]); yield
                S.add("pool", lambda e: e.tensor_scalar(out=Kh1[:], in0=Kh[:], scalar1=c01[:, 1:2], scalar2=None,
                                                        op0=ALU.mult), reads=[tk("Kh"), "c01"], writes=[tk("Kh1")]); yield
                S.add("pe", lambda e: e.matmul(BY[:, 0:128], lhsT=KtT[:], rhs=QtT[:], start=True, stop=True),
                      reads=[tk("KtT"), tk("QtT")], writes=[tY]); yield
                S.add("dve", lambda e: e.tensor_tensor(out=scm[:], in0=BY[:, 0:128], in1=m128[:], op=ALU.mult),
                      reads=[tY, "m128"], writes=[tk("scm")]); yield
                S.add("pe", lambda e: e.matmul(BY[:, 128:256], lhsT=scm[:], rhs=vHb[:, hc], start=True, stop=False),
                      reads=[tk("scm"), "vHb"], writes=[tY]); yield
                S.add("pe", lambda e: e.matmul(BY[:, 128:256], lhsT=Qt0[:], rhs=Sbf[h][:], start=False, stop=False),
                      reads=[tk("Qt0"), "Sbf%d" % h], writes=[tY]); yield
                sr = slice(384, 512)
                S.add("pe", lambda e: e.matmul(BX[:, sr], lhsT=Kh0[:], rhs=vHb[:, hc], start=True, stop=True),
                      reads=[tk("Kh0"), "vHb"], writes=[tX]); yield
                S.add("dve", lambda e: e.scalar_tensor_tensor(out=Sst[h][:], in0=Sst[h][:], scalar=eb[:, 63:64],
                                                              in1=BX[:, sr], op0=ALU.mult, op1=ALU.add),
                      reads=["S%d" % h, tk("eb"), tX], writes=["S%d" % h]); yield
                S.add("act", lambda e: e.activation(out=Sbf[h][:], in_=Sst[h][:], func=AF.Copy),
                      reads=["S%d" % h], writes=["Sbf%d" % h]); yield
                S.add("pe", lambda e: e.matmul(BY[:, 128:256], lhsT=Qt1[:], rhs=Sbf[h][:], start=False, stop=True),
                      reads=[tk("Qt1"), "Sbf%d" % h], writes=[tY]); yield
                S.add("pe", lambda e: e.matmul(BX[:, sr], lhsT=Kh1[:], rhs=vHb[:, hc], start=True, stop=True),
                      reads=[tk("Kh1"), "vHb"], writes=[tX]); yield
                S.add("dve", lambda e: e.scalar_tensor_tensor(out=Sst[h][:], in0=Sst[h][:], scalar=eb[:, 127:128],
                                                              in1=BX[:, sr], op0=ALU.mult, op1=ALU.add),
                      reads=["S%d" % h, tk("eb"), tX], writes=["S%d" % h]); yield
                S.add("act", lambda e: e.activation(out=Sbf[h][:], in_=Sst[h][:], func=AF.Copy),
                      reads=["S%d" % h], writes=["Sbf%d" % h]); yield
                S.add("act", lambda e: e.activation(out=junks[h][:], in_=BY[:, 128:256], func=AF.Square,
                                                    accum_out=so[:, 0:1]), reads=[tY], writes=[tk("junk"), tk("so")]); yield
                S.add("act", lambda e: e.activation(out=so[:], in_=so[:], func=AF.Sqrt, scale=1.0 / HD, bias=EPS * HD),
                      reads=[tk("so")], writes=[tk("so")]); yield
                S.add("dve", lambda e: e.reciprocal(out=so[:], in_=so[:]), reads=[tk("so")], writes=[tk("so")]); yield
                S.add("dve", lambda e: e.scalar_tensor_tensor(out=on1[:], in0=BY[:, 128:256], scalar=so[:, 0:1],
                                                              in1=ogb[:, hc], op0=ALU.mult, op1=ALU.mult),
                      reads=[tY, tk("so"), "ogb"], writes=[tk("on1")]); yield
                S.add("pool", lambda e: e.tensor_tensor(out=on3[:], in0=on1[:], in1=gs[:, hc], op=ALU.mult),
                      reads=[tk("on1"), "gs"], writes=[tk("on3")]); yield
                S.add("pe", lambda e: e.transpose(BYb[:, 768:896], on3[:], ident[:]), reads=[tk("on3"), "ident"],
                      writes=[tY]); yield
                ob = ost[(2 * tt + h) % 4]
                otok = "ost%d" % ((2 * tt + h) % 4)
                S.add("act", lambda e: e.activation(out=ob[:], in_=BYb[:, 768:896], func=AF.Copy), reads=[tY],
                      writes=[otok]); yield
                S.dma("sp", out[h * 128:(h + 1) * 128, ts], ob[:], reads=[otok], writes=["out"]); yield

            gens = [head_ops(0), head_ops(1)]
            while gens:
                for g in list(gens):
                    try:
                        next(g)
                    except StopIteration:
                        gens.remove(g)
        _finish(S, st, ["out"])
    return nc


def hgrn_maps(x2, mod, norm1_gain, w_in, hgrn_lower_bounds, hgrn_out_gain):
    idb, _, _ = _consts()
    t64 = np.triu(np.ones((64, 64), np.float32))
    s64 = np.tril(np.ones((64, 64), np.float32), -1)
    z = np.zeros((64, 64), np.float32)
    tri = np.block([[t64, z], [z, t64]]); stri = np.block([[s64, z], [z, s64]])
    c01 = np.zeros((128, 2), np.float32); c01[:64, 0] = 1.0; c01[64:, 1] = 1.0
    maps = []
    o4 = 3 * 2048 + 16
    for c in range(NCORES):
        cr = np.arange(256 * c, 256 * c + 256)
        cols = np.concatenate([o4 + cr, o4 + 2048 + cr, o4 + 4096 + cr, o4 + 6144 + cr])
        bc = lambda v: np.ascontiguousarray(np.broadcast_to(np.asarray(v, np.float32)[None, :], (128, 256)))
        maps.append({
            "x": x2, "g1": _pk(norm1_gain[0]), "sc1": _pk(mod[D:2 * D]), "sh1": _pk(mod[0:D]),
            "w": np.ascontiguousarray(w_in[0][:, cols]),
            "lb0": bc(hgrn_lower_bounds[0][cr]), "lb1": bc(hgrn_lower_bounds[1][cr]),
            "og": bc(hgrn_out_gain[0].reshape(-1)[cr]),
            "ident": idb, "tri": tri.astype(ml_dtypes.bfloat16), "stri": stri.astype(ml_dtypes.bfloat16),
            "m128": tri, "c01": c01,
        })
    return maps


TOK = SEQ // NCORES


def build_c1(ntl=TOK // 128):
    nc = bass.Bass("TRN2", target_bir_lowering=False)
    tok = ntl * 128
    din = lambda n, s, d: nc.dram_tensor(n, s, d, kind="ExternalInput").ap()
    x_in = din("x", [tok, D], F32)
    m_in = din("mT", [D, tok], BF16)
    w_in = din("w", [D, D], F32)
    gt_in = din("gate1", [128, D], F32)
    g2_in = din("g2", [128, 32], F32); sc_in = din("sc2", [128, 32], F32); sh_in = din("sh2", [128, 32], F32)
    id_in = din("ident", [128, 128], BF16)
    x1_out = nc.dram_tensor("x1", [tok, D], F32, kind="ExternalOutput").ap()
    h2_out = nc.dram_tensor("h2T", [D, tok], BF16, kind="ExternalOutput").ap()
    wv = w_in.rearrange("(k p) n -> k p n", p=128)
    mv = m_in.rearrange("(k p) t -> p k t", p=128)
    h2v = h2_out.rearrange("(k p) t -> p k t", p=128)
    S = Sched(nc)
    with ExitStack() as st:
        sb = lambda n, s, d: st.enter_context(nc.sbuf_tensor(n + "_s", s, d))
        wob = sb("wob", [128, 32, 512], BF16)
        wst = [sb("wst%d" % i, [128, 512], F32) for i in range(3)]
        mt = [sb("mt%d" % i, [128, 32, 128], BF16) for i in range(2)]
        xc = [sb("xc%d" % i, [128, 512], F32) for i in range(2)]
        oc_ = [sb("oc%d" % i, [128, 512], F32) for i in range(2)]
        gt = sb("gt", [128, D], F32)
        a2 = sb("a2", [128, 32], F32); sh2 = sb("sh2", [128, 32], F32); g2 = sb("g2", [128, 32], F32)
        ident = sb("ident", [128, 128], BF16)
        xb = sb("xb", [128, D], F32); xs = sb("xs", [128, D], BF16); junk = sb("junk", [128, D], BF16)
        ss = sb("ss", [128, ntl], F32)
        hT = [sb("hT%d" % i, [128, 32, 128], BF16) for i in range(2)]
        B = [st.enter_context(nc.psum_tensor("B%d" % i, [128, 512], F32)) for i in range(8)]
        Bb = [b[:].bitcast(BF16) for b in B]
        for (t, src, name) in ((g2, g2_in, "g2"), (a2, sc_in, "a2"), (sh2, sh_in, "sh2"), (ident, id_in, "ident"),
                               (gt, gt_in, "gt")):
            S.dma("sp", t[:], src[:, :], writes=[name])
        S.add("dve", lambda e: e.scalar_tensor_tensor(out=a2[:], in0=a2[:], scalar=1.0, in1=g2[:], op0=ALU.add,
                                                      op1=ALU.mult), reads=["a2", "g2"], writes=["a2"])
        n = 0
        for cg in range(8):
            cs = slice(cg * 512, (cg + 1) * 512)
            for k in range(32):
                S.dma("sp", wst[k % 3][:], wv[k][:, cs], writes=["wst%d" % (k % 3)])
                eng = ("act", "dve", "pool")[k % 3]
                if eng == "act":
                    S.add("act", lambda e, k=k: e.activation(out=wob[:, k, :], in_=wst[k % 3][:], func=AF.Copy),
                          reads=["wst%d" % (k % 3)], writes=["wob"])
                else:
                    S.add(eng, lambda e, k=k: e.tensor_copy(out=wob[:, k, :], in_=wst[k % 3][:]),
                          reads=["wst%d" % (k % 3)], writes=["wob"])
            for tt in range(ntl):
                ts = slice(tt * 128, (tt + 1) * 128)
                mb = mt[n % 2]; mtok = "mt%d" % (n % 2)
                xcb = xc[n % 2]; xtok = "xc%d" % (n % 2)
                ob = oc_[n % 2]; otok = "oc%d" % (n % 2)
                bk = 2 + (n % 2)
                n += 1
                S.dma("sp", mb[:], mv[:, :, ts], writes=[mtok])
                S.dma("sp", xcb[:], x_in[ts, cs], writes=[xtok])
                for k in range(32):
                    S.add("pe", lambda e, k=k, mb=mb, bk=bk: e.matmul(B[bk][:, :], lhsT=mb[:, k, :], rhs=wob[:, k, :],
                                                                     start=(k == 0), stop=(k == 31)),
                          reads=[mtok, "wob"], writes=["B%d" % bk])
                S.add("dve", lambda e, ob=ob, bk=bk, cs=cs: e.tensor_tensor(out=ob[:], in0=B[bk][:, :], in1=gt[:, cs],
                                                                           op=ALU.mult),
                      reads=["B%d" % bk, "gt"], writes=[otok])
                S.add("pool", lambda e, ob=ob, xcb=xcb: e.tensor_tensor(out=ob[:], in0=ob[:], in1=xcb[:], op=ALU.add),
                      reads=[otok, xtok], writes=[otok])
                S.dma("sp", x1_out[ts, cs], ob[:], reads=[otok], writes=["x1"])
        for tt in range(ntl):
            ts = slice(tt * 128, (tt + 1) * 128)
            S.dma("sp", xb[:], x1_out[ts, :], reads=["x1"], writes=["xb"])
            S.add("act", lambda e, tt=tt: e.activation(out=junk[:], in_=xb[:], func=AF.Square,
                                                        accum_out=ss[:, tt:tt + 1]), reads=["xb"], writes=["junk", "ss"])
            S.add("act", lambda e, tt=tt: e.activation(out=ss[:, tt:tt + 1], in_=ss[:, tt:tt + 1], func=AF.Sqrt,
                                                        scale=1.0 / D, bias=EPS), reads=["ss"], writes=["ss"])
            S.add("dve", lambda e, tt=tt: e.reciprocal(out=ss[:, tt:tt + 1], in_=ss[:, tt:tt + 1]),
                  reads=["ss"], writes=["ss"])
            S.add("dve", lambda e, tt=tt: e.tensor_scalar(out=xs[:], in0=xb[:], scalar1=ss[:, tt:tt + 1], scalar2=None,
                                                           op0=ALU.mult), reads=["xb", "ss"], writes=["xs"])
            hb = hT[tt % 2]
            htok = "hT%d" % (tt % 2)
            for k4 in range(8):
                bk = k4 % 2
                for j in range(4):
                    k = k4 * 4 + j
                    S.add("pe", lambda e, k=k, j=j, bk=bk: e.transpose(Bb[bk][:, j * 128:(j + 1) * 128],
                                                                       xs[:, k * 128:(k + 1) * 128], ident[:]),
                          reads=["xs", "ident"], writes=["B%d" % bk])
                for j in range(4):
                    k = k4 * 4 + j
                    S.add("act", lambda e, k=k, j=j, bk=bk, hb=hb: e.activation(
                        out=hb[:, k, :], in_=Bb[bk][:, j * 128:(j + 1) * 128], func=AF.Identity,
                        scale=a2[:, k:k + 1], bias=sh2[:, k:k + 1]),
                        reads=["B%d" % bk, "a2", "sh2"], writes=[htok])
            S.dma("sp", h2v[:, :, ts], hb[:], reads=[htok], writes=["h2"])
        _finish(S, st, ["x1", "h2"])
    return nc


def c1_maps(x2, mergedT, mod, norm2_gain, w_out):
    idb, _, _ = _consts()
    maps = []
    gate1 = np.ascontiguousarray(np.broadcast_to(mod[2 * D:3 * D][None, :], (128, D))).astype(np.float32)
    wo = np.ascontiguousarray(w_out[0])
    for c in range(NCORES):
        ts = slice(c * TOK, (c + 1) * TOK)
        maps.append({"x": np.ascontiguousarray(x2[ts]), "mT": np.ascontiguousarray(mergedT[:, ts]), "w": wo,
                     "gate1": gate1, "g2": _pk(norm2_gain[0]), "sc2": _pk(mod[4 * D:5 * D]), "sh2": _pk(mod[3 * D:4 * D]),
                     "ident": idb})
    return maps


def build_p0():
    nc = bass.Bass("TRN2", target_bir_lowering=False)
    din = lambda n, s, d: nc.dram_tensor(n, s, d, kind="ExternalInput").ap()
    ut_in = din("UT", [D, 2048], F32); v_in = din("V", [2048, D], F32); wq_in = din("wq", [512, 2048], F32)
    ut_o = nc.dram_tensor("UTb", [D, 2048], BF16, kind="ExternalOutput").ap()
    v_o = nc.dram_tensor("Vb", [2048, D], BF16, kind="ExternalOutput").ap()
    wq_o = nc.dram_tensor("wqb", [512, 2048], BF16, kind="ExternalOutput").ap()
    S = Sched(nc)
    with ExitStack() as st:
        sb = lambda n, s, d: st.enter_context(nc.sbuf_tensor(n + "_s", s, d))
        fi = [sb("fi%d" % i, [128, 4096], F32) for i in range(3)]
        bo = [sb("bo%d" % i, [128, 4096], BF16) for i in range(3)]
        jobs = []
        for r in range(0, D, 256):
            jobs.append((ut_in[r:r + 256, :].rearrange("(a p) n -> p a n", p=128),
                         ut_o[r:r + 256, :].rearrange("(a p) n -> p a n", p=128), True))
        for r in range(0, 2048, 128):
            jobs.append((v_in[r:r + 128, :], v_o[r:r + 128, :], False))
        for r in range(0, 512, 256):
            jobs.append((wq_in[r:r + 256, :].rearrange("(a p) n -> p a n", p=128),
                         wq_o[r:r + 256, :].rearrange("(a p) n -> p a n", p=128), True))
        for i, (src, dst, two) in enumerate(jobs):
            f = fi[i % 3]; b = bo[i % 3]
            fv = f[:].rearrange("p (a n) -> p a n", a=2) if two else f[:]
            bv = b[:].rearrange("p (a n) -> p a n", a=2) if two else b[:]
            S.dma("sp", fv, src, writes=["fi%d" % (i % 3)])
            eng = ("act", "dve", "pool")[i % 3]
            if eng == "act":
                S.add("act", lambda e, f=f, b=b: e.activation(out=b[:], in_=f[:], func=AF.Copy),
                      reads=["fi%d" % (i % 3)], writes=["bo%d" % (i % 3)])
            else:
                S.add(eng, lambda e, f=f, b=b: e.tensor_copy(out=b[:], in_=f[:]),
                      reads=["fi%d" % (i % 3)], writes=["bo%d" % (i % 3)])
            S.dma("sp", dst, bv, reads=["bo%d" % (i % 3)], writes=["out"])
        _finish(S, st, ["out"])
    return nc


def p0_maps(peer_w_query, peer_u, peer_v):
    UT = peer_u[0].T
    maps = []
    for c in range(NCORES):
        es = slice(c * 2048, (c + 1) * 2048)
        maps.append({"UT": np.ascontiguousarray(UT[:, es]), "V": np.ascontiguousarray(peer_v[0][es]),
                     "wq": np.ascontiguousarray(peer_w_query[0][c * 512:(c + 1) * 512])})
    return maps


NEXP_B = 128


def build_c2(nq=4, tq=2, nb=NEXP_B, G=2):
    nc = bass.Bass("TRN2", target_bir_lowering=False)
    tokq = tq * 128
    tok = nq * tokq
    din = lambda n, s, d: nc.dram_tensor(n, s, d, kind="ExternalInput").ap()
    h_in = din("h2T", [D, tok], BF16)
    x1_in = din("x1", [tok, D], F32)
    g2_in = din("gate2", [128, D], F32)
    wq_in = din("wq", [D, 2048], BF16)
    kt_in = din("keysT", [128, 16, 128], F32)
    ut_in = din("UT", [D, nb * 128], BF16)
    v_in = din("V", [nb * 128, D], BF16)
    id_in = din("ident", [128, 128], BF16)
    y_out = nc.dram_tensor("y", [tok, D], F32, kind="ExternalOutput").ap()
    hv = h_in.rearrange("(k p) t -> p k t", p=128)
    wqv = wq_in.rearrange("(k p) n -> p k n", p=128)
    utv = ut_in.rearrange("(k p) e -> p k e", p=128)
    AX = mybir.AxisListType.X
    S = Sched(nc)
    with ExitStack() as st:
        sb = lambda n, s, d: st.enter_context(nc.sbuf_tensor(n + "_s", s, d))
        hh = sb("hh", [128, 32, tokq], BF16)
        ub = [sb("ub%d" % i, [128, 32, 128], BF16) for i in range(2)]
        qpT = sb("qpT", [128, 16, tokq], BF16)
        kTb = sb("kTb", [128, 16, 128], BF16)
        scr_x = sb("scr_x", [128, 2, 8, 128], F32)
        scr_g = sb("scr_g", [128, 2, 8, 128], F32)
        scr_e = sb("scr_e", [128, 2, 8, 128], F32)
        X2 = [sb("X2_%d" % i, [128, 8, 128], F32) for i in range(2)]
        Xt = [scr_x[:, i] for i in range(2)]; Gh = [scr_g[:, i] for i in range(2)]; Ee = [scr_e[:, i] for i in range(2)]
        ktf = scr_x[:].rearrange("p a h n -> p (a h) n")
        sc = scr_g[:].rearrange("p a h n -> p (a h) n")
        cand = scr_e[:].rearrange("p a h n -> p (a h n)").rearrange("p (h c) -> p h c", h=8)
        XT, GH, EE = ["Xt0", "Xt1"], ["Gh0", "Gh1"], ["Ee0", "Ee1"]
        mx = sb("mx", [128, 16, 16], F32); tmpv = sb("tmpv", [128, 128], F32); tmpc = sb("tmpc", [128, 256], F32)
        c16 = sb("c16", [128, 8, 16], F32)
        th = sb("th", [128, 8], F32); mm = sb("mm", [128, 8], F32); nm = sb("nm", [128, 8], F32)
        zz = sb("zz", [128, 8], F32); m2 = sb("m2", [128, 8], F32); dm = sb("dm", [128, 8], F32)
        ej = sb("ej", [128, 16], F32)
        L2 = [sb("L2_%d" % i, [128, 8, 128], F32) for i in range(tq)]
        D1 = [sb("D1_%d" % i, [128, 8, 128], F32) for i in range(tq)]
        e1 = [sb("e1_%d" % i, [128, 8], F32) for i in range(tq)]
        acc = sb("acc", [128, tq, D], F32)
        vbs = [sb("vb%d" % i, [128, D], BF16) for i in range(2 * G)]
        ga = [sb("ga%d" % i, [128, tokq], F32) for i in range(2)]
        Gtf = [sb("Gtf%d" % i, [128, 128], F32) for i in range(2)]
        Gt = [sb("Gt%d" % i, [128, 128], BF16) for i in range(4)]
        wTs = [sb("wT%d" % i, [128, tokq], BF16) for i in range(2 * G)]
        gch = sb("gch", [128, 1024], F32); xch = [sb("xch%d" % i, [128, 1024], F32) for i in range(2)]
        ident = sb("ident", [128, 128], BF16)
        B = [st.enter_context(nc.psum_tensor("B%d" % i, [128, 512], F32)) for i in range(8)]
        Bb = [b[:].bitcast(BF16) for b in B]

        S.dma("sp", ident[:], id_in[:, :], writes=["ident"])
        S.dma("sp", ktf, kt_in[:, :, :], writes=XT)
        S.add("dve", lambda e: e.tensor_copy(out=kTb[:], in_=ktf), reads=XT, writes=["kTb"])
        for qi in range(nq):
            t0 = qi * tokq
            S.dma("sp", hh[:], hv[:, :, t0:t0 + tokq], writes=["hh"])
            for hp in range(16):
                u = ub[hp % 2]; utok = "ub%d" % (hp % 2)
                S.dma("sp", u[:], wqv[:, :, hp * 128:(hp + 1) * 128], writes=[utok])
                bk = 2 + hp % 2
                for k in range(32):
                    S.add("pe", lambda e, k=k, bk=bk, u=u: e.matmul(B[bk][:, 0:tokq], lhsT=u[:, k, :], rhs=hh[:, k, :],
                                                                   start=(k == 0), stop=(k == 31)),
                          reads=[utok, "hh"], writes=["B%d" % bk])
                S.add("act", lambda e, hp=hp, bk=bk: e.activation(out=qpT[:, hp, :], in_=B[bk][:, 0:tokq], func=AF.Copy),
                      reads=["B%d" % bk], writes=["qpT"])
            for tt in range(tq):
                tsl = slice(tt * 128, (tt + 1) * 128)
                for hp in range(16):
                    bk = 4 + hp // 4
                    S.add("pe", lambda e, hp=hp, bk=bk, tsl=tsl: e.matmul(
                        B[bk][:, (hp % 4) * 128:(hp % 4 + 1) * 128], lhsT=qpT[:, hp, tsl], rhs=kTb[:, hp, :],
                        start=True, stop=True), reads=["qpT", "kTb"], writes=["B%d" % bk])
                for g in range(4):
                    S.add("act", lambda e, g=g: e.activation(
                        out=sc[:, g * 4:(g + 1) * 4, :].rearrange("p a n -> p (a n)"), in_=B[4 + g][:, :], func=AF.Copy),
                        reads=["B%d" % (4 + g)], writes=GH)
                for hp in range(16):
                    S.add("dve", lambda e, hp=hp: e.max(out=mx[:, hp, 0:8], in_=sc[:, hp, :]), reads=GH, writes=["mx"])
                    S.add("dve", lambda e, hp=hp: e.match_replace(out=tmpv[:], in_to_replace=mx[:, hp, 0:8],
                                                                  in_values=sc[:, hp, :], imm_value=-1e30),
                          reads=GH + ["mx"], writes=["tmpv"])
                    S.add("dve", lambda e, hp=hp: e.max(out=mx[:, hp, 8:16], in_=tmpv[:]), reads=["tmpv"], writes=["mx"])
                for h in range(8):
                    S.add("dve", lambda e, h=h: e.tensor_tensor(
                        out=cand[:, h, :].rearrange("p (a b) -> p a b", a=16),
                        in0=mx[:, 2 * h, :].unsqueeze(2).to_broadcast([128, 16, 16]),
                        in1=mx[:, 2 * h + 1, :].unsqueeze(1).to_broadcast([128, 16, 16]), op=ALU.add),
                        reads=["mx"], writes=EE)
                    S.add("dve", lambda e, h=h: e.max(out=c16[:, h, 0:8], in_=cand[:, h, :]), reads=EE, writes=["c16"])
                    S.add("dve", lambda e, h=h: e.match_replace(out=tmpc[:], in_to_replace=c16[:, h, 0:8],
                                                                in_values=cand[:, h, :], imm_value=-1e30),
                          reads=EE + ["c16"], writes=["tmpc"])
                    S.add("dve", lambda e, h=h: e.max(out=c16[:, h, 8:16], in_=tmpc[:]), reads=["tmpc"], writes=["c16"])
                S.add("dve", lambda e: e.tensor_reduce(out=th[:], in_=c16[:], axis=AX, op=ALU.min), reads=["c16"], writes=["th"])
                S.add("dve", lambda e: e.tensor_reduce(out=mm[:], in_=c16[:], axis=AX, op=ALU.max), reads=["c16"], writes=["mm"])
                S.add("dve", lambda e: e.tensor_scalar(out=nm[:], in0=mm[:], scalar1=-1.0, scalar2=None, op0=ALU.mult),
                      reads=["mm"], writes=["nm"])
                for h in range(8):
                    S.add("act", lambda e, h=h: e.activation(out=ej[:], in_=c16[:, h, :], func=AF.Exp, bias=nm[:, h:h + 1],
                                                              accum_out=zz[:, h:h + 1]),
                          reads=["c16", "nm"], writes=["ej", "zz"])
                S.add("act", lambda e: e.activation(out=zz[:], in_=zz[:], func=AF.Ln), reads=["zz"], writes=["zz"])
                sc4 = sc.rearrange("p (h two) n -> p h two n", two=2)
                S.add("dve", lambda e: e.tensor_reduce(out=m2[:], in_=sc4[:, :, 1, :], axis=AX, op=ALU.max),
                      reads=GH, writes=["m2"])
                S.add("dve", lambda e, tt=tt: e.tensor_tensor(out=L2[tt][:], in0=sc4[:, :, 1, :],
                                                              in1=m2[:].unsqueeze(2).to_broadcast([128, 8, 128]),
                                                              op=ALU.subtract), reads=GH + ["m2"], writes=["L2_%d" % tt])
                S.add("dve", lambda e: e.tensor_tensor(out=dm[:], in0=m2[:], in1=th[:], op=ALU.subtract),
                      reads=["m2", "th"], writes=["dm"])
                S.add("dve", lambda e: e.tensor_scalar(out=dm[:], in0=dm[:], scalar1=1e-4, scalar2=None, op0=ALU.add),
                      reads=["dm"], writes=["dm"])
                S.add("dve", lambda e, tt=tt: e.tensor_tensor(out=D1[tt][:], in0=sc4[:, :, 0, :],
                                                              in1=dm[:].unsqueeze(2).to_broadcast([128, 8, 128]),
                                                              op=ALU.add), reads=GH + ["dm"], writes=["D1_%d" % tt])
                S.add("dve", lambda e, tt=tt: e.tensor_tensor(out=e1[tt][:], in0=th[:], in1=mm[:], op=ALU.subtract),
                      reads=["th", "mm"], writes=["e1_%d" % tt])
                S.add("dve", lambda e, tt=tt: e.tensor_tensor(out=e1[tt][:], in0=e1[tt][:], in1=zz[:], op=ALU.subtract),
                      reads=["e1_%d" % tt, "zz"], writes=["e1_%d" % tt])
            S.add("pool", lambda e: e.memset(acc[:], 0.0), writes=["acc"])
            gcnt = [0]

            def stage_a(b):
                u = ub[b % 2]; utok = "ub%d" % (b % 2)
                v = vbs[b % (2 * G)]; vtok = "vb%d" % (b % (2 * G))
                S.dma("sp", u[:], utv[:, :, b * 128:(b + 1) * 128], writes=[utok])
                S.dma("sp", v[:], v_in[b * 128:(b + 1) * 128, :], writes=[vtok])
                pb = 2 + b % 2
                for k in range(32):
                    S.add("pe", lambda e, k=k: e.matmul(B[pb][:, 0:tokq], lhsT=u[:, k, :], rhs=hh[:, k, :],
                                                        start=(k == 0), stop=(k == 31)),
                          reads=[utok, "hh"], writes=["B%d" % pb])
                for tt in range(tq):
                    i = gcnt[0] % 2
                    gcnt[0] += 1
                    gi = (b % 2) * 2 + tt
                    S.add("dve", lambda e, tt=tt, i=i: e.tensor_tensor(
                        out=Xt[i], in0=L2[tt][:], in1=D1[tt][:, :, b:b + 1].to_broadcast([128, 8, 128]), op=ALU.add),
                        reads=["L2_%d" % tt, "D1_%d" % tt], writes=[XT[i]])
                    S.add("pool", lambda e, tt=tt, i=i: e.tensor_tensor(
                        out=X2[i][:], in0=Xt[i], in1=e1[tt][:].unsqueeze(2).to_broadcast([128, 8, 128]), op=ALU.add),
                        reads=[XT[i], "e1_%d" % tt], writes=["X2_%d" % i])
                    S.add("act", lambda e, i=i: e.activation(out=Ee[i], in_=X2[i][:], func=AF.Exp),
                          reads=["X2_%d" % i], writes=[EE[i]])
                    S.add("dve", lambda e, i=i: e.scalar_tensor_tensor(out=Gh[i], in0=Xt[i], scalar=0.0, in1=Ee[i],
                                                                       op0=ALU.is_ge, op1=ALU.mult),
                          reads=[XT[i], EE[i]], writes=[GH[i]])
                    S.add("pool", lambda e, i=i: e.tensor_tensor(out=Gh[i][:, 0:4, :], in0=Gh[i][:, 0:4, :], in1=Gh[i][:, 4:8, :],
                                                                 op=ALU.add), reads=[GH[i]], writes=[GH[i]])
                    S.add("pool", lambda e, i=i: e.tensor_tensor(out=Gh[i][:, 0:2, :], in0=Gh[i][:, 0:2, :], in1=Gh[i][:, 2:4, :],
                                                                 op=ALU.add), reads=[GH[i]], writes=[GH[i]])
                    S.add("pool", lambda e, i=i, gi=gi: e.tensor_tensor(out=Gt[gi][:], in0=Gh[i][:, 0, :], in1=Gh[i][:, 1, :],
                                                                        op=ALU.add), reads=[GH[i]], writes=["Gt%d" % gi])

            def stage_b(b):
                pb = 2 + b % 2
                tb = b % 2
                g_ = ga[b % 2]; gtok = "ga%d" % (b % 2)
                w = wTs[b % (2 * G)]; wtok = "wT%d" % (b % (2 * G))
                for tt in range(tq):
                    gi = (b % 2) * 2 + tt
                    S.add("pe", lambda e, tt=tt, gi=gi: e.transpose(Bb[tb][:, tt * 128:(tt + 1) * 128], Gt[gi][:], ident[:]),
                          reads=["Gt%d" % gi, "ident"], writes=["B%d" % tb])
                S.add("act", lambda e: e.activation(out=g_[:], in_=B[pb][:, 0:tokq], func=AF.Gelu),
                      reads=["B%d" % pb], writes=[gtok])
                S.add("dve", lambda e: e.tensor_tensor(out=w[:], in0=g_[:], in1=Bb[tb][:, 0:tokq], op=ALU.mult),
                      reads=[gtok, "B%d" % tb], writes=[wtok])

            pcnt = [0]

            def stage_c(b0):
                for tt in range(tq):
                    for dc in range(8):
                        bk = 4 + pcnt[0] % 4
                        pcnt[0] += 1
                        for j in range(G):
                            b = b0 + j
                            w = wTs[b % (2 * G)]; wtok = "wT%d" % (b % (2 * G))
                            v = vbs[b % (2 * G)]; vtok = "vb%d" % (b % (2 * G))
                            S.add("pe", lambda e, tt=tt, dc=dc, bk=bk, w=w, v=v, j=j: e.matmul(
                                B[bk][:, :], lhsT=w[:, tt * 128:(tt + 1) * 128], rhs=v[:, dc * 512:(dc + 1) * 512],
                                start=(j == 0), stop=(j == G - 1)), reads=[wtok, vtok], writes=["B%d" % bk])
                        S.add("dve", lambda e, tt=tt, dc=dc, bk=bk: e.tensor_tensor(
                            out=acc[:, tt, dc * 512:(dc + 1) * 512], in0=acc[:, tt, dc * 512:(dc + 1) * 512],
                            in1=B[bk][:, :], op=ALU.add), reads=["acc", "B%d" % bk], writes=["acc"])

            stage_a(0)
            for b in range(nb):
                if b + 1 < nb:
                    stage_a(b + 1)
                stage_b(b)
                if (b + 1) % G == 0:
                    stage_c(b + 1 - G)
            n = 0
            for cc in range(4):
                cs_ = slice(cc * 1024, (cc + 1) * 1024)
                S.dma("sp", gch[:], g2_in[:, cs_], writes=["gch"])
                for tt in range(tq):
                    rs = slice(t0 + tt * 128, t0 + (tt + 1) * 128)
                    xc = xch[n % 2]; xtok = "xch%d" % (n % 2)
                    n += 1
                    S.dma("sp", xc[:], x1_in[rs, cs_], writes=[xtok])
                    S.add("dve", lambda e, tt=tt, cs_=cs_: e.tensor_tensor(out=acc[:, tt, cs_], in0=acc[:, tt, cs_], in1=gch[:],
                                                                          op=ALU.mult), reads=["acc", "gch"], writes=["acc"])
                    S.add("pool", lambda e, tt=tt, cs_=cs_, xc=xc: e.tensor_tensor(out=xc[:], in0=xc[:], in1=acc[:, tt, cs_],
                                                                                 op=ALU.add), reads=["acc", xtok], writes=[xtok])
                    S.dma("sp", y_out[rs, cs_], xc[:], reads=[xtok], writes=["y"])
        _finish(S, st, ["y"])
    return nc


def c2_maps(h2T, x1, mod, wqb, peer_sub_keys, UTb, Vb, tok):
    idb, _, _ = _consts()
    gate2 = np.ascontiguousarray(np.broadcast_to(mod[5 * D:6 * D][None, :], (128, D))).astype(np.float32)
    keysT = np.ascontiguousarray(np.transpose(peer_sub_keys[0].reshape(16, 128, 128), (2, 0, 1)))
    maps = []
    for c in range(NCORES):
        ts = slice(c * tok, (c + 1) * tok)
        maps.append({"h2T": np.ascontiguousarray(h2T[:, ts]), "x1": np.ascontiguousarray(x1[ts]), "gate2": gate2,
                     "wq": wqb, "keysT": keysT, "UT": UTb, "V": Vb, "ident": idb})
    return maps


def _run(nc, maps):
    return run_bass_kernel_spmd(nc, maps, core_ids=list(range(NCORES))).results


def kernel(x, c, ada_w, ada_b, norm1_gain, norm2_gain, w_in, fox_f_bias, fox_q_gain, fox_k_gain,
           hgrn_lower_bounds, hgrn_out_gain, w_out, peer_w_query, peer_sub_keys, peer_u, peer_v):
    f = lambda a: np.asarray(a)
    x, w_in = f(x), f(w_in)
    mod = run_mod(f(c), f(ada_w), f(ada_b))
    x2 = np.ascontiguousarray(x[0])
    fox = _run(build_fox(), fox_maps(x2, mod, f(norm1_gain), w_in, f(fox_f_bias), f(fox_q_gain), f(fox_k_gain)))
    hg = _run(build_hgrn(), hgrn_maps(x2, mod, f(norm1_gain), w_in, f(hgrn_lower_bounds), f(hgrn_out_gain)))
    mergedT = np.concatenate([np.asarray(r["mT"]) for r in fox] + [np.asarray(r["mT"]) for r in hg], axis=0)
    del fox, hg
    c1 = _run(build_c1(), c1_maps(x2, mergedT, mod, f(norm2_gain), f(w_out)))
    x1 = np.concatenate([np.asarray(r["x1"]) for r in c1], axis=0)
    h2T = np.concatenate([np.asarray(r["h2T"]) for r in c1], axis=1)
    del c1, mergedT
    p0 = _run(build_p0(), p0_maps(f(peer_w_query), f(peer_u), f(peer_v)))
    UTb = np.concatenate([np.asarray(r["UTb"]) for r in p0], axis=1)
    Vb = np.concatenate([np.asarray(r["Vb"]) for r in p0], axis=0)
    wqb = np.concatenate([np.asarray(r["wqb"]) for r in p0], axis=0)
    del p0
    c2 = _run(build_c2(), c2_maps(h2T, x1, mod, wqb, f(peer_sub_keys), UTb, Vb, TOK))
    y = np.concatenate([np.asarray(r["y"]) for r in c2], axis=0)
    return y.reshape(1, SEQ, D).astype(np.float32)
```

```python
from contextlib import ExitStack

import numpy as np
import ml_dtypes

import concourse.bass as bass
import concourse.mybir as mybir
from concourse.bass_utils import run_bass_kernel_spmd

F32 = mybir.dt.float32
BF16 = mybir.dt.bfloat16
AF = mybir.ActivationFunctionType
ALU = mybir.AluOpType

NCORES = 8
D = 4096
SEQ = 8192
HD = 128
EPS = 1e-6
ENGS = ("pe", "act", "dve", "pool", "sp")


class _Op:
    __slots__ = ("eng", "fn", "deps", "dma", "sig", "sem", "val", "idx")

    def __init__(self, eng, fn, dma):
        self.eng = eng; self.fn = fn; self.deps = set(); self.dma = dma
        self.sig = False; self.sem = None; self.val = 0; self.idx = 0


class Sched:
    def __init__(self, nc, n_dsem=10):
        self.nc = nc
        self.ops = {e: [] for e in ENGS}
        self.lastw = {}
        self.readers = {}
        self.n_dsem = n_dsem

    def add(self, eng, fn, reads=(), writes=(), dma=False):
        op = _Op(eng, fn, dma)
        deps = set()
        for t in reads:
            w = self.lastw.get(t)
            if w is not None:
                deps.add(w)
        for t in writes:
            w = self.lastw.get(t)
            if w is not None:
                deps.add(w)
            for r in self.readers.get(t, ()):
                deps.add(r)
        for t in reads:
            self.readers.setdefault(t, []).append(op)
        for t in writes:
            self.lastw[t] = op
            self.readers[t] = []
        deps.discard(op)
        op.deps = deps
        op.idx = len(self.ops[eng])
        self.ops[eng].append(op)
        return op

    def dma(self, eng, out, in_, reads=(), writes=()):
        return self.add(eng, lambda e: e.dma_start(out=out, in_=in_), reads, writes, dma=True)

    def emit(self, stack):
        nc = self.nc

        def skip(d, op):
            return d.eng == "pe" and op.eng == "pe" and not d.dma and not op.dma

        for e in ENGS:
            for op in self.ops[e]:
                for d in op.deps:
                    if not skip(d, op):
                        d.sig = True
        csem = {e: stack.enter_context(nc.semaphore("c_" + e)) for e in ENGS}
        dsems = {e: [stack.enter_context(nc.semaphore("d_%s%d" % (e, i))) for i in range(self.n_dsem)]
                 for e in ("sp", "act", "pool")}
        for e in ENGS:
            cnt = 0
            dcnt = 0
            duse = [0] * self.n_dsem
            for op in self.ops[e]:
                if op.dma:
                    k = dcnt % self.n_dsem
                    dcnt += 1
                    duse[k] += 1
                    op.sem = dsems[e][k]
                    op.val = 16 * duse[k]
                    op.sig = True
                elif op.sig:
                    cnt += 1
                    op.sem = csem[e]
                    op.val = cnt
        blk = stack.enter_context(nc.Block())

        def run(e):
            def body(eng):
                waited = {}
                dprev = {}

                def wait(d):
                    key = id(d.sem)
                    if waited.get(key, 0) >= d.val:
                        return
                    eng.wait_ge(d.sem, d.val)
                    waited[key] = d.val

                for op in self.ops[e]:
                    for d in sorted(op.deps, key=lambda o: (o.eng, o.idx)):
                        if not skip(d, op):
                            wait(d)
                    if op.dma:
                        p = dprev.get(id(op.sem))
                        if p is not None:
                            wait(p)
                        dprev[id(op.sem)] = op
                    ins = op.fn(eng)
                    if op.sig:
                        ins.then_inc(op.sem, 16 if op.dma else 1)
            return body

        blk.tensor(run("pe"))
        blk.scalar(run("act"))
        blk.vector(run("dve"))
        blk.gpsimd(run("pool"))
        blk.sync(run("sp"))


def _finish(S, st, out_tokens):
    S.add("sp", lambda e: e.nop(), reads=list(out_tokens))
    S.emit(st)


MODC = 6 * D // NCORES


def build_mod():
    nc = bass.Bass("TRN2", target_bir_lowering=False)
    c_in = nc.dram_tensor("c", [128, 32], F32, kind="ExternalInput").ap()
    w_in = nc.dram_tensor("w", [D, MODC], F32, kind="ExternalInput").ap()
    b_in = nc.dram_tensor("b", [1, MODC], F32, kind="ExternalInput").ap()
    out = nc.dram_tensor("mod", [1, MODC], F32, kind="ExternalOutput").ap()
    wv = w_in.rearrange("(p k) n -> k p n", k=32)
    S = Sched(nc)
    with ExitStack() as st:
        ct = st.enter_context(nc.sbuf_tensor("ct", [128, 32], F32))
        cs = st.enter_context(nc.sbuf_tensor("cs", [128, 32], F32))
        acc = st.enter_context(nc.sbuf_tensor("acc", [128, MODC], F32))
        wts = [st.enter_context(nc.sbuf_tensor("wt%d" % i, [128, MODC], F32)) for i in range(3)]
        ones = st.enter_context(nc.sbuf_tensor("ones", [128, 1], F32))
        bt = st.enter_context(nc.sbuf_tensor("bt", [1, MODC], F32))
        res = st.enter_context(nc.sbuf_tensor("res", [1, MODC], F32))
        pss = [st.enter_context(nc.psum_tensor("ps%d" % i, [128, 512], F32)) for i in range(6)]
        S.dma("sp", ct[:], c_in[:, :], writes=["ct"])
        S.dma("sp", bt[:], b_in[:, :], writes=["bt"])
        S.add("act", lambda e: e.activation(out=cs[:], in_=ct[:], func=AF.Silu), reads=["ct"], writes=["cs"])
        S.add("pool", lambda e: e.memset(ones[:], 1.0), writes=["ones"])
        for k in range(32):
            wt = wts[k % 3]
            tok = "wt%d" % (k % 3)
            S.dma("sp", wt[:], wv[k], writes=[tok])
            if k == 0:
                S.add("dve", lambda e, wt=wt: e.tensor_scalar(out=acc[:], in0=wt[:], scalar1=cs[:, 0:1], scalar2=None,
                                                               op0=ALU.mult), reads=[tok, "cs"], writes=["acc"])
            else:
                S.add("dve", lambda e, wt=wt, k=k: e.scalar_tensor_tensor(out=acc[:], in0=wt[:], scalar=cs[:, k:k + 1],
                                                                           in1=acc[:], op0=ALU.mult, op1=ALU.add),
                      reads=[tok, "cs", "acc"], writes=["acc"])
        for j in range(6):
            S.add("pe", lambda e, j=j: e.matmul(pss[j][0:1, :], lhsT=ones[:, 0:1], rhs=acc[:, j * 512:(j + 1) * 512],
                                                start=True, stop=True), reads=["ones", "acc"], writes=["ps%d" % j])
            S.add("dve", lambda e, j=j: e.tensor_tensor(out=res[0:1, j * 512:(j + 1) * 512], in0=pss[j][0:1, :],
                                                        in1=bt[0:1, j * 512:(j + 1) * 512], op=ALU.add),
                  reads=["ps%d" % j, "bt"], writes=["res%d" % j])
        S.dma("sp", out[:, :], res[:], reads=["res%d" % j for j in range(6)], writes=["out"])
        _finish(S, st, ["out"])
    return nc


def run_mod(c, ada_w, ada_b):
    nc = build_mod()
    c2 = np.ascontiguousarray(c.reshape(128, 32))
    maps = []
    for i in range(NCORES):
        sl = slice(i * MODC, (i + 1) * MODC)
        maps.append({"c": c2, "w": np.ascontiguousarray(ada_w[0][:, sl]),
                     "b": np.ascontiguousarray(ada_b[0][sl].reshape(1, MODC))})
    res = run_bass_kernel_spmd(nc, maps, core_ids=list(range(NCORES)))
    return np.concatenate([np.asarray(r["mod"]).reshape(-1) for r in res.results])


def build_n1(ntl=SEQ // NCORES // 128):
    nc = bass.Bass("TRN2", target_bir_lowering=False)
    tok = ntl * 128
    din = lambda n, s, d: nc.dram_tensor(n, s, d, kind="ExternalInput").ap()
    x_in = din("x", [tok, D], F32)
    g_in = din("g1", [128, 32], F32); sc_in = din("sc1", [128, 32], F32); sh_in = din("sh1", [128, 32], F32)
    id_in = din("ident", [128, 128], BF16)
    h_out = nc.dram_tensor("hT", [D, tok], BF16, kind="ExternalOutput").ap()
    hv = h_out.rearrange("(k p) t -> p k t", p=128)
    S = Sched(nc)
    with ExitStack() as st:
        sb = lambda n, s, d: st.enter_context(nc.sbuf_tensor(n + "_s", s, d))
        a1 = sb("a1", [128, 32], F32); sh1 = sb("sh1", [128, 32], F32); g1 = sb("g1", [128, 32], F32)
        ident = sb("ident", [128, 128], BF16)
        xb = [sb("xb%d" % i, [128, D], F32) for i in range(2)]
        xs = [sb("xs%d" % i, [128, D], BF16) for i in range(2)]
        ss = sb("ss", [128, ntl], F32)
        hT = [sb("hT%d" % i, [128, 32, 128], BF16) for i in range(2)]
        B = [st.enter_context(nc.psum_tensor("B%d" % i, [128, 512], F32)) for i in range(4)]
        Bb = [b[:].bitcast(BF16) for b in B]
        for (t, src, name) in ((g1, g_in, "g1"), (a1, sc_in, "a1"), (sh1, sh_in, "sh1"), (ident, id_in, "ident")):
            S.dma("sp", t[:], src[:, :], writes=[name])
        S.add("dve", lambda e: e.scalar_tensor_tensor(out=a1[:], in0=a1[:], scalar=1.0, in1=g1[:], op0=ALU.add,
                                                      op1=ALU.mult), reads=["a1", "g1"], writes=["a1"])
        for tt in range(ntl):
            ts = slice(tt * 128, (tt + 1) * 128)
            x_ = xb[tt % 2]; xtok = "xb%d" % (tt % 2); s_ = xs[tt % 2]; stok = "xs%d" % (tt % 2)
            S.dma("sp", x_[:], x_in[ts, :], writes=[xtok])
            S.add("act", lambda e, tt=tt, x_=x_, s_=s_: e.activation(out=s_[:], in_=x_[:], func=AF.Square,
                                                                    accum_out=ss[:, tt:tt + 1]),
                  reads=[xtok], writes=[stok, "ss"])
            S.add("act", lambda e, tt=tt: e.activation(out=ss[:, tt:tt + 1], in_=ss[:, tt:tt + 1], func=AF.Sqrt,
                                                        scale=1.0 / D, bias=EPS), reads=["ss"], writes=["ss"])
            S.add("dve", lambda e, tt=tt: e.reciprocal(out=ss[:, tt:tt + 1], in_=ss[:, tt:tt + 1]),
                  reads=["ss"], writes=["ss"])
            S.add("dve", lambda e, tt=tt, x_=x_, s_=s_: e.tensor_scalar(out=s_[:], in0=x_[:], scalar1=ss[:, tt:tt + 1],
                                                                       scalar2=None, op0=ALU.mult),
                  reads=[xtok, "ss"], writes=[stok])
            hb = hT[tt % 2]
            htok = "hT%d" % (tt % 2)
            for k4 in range(8):
                bk = k4 % 4
                for j in range(4):
                    k = k4 * 4 + j
                    S.add("pe", lambda e, k=k, j=j, bk=bk, s_=s_: e.transpose(Bb[bk][:, j * 128:(j + 1) * 128],
                                                                             s_[:, k * 128:(k + 1) * 128], ident[:]),
                          reads=[stok, "ident"], writes=["B%d" % bk])
                for j in range(4):
                    k = k4 * 4 + j
                    S.add("act", lambda e, k=k, j=j, bk=bk, hb=hb: e.activation(
                        out=hb[:, k, :], in_=Bb[bk][:, j * 128:(j + 1) * 128], func=AF.Identity,
                        scale=a1[:, k:k + 1], bias=sh1[:, k:k + 1]),
                        reads=["B%d" % bk, "a1", "sh1"], writes=[htok])
            S.dma("sp", hv[:, :, ts], hb[:], reads=[htok], writes=["h"])
        _finish(S, st, ["h"])
    return nc


def n1_maps(x2, mod, norm1_gain):
    idb, _, _ = _consts()
    tok = SEQ // NCORES
    return [{"x": np.ascontiguousarray(x2[c * tok:(c + 1) * tok]), "g1": _pk(norm1_gain[0]), "sc1": _pk(mod[D:2 * D]),
             "sh1": _pk(mod[0:D]), "ident": idb} for c in range(NCORES)]


NT = SEQ // 128
FC = 770


def _consts():
    ident = np.eye(128, dtype=np.float32)
    ut = np.triu(np.ones((128, 128), np.float32))
    return ident.astype(ml_dtypes.bfloat16), ut, ut.astype(ml_dtypes.bfloat16)


def build_fox(nt=NT):
    nc = bass.Bass("TRN2", target_bir_lowering=False)
    seq = nt * 128
    h_in = nc.dram_tensor("hT", [D, seq], BF16, kind="ExternalInput").ap()
    hTv = h_in.rearrange("(k p) t -> p k t", p=128)
    w_in = nc.dram_tensor("w", [D, FC], F32, kind="ExternalInput").ap()
    fb_in = nc.dram_tensor("fb", [128, 2], F32, kind="ExternalInput").ap()
    gq_in = nc.dram_tensor("gq", [128, 2], F32, kind="ExternalInput").ap()
    gk_in = nc.dram_tensor("gk", [128, 2], F32, kind="ExternalInput").ap()
    id_in = nc.dram_tensor("ident", [128, 128], BF16, kind="ExternalInput").ap()
    ut_in = nc.dram_tensor("ut", [128, 128], F32, kind="ExternalInput").ap()
    utb_in = nc.dram_tensor("utb", [128, 128], BF16, kind="ExternalInput").ap()
    neg_in = nc.dram_tensor("negm", [128, 128], BF16, kind="ExternalInput").ap()
    idf_in = nc.dram_tensor("identf", [128, 128], F32, kind="ExternalInput").ap()
    out = nc.dram_tensor("mT", [256, seq], BF16, kind="ExternalOutput").ap()
    wv = w_in.rearrange("(k p) n -> k p n", p=128)
    S = Sched(nc)
    with ExitStack() as st:
        sb = lambda n, s, d: st.enter_context(nc.sbuf_tensor(n, s, d))
        wF = sb("wF", [128, 32, FC], BF16)
        wst = [sb("wst%d" % i, [128, FC], F32) for i in range(2)]
        qT = [sb("qT%d" % h, [128, seq], BF16) for h in range(2)]
        kT = [sb("kT%d" % h, [128, seq], BF16) for h in range(2)]
        Vx = [sb("Vx%d" % h, [128, nt, 129], BF16) for h in range(2)]
        fl = sb("fl", [128, 2, nt], F32)
        hT = [sb("hT%d" % i, [128, 32, 128], BF16) for i in range(3)]
        ss = sb("ss", [128, nt], F32)
        sq4 = sb("sq4", [128, 4], F32)
        qn = [sb("qn%d" % i, [128, 128], BF16) for i in range(4)]
        fb = sb("fb_s", [128, 2], F32); gq = sb("gq_s", [128, 2], F32); gk = sb("gk_s", [128, 2], F32)
        ident = sb("ident_s", [128, 128], BF16); ut = sb("ut_s", [128, 128], F32); utb = sb("utb_s", [128, 128], BF16)
        onesf = sb("onesf", [128, 128], F32)
        negm = sb("negm_s", [128, 128], BF16); identf = sb("identf_s", [128, 128], F32)
        cum2 = sb("cum2", [128, 128], F32); off2 = sb("off2", [128, 128], F32)
        ctf = sb("ctf", [128, 128], F32); cthi = sb("cthi", [128, 128], BF16); ctr = sb("ctr", [128, 128], F32)
        ctlo = sb("ctlo", [128, 128], BF16)
        CT2 = [sb("CT2_%d" % h, [128, 128], BF16) for h in range(2)]
        E2 = [sb("E2_%d" % i, [128, 128], BF16) for i in range(2)]
        junk = sb("junk", [128, 128], BF16)
        cum = sb("cum", [128, 2, nt], F32); off = sb("off", [128, 2, nt], F32); tot = sb("tot", [128, 2, nt], F32)
        lf = sb("lf", [128, 2, nt], F32)
        negb2 = [sb("negb%d" % i, [128, nt], F32) for i in range(2)]
        rden2 = [sb("rden%d" % i, [128, 1], F32) for i in range(2)]
        on2 = [sb("on%d" % i, [128, 128], BF16) for i in range(2)]
        pT = [sb("pT%d" % i, [128, 128], BF16) for i in range(3)]
        rden = sb("rden", [128, 1], F32)
        on = sb("on", [128, 128], BF16)
        ost = [sb("ost%d" % i, [128, 128], BF16) for i in range(2)]
        B = [st.enter_context(nc.psum_tensor("B%d" % i, [128, 512], F32)) for i in range(8)]
        Bb = [b[:].bitcast(BF16) for b in B]

        for (t, src, name) in ((fb, fb_in, "fb"),
                               (gq, gq_in, "gq"), (gk, gk_in, "gk"), (ident, id_in, "ident"), (ut, ut_in, "ut"),
                               (utb, utb_in, "utb"), (negm, neg_in, "negm"), (identf, idf_in, "identf")):
            S.dma("sp", t[:], src[:, :], writes=[name])
        S.add("dve", lambda e: e.tensor_scalar(out=fb[:], in0=fb[:], scalar1=-1.0, scalar2=None, op0=ALU.mult),
              reads=["fb"], writes=["fb"])
        S.add("pool", lambda e: e.memset(onesf[:], 1.0), writes=["onesf"])
        for h in range(2):
            S.add("pool", lambda e, h=h: e.memset(Vx[h][:, :, 128:129], 1.0), writes=["Vx%d" % h])
        for k in range(32):
            S.dma("sp", wst[k % 2][:], wv[k], writes=["wst%d" % (k % 2)])
            eng = ("act", "dve", "pool")[k % 3]
            if eng == "act":
                S.add("act", lambda e, k=k: e.activation(out=wF[:, k, :], in_=wst[k % 2][:], func=AF.Copy),
                      reads=["wst%d" % (k % 2)], writes=["wF"])
            else:
                S.add(eng, lambda e, k=k: e.tensor_copy(out=wF[:, k, :], in_=wst[k % 2][:]),
                      reads=["wst%d" % (k % 2)], writes=["wF"])

        for tt in range(nt):
            ts = slice(tt * 128, (tt + 1) * 128)
            hb = hT[tt % 3]
            htok = "hT%d" % (tt % 3)
            S.dma("sp", hb[:], hTv[:, :, ts], writes=[htok])
            for cg, (c0, c1) in enumerate(((0, 512), (512, FC))):
                for k in range(32):
                    S.add("pe", lambda e, k=k, cg=cg, c0=c0, c1=c1, hb=hb: e.matmul(
                        B[2 + cg][:, 0:c1 - c0], lhsT=hb[:, k, :], rhs=wF[:, k, c0:c1], start=(k == 0), stop=(k == 31)),
                        reads=[htok, "wF"], writes=["B%d" % (2 + cg)])
            for i in range(4):
                S.add("act", lambda e, i=i: e.activation(out=junk[:, 0:128], in_=B[2][:, i * 128:(i + 1) * 128],
                                                          func=AF.Square, accum_out=sq4[:, i:i + 1]),
                      reads=["B2"], writes=["junk", "sq4"])
            S.add("act", lambda e: e.activation(out=sq4[:], in_=sq4[:], func=AF.Sqrt, scale=1.0 / HD, bias=EPS),
                  reads=["sq4"], writes=["sq4"])
            S.add("dve", lambda e: e.reciprocal(out=sq4[:], in_=sq4[:]), reads=["sq4"], writes=["sq4"])
            for i in range(4):
                S.add("dve", lambda e, i=i: e.tensor_scalar(out=qn[i][:], in0=B[2][:, i * 128:(i + 1) * 128],
                                                             scalar1=sq4[:, i:i + 1], scalar2=None, op0=ALU.mult),
                      reads=["B2", "sq4"], writes=["qn%d" % i])
                S.add("pe", lambda e, i=i: e.transpose(Bb[4][:, i * 128:(i + 1) * 128], qn[i][:], ident[:]),
                      reads=["qn%d" % i, "ident"], writes=["B4"])
                h = i % 2
                dst, gg, gname = (qT[h], gq, "gq") if i < 2 else (kT[h], gk, "gk")
                S.add("act", lambda e, i=i, dst=dst, gg=gg, h=h, ts=ts: e.activation(
                    out=dst[:, ts], in_=Bb[4][:, i * 128:(i + 1) * 128], func=AF.Copy, scale=gg[:, h:h + 1]),
                    reads=["B4", gname], writes=["qk%d" % i])
            for h in range(2):
                S.add("dve", lambda e, h=h, tt=tt: e.tensor_copy(out=Vx[h][:, tt, 0:128], in_=B[3][:, h * 128:(h + 1) * 128]),
                      reads=["B3"], writes=["Vx%d" % h])
            S.add("dve", lambda e, tt=tt: e.tensor_copy(out=fl[:, :, tt], in_=B[3][:, 256:258]),
                  reads=["B3"], writes=["fl"])

        for h in range(2):
            S.add("act", lambda e, h=h: e.activation(out=lf[:, h, :], in_=fl[:, h, :], func=AF.Exp, scale=-1.0,
                                                      bias=fb[:, h:h + 1]), reads=["fl", "fb"], writes=["lf"])
        S.add("act", lambda e: e.activation(out=lf[:], in_=lf[:], func=AF.Ln, scale=1.0, bias=1.0),
              reads=["lf"], writes=["lf"])
        S.add("dve", lambda e: e.tensor_scalar(out=lf[:], in0=lf[:], scalar1=-1.0, scalar2=None, op0=ALU.mult),
              reads=["lf"], writes=["lf"])
        lf2 = lf[:].rearrange("p h t -> p (h t)")
        n2 = 2 * nt
        S.add("pe", lambda e: e.matmul(B[0][:, 0:n2], lhsT=ut[:], rhs=lf2, start=True, stop=True),
              reads=["ut", "lf"], writes=["B0"])
        S.add("pe", lambda e: e.matmul(B[1][:, 0:n2], lhsT=onesf[:], rhs=lf2, start=True, stop=True),
              reads=["onesf", "lf"], writes=["B1"])
        S.add("dve", lambda e: e.tensor_copy(out=tot[:].rearrange("p h t -> p (h t)"), in_=B[1][:, 0:n2]),
              reads=["B1"], writes=["tot"])
        S.add("pool", lambda e: e.memset(off[:], 0.0), writes=["off"])
        for j in range(1, nt):
            S.add("dve", lambda e, j=j: e.tensor_tensor(out=off[:, :, j], in0=off[:, :, j - 1], in1=tot[:, :, j - 1],
                                                        op=ALU.add), reads=["off", "tot"], writes=["off"])
        S.add("dve", lambda e: e.tensor_tensor(out=cum[:].rearrange("p h t -> p (h t)"), in0=B[0][:, 0:n2],
                                               in1=off[:].rearrange("p h t -> p (h t)"), op=ALU.add),
              reads=["B0", "off"], writes=["cum"])

        scale = float(HD) ** -0.5
        for h in range(2):
            S.add("pool", lambda e: e.memset(cum2[:], 0.0), writes=["cum2"])
            S.add("pool", lambda e: e.memset(off2[:], 0.0), writes=["off2"])
            for r in range(2):
                S.add("dve", lambda e, h=h, r=r: e.tensor_copy(out=cum2[:, r * 64:r * 64 + nt], in_=cum[:, h, :]),
                      reads=["cum"], writes=["cum2"])
                S.add("dve", lambda e, h=h, r=r: e.tensor_copy(out=off2[:, r * 64:r * 64 + nt], in_=off[:, h, :]),
                      reads=["off"], writes=["off2"])
            S.add("pe", lambda e: e.transpose(B[0][:, 0:128], cum2[:], identf[:]), reads=["cum2", "identf"], writes=["B0"])
            S.add("pe", lambda e: e.transpose(B[1][:, 0:128], off2[:], identf[:]), reads=["off2", "identf"], writes=["B1"])
            S.add("dve", lambda e: e.tensor_copy(out=ctr[:], in_=B[1][:, 0:128]), reads=["B1"], writes=["ctr"])
            S.add("dve", lambda e: e.tensor_scalar(out=ctf[:], in0=B[0][:, 0:128], scalar1=ctr[:, 0:1], scalar2=1.0 / scale,
                                                   op0=ALU.subtract, op1=ALU.mult), reads=["B0", "ctr"], writes=["ctf"])
            S.add("dve", lambda e: e.tensor_copy(out=cthi[:], in_=ctf[:]), reads=["ctf"], writes=["cthi"])
            S.add("dve", lambda e: e.tensor_tensor(out=ctr[:], in0=ctf[:], in1=cthi[:], op=ALU.subtract),
                  reads=["ctf", "cthi"], writes=["ctr"])
            S.add("dve", lambda e: e.tensor_copy(out=ctlo[:], in_=ctr[:]), reads=["ctr"], writes=["ctlo"])
            S.add("dve", lambda e, h=h: e.tensor_copy(out=CT2[h][0:64, :], in_=cthi[0:64, :]), reads=["cthi"],
                  writes=["CT2_%d" % h])
            S.add("dve", lambda e, h=h: e.tensor_copy(out=CT2[h][64:128, :], in_=ctlo[64:128, :]), reads=["ctlo"],
                  writes=["CT2_%d" % h])
        for h in range(2):
            pairs = [(qb, kb) for qb in range(nt) for kb in range(qb + 1)]

            def emit_s(i, h=h):
                qb, kb = pairs[i]
                qs_ = slice(qb * 128, (qb + 1) * 128)
                sbk = 4 + (i % 2)
                e2 = E2[qb % 2]; e2tok = "E2_%d" % (qb % 2)
                if kb == 0:
                    S.add("dve", lambda e, qb=qb, e2=e2: e.tensor_tensor(
                        out=e2[:], in0=ident[:, qb:qb + 1].to_broadcast([128, 128]),
                        in1=ident[:, 64 + qb:65 + qb].to_broadcast([128, 128]), op=ALU.add),
                        reads=["ident"], writes=[e2tok])
                S.add("pe", lambda e: e.matmul(B[sbk][:, 0:128], lhsT=kT[h][:, kb * 128:(kb + 1) * 128], rhs=qT[h][:, qs_],
                                               start=True, stop=False),
                      reads=["qk%d" % h, "qk%d" % (2 + h)], writes=["B%d" % sbk])
                S.add("pe", lambda e: e.matmul(B[sbk][:, 0:128], lhsT=e2[:], rhs=CT2[h][:], start=False, stop=(kb != qb)),
                      reads=[e2tok, "CT2_%d" % h], writes=["B%d" % sbk])
                if kb == qb:
                    S.add("pe", lambda e: e.matmul(B[sbk][:, 0:128], lhsT=ident[:], rhs=negm[:], start=False, stop=True),
                          reads=["ident", "negm"], writes=["B%d" % sbk])

            def emit_rest(i, h=h):
                qb, kb = pairs[i]
                qs_ = slice(qb * 128, (qb + 1) * 128)
                sbk = 4 + (i % 2)
                pt = pT[i % 3]; ptok = "pT%d" % (i % 3)
                ab = 6 + (qb % 2)
                nb_ = negb2[qb % 2]; nbtok = "negb%d" % (qb % 2)
                if kb == 0:
                    S.add("dve", lambda e: e.tensor_scalar(
                        out=nb_[:, 0:qb + 1], in0=cum[:, h, 0:qb + 1], scalar1=-1.0, scalar2=off[:, h, qb:qb + 1],
                        op0=ALU.mult, op1=ALU.add), reads=["cum", "off"], writes=[nbtok])
                S.add("act", lambda e: e.activation(out=pt[:], in_=B[sbk][:, 0:128], func=AF.Exp, scale=scale,
                                                    bias=nb_[:, kb:kb + 1]),
                      reads=["B%d" % sbk, nbtok], writes=[ptok])
                S.add("pe", lambda e: e.matmul(B[ab][:, 0:129], lhsT=pt[:], rhs=Vx[h][:, kb, :], start=(kb == 0),
                                               stop=(kb == qb)), reads=[ptok, "Vx%d" % h], writes=["B%d" % ab])
                if kb == qb:
                    rd = rden2[qb % 2]; rdtok = "rden%d" % (qb % 2)
                    onb = on2[qb % 2]; ontok = "on%d" % (qb % 2)
                    S.add("dve", lambda e: e.reciprocal(out=rd[:], in_=B[ab][:, 128:129]), reads=["B%d" % ab], writes=[rdtok])
                    S.add("dve", lambda e: e.tensor_scalar(out=onb[:], in0=B[ab][:, 0:128], scalar1=rd[:, 0:1],
                                                           scalar2=None, op0=ALU.mult),
                          reads=["B%d" % ab, rdtok], writes=[ontok])
                    tb = qb % 2
                    S.add("pe", lambda e: e.transpose(Bb[tb][:, 0:128], onb[:], ident[:]), reads=[ontok, "ident"],
                          writes=["B%d" % tb])
                    ob = ost[qb % 2]
                    S.add("act", lambda e: e.activation(out=ob[:], in_=Bb[tb][:, 0:128], func=AF.Copy),
                          reads=["B%d" % tb], writes=["ost%d" % (qb % 2)])
                    S.dma("sp", out[h * 128:(h + 1) * 128, qs_], ob[:], reads=["ost%d" % (qb % 2)], writes=["out"])

            emit_s(0)
            for i in range(len(pairs)):
                if i + 1 < len(pairs):
                    emit_s(i + 1)
                emit_rest(i)
        _finish(S, st, ["out"])
    return nc


def _pk(v):
    return np.ascontiguousarray(np.asarray(v, np.float32).reshape(32, 128).T)


def fox_maps(hT, w_in, fox_f_bias, fox_q_gain, fox_k_gain):
    idb, ut, utb = _consts()
    maps = []
    for c in range(NCORES):
        hs = [2 * c, 2 * c + 1]
        cols = np.concatenate([np.arange(256 * c, 256 * c + 256), 2048 + np.arange(256 * c, 256 * c + 256),
                               4096 + np.arange(256 * c, 256 * c + 256), 6144 + np.array(hs)])
        maps.append({
            "hT": hT,
            "w": np.ascontiguousarray(w_in[0][:, cols]),
            "fb": np.ascontiguousarray(np.broadcast_to(fox_f_bias[0][hs][None, :], (128, 2))).astype(np.float32),
            "gq": np.ascontiguousarray(fox_q_gain[0][hs].T), "gk": np.ascontiguousarray(fox_k_gain[0][hs].T),
            "ident": idb, "ut": ut, "utb": utb, "identf": np.eye(128, dtype=np.float32),
            "negm": (np.tril(np.ones((128, 128), np.float32), -1) * -30000.0).astype(ml_dtypes.bfloat16),
        })
    return maps


HC = 1024


def build_hgrn(nt=NT):
    nc = bass.Bass("TRN2", target_bir_lowering=False)
    seq = nt * 128
    din = lambda n, s, d: nc.dram_tensor(n, s, d, kind="ExternalInput").ap()
    h_in = din("hT", [D, seq], BF16)
    hTv = h_in.rearrange("(k p) t -> p k t", p=128)
    w_in = din("w", [D, HC], F32)
    lb0_in = din("lb0", [128, 256], F32); lb1_in = din("lb1", [128, 256], F32); og_in = din("og", [128, 256], F32)
    id_in = din("ident", [128, 128], BF16)
    tri_in = din("tri", [128, 128], BF16); stri_in = din("stri", [128, 128], BF16); m_in = din("m128", [128, 128], F32)
    c01_in = din("c01", [128, 2], F32)
    out = nc.dram_tensor("mT", [256, seq], BF16, kind="ExternalOutput").ap()
    wv = w_in.rearrange("(k p) n -> k p n", p=128)
    S = Sched(nc)
    with ExitStack() as st:
        sb = lambda n, s, d: st.enter_context(nc.sbuf_tensor(n + "_s", s, d))
        wH = sb("wH", [128, 32, HC], BF16)
        wst = [sb("wst%d" % i, [128, HC], F32) for i in range(2)]
        hT = [sb("hT%d" % i, [128, 32, 128], BF16) for i in range(3)]
        ss = sb("ss", [128, nt], F32)
        lbb = sb("lbb", [128, 256], F32); omlb = sb("omlb", [128, 256], F32); ogb = sb("ogb", [128, 256], F32)
        ident = sb("ident", [128, 128], BF16)
        tri = sb("tri", [128, 128], BF16); stri = sb("stri", [128, 128], BF16); m128 = sb("m128", [128, 128], F32)
        c01 = sb("c01", [128, 2], F32)
        junk = sb("junk", [128, D], BF16)
        sg = sb("sg", [128, 256], F32); gl = sb("gl", [128, 256], F32)
        ghi = sb("ghi", [128, 256], BF16); glo = sb("glo", [128, 256], BF16); gr = sb("gr", [128, 256], F32)
        omfb = sb("omfb", [128, 256], BF16); qsb = sb("qsb", [128, 256], BF16)
        gs = sb("gs", [128, 256], F32); vHb = sb("vHb", [128, 256], BF16)
        ebs = [sb("eb%d" % h, [128, 128], F32) for h in range(2)]
        enbs = [sb("enb%d" % h, [128, 128], F32) for h in range(2)]
        ers = [sb("er%d" % h, [128, 128], F32) for h in range(2)]
        QtTs = [sb("QtT%d" % h, [128, 128], BF16) for h in range(2)]
        Qt0s = [sb("Qt0%d" % h, [128, 128], BF16) for h in range(2)]
        Qt1s = [sb("Qt1%d" % h, [128, 128], BF16) for h in range(2)]
        KtTs = [sb("KtT%d" % h, [128, 128], BF16) for h in range(2)]
        Khs = [sb("Kh%d" % h, [128, 128], BF16) for h in range(2)]
        Kh0s = [sb("Kh0%d" % h, [128, 128], BF16) for h in range(2)]
        Kh1s = [sb("Kh1%d" % h, [128, 128], BF16) for h in range(2)]
        scms = [sb("scm%d" % h, [128, 128], BF16) for h in range(2)]
        junks = [sb("junkh%d" % h, [128, 128], BF16) for h in range(2)]
        Sst = [sb("S%d" % h, [128, 128], F32) for h in range(2)]
        Sbf = [sb("Sbf%d" % h, [128, 128], BF16) for h in range(2)]
        sos = [sb("so%d" % h, [128, 1], F32) for h in range(2)]
        on1s = [sb("on1%d" % h, [128, 128], F32) for h in range(2)]
        on3s = [sb("on3%d" % h, [128, 128], BF16) for h in range(2)]
        ost = [sb("ost%d" % i, [128, 128], BF16) for i in range(4)]
        B = [st.enter_context(nc.psum_tensor("B%d" % i, [128, 512], F32)) for i in range(8)]
        Bb = [b[:].bitcast(BF16) for b in B]

        for (t, src, name) in ((lbb, lb0_in, "lbb"),
                               (omlb, lb1_in, "omlb"), (ogb, og_in, "ogb"), (ident, id_in, "ident"),
                               (tri, tri_in, "tri"), (stri, stri_in, "stri"), (m128, m_in, "m128"), (c01, c01_in, "c01")):
            S.dma("sp", t[:], src[:, :], writes=[name])
        S.add("dve", lambda e: e.tensor_tensor(out=lbb[:], in0=lbb[:], in1=omlb[:], op=ALU.subtract),
              reads=["lbb", "omlb"], writes=["lbb"])
        S.add("act", lambda e: e.activation(out=lbb[:], in_=lbb[:], func=AF.Sigmoid), reads=["lbb"], writes=["lbb"])
        S.add("dve", lambda e: e.tensor_scalar(out=omlb[:], in0=lbb[:], scalar1=-1.0, scalar2=1.0, op0=ALU.mult,
                                               op1=ALU.add), reads=["lbb"], writes=["omlb"])
        for h in range(2):
            S.add("pool", lambda e, h=h: e.memset(Sst[h][:], 0.0), writes=["S%d" % h])
            S.add("pool", lambda e, h=h: e.memset(Sbf[h][:], 0.0), writes=["Sbf%d" % h])
        for h in range(2):
            S.add("pool", lambda e, h=h: e.memset(Qt0s[h][:], 0.0), writes=["Qt0_h%d" % h])
            S.add("pool", lambda e, h=h: e.memset(Qt1s[h][:], 0.0), writes=["Qt1_h%d" % h])
        for k in range(32):
            S.dma("sp", wst[k % 2][:], wv[k], writes=["wst%d" % (k % 2)])
            eng = ("act", "dve", "pool")[k % 3]
            if eng == "act":
                S.add("act", lambda e, k=k: e.activation(out=wH[:, k, :], in_=wst[k % 2][:], func=AF.Copy),
                      reads=["wst%d" % (k % 2)], writes=["wH"])
            else:
                S.add(eng, lambda e, k=k: e.tensor_copy(out=wH[:, k, :], in_=wst[k % 2][:]),
                      reads=["wst%d" % (k % 2)], writes=["wH"])

        oc = 0
        for tt in range(nt):
            ts = slice(tt * 128, (tt + 1) * 128)
            hb = hT[tt % 3]
            htok = "hT%d" % (tt % 3)
            S.dma("sp", hb[:], hTv[:, :, ts], writes=[htok])
            for cg in range(2):
                for k in range(32):
                    S.add("pe", lambda e, k=k, cg=cg, hb=hb: e.matmul(
                        B[2 + cg][:, :], lhsT=hb[:, k, :], rhs=wH[:, k, cg * 512:(cg + 1) * 512],
                        start=(k == 0), stop=(k == 31)), reads=[htok, "wH"], writes=["B%d" % (2 + cg)])
            S.add("act", lambda e: e.activation(out=sg[:], in_=B[2][:, 256:512], func=AF.Sigmoid),
                  reads=["B2"], writes=["sg"])
            S.add("dve", lambda e: e.tensor_tensor(out=sg[:], in0=sg[:], in1=omlb[:], op=ALU.mult),
                  reads=["sg", "omlb"], writes=["sg"])
            S.add("dve", lambda e: e.tensor_tensor(out=sg[:], in0=sg[:], in1=lbb[:], op=ALU.add),
                  reads=["sg", "lbb"], writes=["sg"])
            S.add("act", lambda e: e.activation(out=gl[:], in_=sg[:], func=AF.Ln), reads=["sg"], writes=["gl"])
            S.add("dve", lambda e: e.tensor_copy(out=ghi[:], in_=gl[:]), reads=["gl"], writes=["ghi"])
            S.add("dve", lambda e: e.tensor_tensor(out=gr[:], in0=gl[:], in1=ghi[:], op=ALU.subtract),
                  reads=["gl", "ghi"], writes=["gr"])
            S.add("dve", lambda e: e.tensor_copy(out=glo[:], in_=gr[:]), reads=["gr"], writes=["glo"])
            S.add("dve", lambda e: e.tensor_scalar(out=omfb[:], in0=sg[:], scalar1=-1.0, scalar2=1.0, op0=ALU.mult,
                                                   op1=ALU.add), reads=["sg"], writes=["omfb"])
            S.add("act", lambda e: e.activation(out=qsb[:], in_=B[2][:, 0:256], func=AF.Silu),
                  reads=["B2"], writes=["qsb"])
            S.add("act", lambda e: e.activation(out=gs[:], in_=B[3][:, 256:512], func=AF.Silu),
                  reads=["B3"], writes=["gs"])
            S.add("act", lambda e: e.activation(out=vHb[:], in_=B[3][:, 0:256], func=AF.Copy),
                  reads=["B3"], writes=["vHb"])
            def head_ops(h, tt=tt, ts=ts):
                hc = slice(h * 128, (h + 1) * 128)
                X, Y = 4 + h, 6 + h
                BX, BY, BXb, BYb = B[X], B[Y], Bb[X], Bb[Y]
                tX, tY = "B%d" % X, "B%d" % Y
                tk = lambda n: "%s_h%d" % (n, h)
                eb, enb, er = ebs[h], enbs[h], ers[h]
                QtT, Qt0, Qt1, KtT = QtTs[h], Qt0s[h], Qt1s[h], KtTs[h]
                Kh, Kh0, Kh1, scm = Khs[h], Kh0s[h], Kh1s[h], scms[h]
                so, on1, on3 = sos[h], on1s[h], on3s[h]
                S.add("pe", lambda e: e.matmul(BX[:, 0:128], lhsT=ghi[:, hc], rhs=tri[:], start=True, stop=False),
                      reads=["ghi", "tri"], writes=[tX]); yield
                S.add("pe", lambda e: e.matmul(BX[:, 0:128], lhsT=glo[:, hc], rhs=tri[:], start=False, stop=True),
                      reads=["glo", "tri"], writes=[tX]); yield
                S.add("pe", lambda e: e.matmul(BX[:, 128:256], lhsT=stri[:], rhs=ghi[:, hc], start=True, stop=False),
                      reads=["ghi", "stri"], writes=[tX]); yield
                S.add("pe", lambda e: e.matmul(BX[:, 128:256], lhsT=stri[:], rhs=glo[:, hc], start=False, stop=True),
                      reads=["glo", "stri"], writes=[tX]); yield
                S.add("act", lambda e: e.activation(out=eb[:], in_=BX[:, 0:128], func=AF.Exp), reads=[tX],
                      writes=[tk("eb")]); yield
                S.add("act", lambda e: e.activation(out=enb[:], in_=BX[:, 0:128], func=AF.Exp, scale=-1.0),
                      reads=[tX], writes=[tk("enb")]); yield
                S.add("act", lambda e: e.activation(out=er[:], in_=BX[:, 128:256], func=AF.Exp), reads=[tX],
                      writes=[tk("er")]); yield
                S.add("pe", lambda e: e.transpose(BXb[:, 512:640], qsb[:, hc], ident[:]), reads=["qsb", "ident"],
                      writes=[tX]); yield
                S.add("pe", lambda e: e.transpose(BXb[:, 640:768], omfb[:, hc], ident[:]), reads=["omfb", "ident"],
                      writes=[tX]); yield
                S.add("dve", lambda e: e.tensor_tensor(out=QtT[:], in0=BXb[:, 512:640], in1=eb[:], op=ALU.mult),
                      reads=[tX, tk("eb")], writes=[tk("QtT")]); yield
                S.add("pool", lambda e: e.tensor_copy(out=Qt0[:, 0:64], in_=QtT[:, 0:64]), reads=[tk("QtT")],
                      writes=[tk("Qt0")]); yield
                S.add("pool", lambda e: e.tensor_copy(out=Qt1[:, 64:128], in_=QtT[:, 64:128]), reads=[tk("QtT")],
                      writes=[tk("Qt1")]); yield
                S.add("dve", lambda e: e.tensor_tensor(out=KtT[:], in0=BXb[:, 640:768], in1=enb[:], op=ALU.mult),
                      reads=[tX, tk("enb")], writes=[tk("KtT")]); yield
                S.add("dve", lambda e: e.tensor_tensor(out=Kh[:], in0=omfb[:, hc], in1=er[:], op=ALU.mult),
                      reads=["omfb", tk("er")], writes=[tk("Kh")]); yield
                S.add("pool", lambda e: e.tensor_scalar(out=Kh0[:], in0=Kh[:], scalar1=c01[:, 0:1], scalar2=None,
                                                        op0=ALU.mult), reads=[tk("Kh"), "c01"], writes=[tk("Kh0")]); yield
                S.add("pool", lambda e: e.tensor_scalar(out=Kh1[:], in0=Kh[:], scalar1=c01[:, 1:2], scalar2=None,
                                                        op0=ALU.mult), reads=[tk("Kh"), "c01"], writes=[tk("Kh1")]); yield
                S.add("pe", lambda e: e.matmul(BY[:, 0:128], lhsT=KtT[:], rhs=QtT[:], start=True, stop=True),
                      reads=[tk("KtT"), tk("QtT")], writes=[tY]); yield
                S.add("dve", lambda e: e.tensor_tensor(out=scm[:], in0=BY[:, 0:128], in1=m128[:], op=ALU.mult),
                      reads=[tY, "m128"], writes=[tk("scm")]); yield
                S.add("pe", lambda e: e.matmul(BY[:, 128:256], lhsT=scm[:], rhs=vHb[:, hc], start=True, stop=False),
                      reads=[tk("scm"), "vHb"], writes=[tY]); yield
                S.add("pe", lambda e: e.matmul(BY[:, 128:256], lhsT=Qt0[:], rhs=Sbf[h][:], start=False, stop=False),
                      reads=[tk("Qt0"), "Sbf%d" % h], writes=[tY]); yield
                sr = slice(384, 512)
                S.add("pe", lambda e: e.matmul(BX[:, sr], lhsT=Kh0[:], rhs=vHb[:, hc], start=True, stop=True),
                      reads=[tk("Kh0"), "vHb"], writes=[tX]); yield
                S.add("dve", lambda e: e.scalar_tensor_tensor(out=Sst[h][:], in0=Sst[h][:], scalar=eb[:, 63:64],
                                                              in1=BX[:, sr], op0=ALU.mult, op1=ALU.add),
                      reads=["S%d" % h, tk("eb"), tX], writes=["S%d" % h]); yield
                S.add("act", lambda e: e.activation(out=Sbf[h][:], in_=Sst[h][:], func=AF.Copy),
                      reads=["S%d" % h], writes=["Sbf%d" % h]); yield
                S.add("pe", lambda e: e.matmul(BY[:, 128:256], lhsT=Qt1[:], rhs=Sbf[h][:], start=False, stop=True),
                      reads=[tk("Qt1"), "Sbf%d" % h], writes=[tY]); yield
                S.add("pe", lambda e: e.matmul(BX[:, sr], lhsT=Kh1[:], rhs=vHb[:, hc], start=True, stop=True),
                      reads=[tk("Kh1"), "vHb"], writes=[tX]); yield
                S.add("dve", lambda e: e.scalar_tensor_tensor(out=Sst[h][:], in0=Sst[h][:], scalar=eb[:, 127:128],
                                                              in1=BX[:, sr], op0=ALU.mult, op1=ALU.add),
                      reads=["S%d" % h, tk("eb"), tX], writes=["S%d" % h]); yield
                S.add("act", lambda e: e.activation(out=Sbf[h][:], in_=Sst[h][:], func=AF.Copy),
                      reads=["S%d" % h], writes=["Sbf%d" % h]); yield
                S.add("act", lambda e: e.activation(out=junks[h][:], in_=BY[:, 128:256], func=AF.Square,
                                                    accum_out=so[:, 0:1]), reads=[tY], writes=[tk("junk"), tk("so")]); yield
                S.add("act", lambda e: e.activation(out=so[:], in_=so[:], func=AF.Sqrt, scale=1.0 / HD, bias=EPS * HD),
                      reads=[tk("so")], writes=[tk("so")]); yield
                S.add("dve", lambda e: e.reciprocal(out=so[:], in_=so[:]), reads=[tk("so")], writes=[tk("so")]); yield
                S.add("dve", lambda e: e.scalar_tensor_tensor(out=on1[:], in0=BY[:, 128:256], scalar=so[:, 0:1],
                                                              in1=ogb[:, hc], op0=ALU.mult, op1=ALU.mult),
                      reads=[tY, tk("so"), "ogb"], writes=[tk("on1")]); yield
                S.add("pool", lambda e: e.tensor_tensor(out=on3[:], in0=on1[:], in1=gs[:, hc], op=ALU.mult),
                      reads=[tk("on1"), "gs"], writes=[tk("on3")]); yield
                S.add("pe", lambda e: e.transpose(BYb[:, 768:896], on3[:], ident[:]), reads=[tk("on3"), "ident"],
                      writes=[tY]); yield
                ob = ost[(2 * tt + h) % 4]
                otok = "ost%d" % ((2 * tt + h) % 4)
                S.add("act", lambda e: e.activation(out=ob[:], in_=BYb[:, 768:896], func=AF.Copy), reads=[tY],
                      writes=[otok]); yield
                S.dma("sp", out[h * 128:(h + 1) * 128, ts], ob[:], reads=[otok], writes=["out"]); yield

            gens = [head_ops(0), head_ops(1)]
            while gens:
                for g in list(gens):
                    try:
                        next(g)
                    except StopIteration:
                        gens.remove(g)
        _finish(S, st, ["out"])
    return nc


def hgrn_maps(hT, w_in, hgrn_lower_bounds, hgrn_out_gain):
    idb, _, _ = _consts()
    t64 = np.triu(np.ones((64, 64), np.float32))
    s64 = np.tril(np.ones((64, 64), np.float32), -1)
    z = np.zeros((64, 64), np.float32)
    tri = np.block([[t64, z], [z, t64]]); stri = np.block([[s64, z], [z, s64]])
    c01 = np.zeros((128, 2), np.float32); c01[:64, 0] = 1.0; c01[64:, 1] = 1.0
    maps = []
    o4 = 3 * 2048 + 16
    for c in range(NCORES):
        cr = np.arange(256 * c, 256 * c + 256)
        cols = np.concatenate([o4 + cr, o4 + 2048 + cr, o4 + 4096 + cr, o4 + 6144 + cr])
        bc = lambda v: np.ascontiguousarray(np.broadcast_to(np.asarray(v, np.float32)[None, :], (128, 256)))
        maps.append({
            "hT": hT,
            "w": np.ascontiguousarray(w_in[0][:, cols]),
            "lb0": bc(hgrn_lower_bounds[0][cr]), "lb1": bc(hgrn_lower_bounds[1][cr]),
            "og": bc(hgrn_out_gain[0].reshape(-1)[cr]),
            "ident": idb, "tri": tri.astype(ml_dtypes.bfloat16), "stri": stri.astype(ml_dtypes.bfloat16),
            "m128": tri, "c01": c01,
        })
    return maps


TOK = SEQ // NCORES


def build_c1(ntl=TOK // 128):
    nc = bass.Bass("TRN2", target_bir_lowering=False)
    tok = ntl * 128
    din = lambda n, s, d: nc.dram_tensor(n, s, d, kind="ExternalInput").ap()
    x_in = din("x", [tok, D], F32)
    m_in = din("mT", [D, tok], BF16)
    w_in = din("w", [D, D], F32)
    gt_in = din("gate1", [128, D], F32)
    g2_in = din("g2", [128, 32], F32); sc_in = din("sc2", [128, 32], F32); sh_in = din("sh2", [128, 32], F32)
    id_in = din("ident", [128, 128], BF16)
    x1_out = nc.dram_tensor("x1", [tok, D], F32, kind="ExternalOutput").ap()
    h2_out = nc.dram_tensor("h2T", [D, tok], BF16, kind="ExternalOutput").ap()
    wv = w_in.rearrange("(k p) n -> k p n", p=128)
    mv = m_in.rearrange("(k p) t -> p k t", p=128)
    h2v = h2_out.rearrange("(k p) t -> p k t", p=128)
    S = Sched(nc)
    with ExitStack() as st:
        sb = lambda n, s, d: st.enter_context(nc.sbuf_tensor(n + "_s", s, d))
        wob = sb("wob", [128, 32, 512], BF16)
        wst = [sb("wst%d" % i, [128, 512], F32) for i in range(3)]
        mt = [sb("mt%d" % i, [128, 32, 128], BF16) for i in range(2)]
        xc = [sb("xc%d" % i, [128, 512], F32) for i in range(2)]
        oc_ = [sb("oc%d" % i, [128, 512], F32) for i in range(2)]
        gt = sb("gt", [128, D], F32)
        a2 = sb("a2", [128, 32], F32); sh2 = sb("sh2", [128, 32], F32); g2 = sb("g2", [128, 32], F32)
        ident = sb("ident", [128, 128], BF16)
        xb = sb("xb", [128, D], F32); xs = sb("xs", [128, D], BF16); junk = sb("junk", [128, D], BF16)
        ss = sb("ss", [128, ntl], F32)
        hT = [sb("hT%d" % i, [128, 32, 128], BF16) for i in range(2)]
        B = [st.enter_context(nc.psum_tensor("B%d" % i, [128, 512], F32)) for i in range(8)]
        Bb = [b[:].bitcast(BF16) for b in B]
        for (t, src, name) in ((g2, g2_in, "g2"), (a2, sc_in, "a2"), (sh2, sh_in, "sh2"), (ident, id_in, "ident"),
                               (gt, gt_in, "gt")):
            S.dma("sp", t[:], src[:, :], writes=[name])
        S.add("dve", lambda e: e.scalar_tensor_tensor(out=a2[:], in0=a2[:], scalar=1.0, in1=g2[:], op0=ALU.add,
                                                      op1=ALU.mult), reads=["a2", "g2"], writes=["a2"])
        n = 0
        for cg in range(8):
            cs = slice(cg * 512, (cg + 1) * 512)
            for k in range(32):
                S.dma("sp", wst[k % 3][:], wv[k][:, cs], writes=["wst%d" % (k % 3)])
                eng = ("act", "dve", "pool")[k % 3]
                if eng == "act":
                    S.add("act", lambda e, k=k: e.activation(out=wob[:, k, :], in_=wst[k % 3][:], func=AF.Copy),
                          reads=["wst%d" % (k % 3)], writes=["wob"])
                else:
                    S.add(eng, lambda e, k=k: e.tensor_copy(out=wob[:, k, :], in_=wst[k % 3][:]),
                          reads=["wst%d" % (k % 3)], writes=["wob"])
            for tt in range(ntl):
                ts = slice(tt * 128, (tt + 1) * 128)
                mb = mt[n % 2]; mtok = "mt%d" % (n % 2)
                xcb = xc[n % 2]; xtok = "xc%d" % (n % 2)
                ob = oc_[n % 2]; otok = "oc%d" % (n % 2)
                bk = 2 + (n % 2)
                n += 1
                S.dma("sp", mb[:], mv[:, :, ts], writes=[mtok])
                S.dma("sp", xcb[:], x_in[ts, cs], writes=[xtok])
                for k in range(32):
                    S.add("pe", lambda e, k=k, mb=mb, bk=bk: e.matmul(B[bk][:, :], lhsT=mb[:, k, :], rhs=wob[:, k, :],
                                                                     start=(k == 0), stop=(k == 31)),
                          reads=[mtok, "wob"], writes=["B%d" % bk])
                S.add("dve", lambda e, ob=ob, bk=bk, cs=cs: e.tensor_tensor(out=ob[:], in0=B[bk][:, :], in1=gt[:, cs],
                                                                           op=ALU.mult),
                      reads=["B%d" % bk, "gt"], writes=[otok])
                S.add("pool", lambda e, ob=ob, xcb=xcb: e.tensor_tensor(out=ob[:], in0=ob[:], in1=xcb[:], op=ALU.add),
                      reads=[otok, xtok], writes=[otok])
                S.dma("sp", x1_out[ts, cs], ob[:], reads=[otok], writes=["x1"])
        for tt in range(ntl):
            ts = slice(tt * 128, (tt + 1) * 128)
            S.dma("sp", xb[:], x1_out[ts, :], reads=["x1"], writes=["xb"])
            S.add("act", lambda e, tt=tt: e.activation(out=junk[:], in_=xb[:], func=AF.Square,
                                                        accum_out=ss[:, tt:tt + 1]), reads=["xb"], writes=["junk", "ss"])
            S.add("act", lambda e, tt=tt: e.activation(out=ss[:, tt:tt + 1], in_=ss[:, tt:tt + 1], func=AF.Sqrt,
                                                        scale=1.0 / D, bias=EPS), reads=["ss"], writes=["ss"])
            S.add("dve", lambda e, tt=tt: e.reciprocal(out=ss[:, tt:tt + 1], in_=ss[:, tt:tt + 1]),
                  reads=["ss"], writes=["ss"])
            S.add("dve", lambda e, tt=tt: e.tensor_scalar(out=xs[:], in0=xb[:], scalar1=ss[:, tt:tt + 1], scalar2=None,
                                                           op0=ALU.mult), reads=["xb", "ss"], writes=["xs"])
            hb = hT[tt % 2]
            htok = "hT%d" % (tt % 2)
            for k4 in range(8):
                bk = k4 % 2
                for j in range(4):
                    k = k4 * 4 + j
                    S.add("pe", lambda e, k=k, j=j, bk=bk: e.transpose(Bb[bk][:, j * 128:(j + 1) * 128],
                                                                       xs[:, k * 128:(k + 1) * 128], ident[:]),
                          reads=["xs", "ident"], writes=["B%d" % bk])
                for j in range(4):
                    k = k4 * 4 + j
                    S.add("act", lambda e, k=k, j=j, bk=bk, hb=hb: e.activation(
                        out=hb[:, k, :], in_=Bb[bk][:, j * 128:(j + 1) * 128], func=AF.Identity,
                        scale=a2[:, k:k + 1], bias=sh2[:, k:k + 1]),
                        reads=["B%d" % bk, "a2", "sh2"], writes=[htok])
            S.dma("sp", h2v[:, :, ts], hb[:], reads=[htok], writes=["h2"])
        _finish(S, st, ["x1", "h2"])
    return nc


def c1_maps(x2, mergedT, mod, norm2_gain, w_out):
    idb, _, _ = _consts()
    maps = []
    gate1 = np.ascontiguousarray(np.broadcast_to(mod[2 * D:3 * D][None, :], (128, D))).astype(np.float32)
    wo = np.ascontiguousarray(w_out[0])
    for c in range(NCORES):
        ts = slice(c * TOK, (c + 1) * TOK)
        maps.append({"x": np.ascontiguousarray(x2[ts]), "mT": np.ascontiguousarray(mergedT[:, ts]), "w": wo,
                     "gate1": gate1, "g2": _pk(norm2_gain[0]), "sc2": _pk(mod[4 * D:5 * D]), "sh2": _pk(mod[3 * D:4 * D]),
                     "ident": idb})
    return maps


def build_p0():
    nc = bass.Bass("TRN2", target_bir_lowering=False)
    din = lambda n, s, d: nc.dram_tensor(n, s, d, kind="ExternalInput").ap()
    ut_in = din("UT", [D, 2048], F32); v_in = din("V", [2048, D], F32); wq_in = din("wq", [512, 2048], F32)
    ut_o = nc.dram_tensor("UTb", [D, 2048], BF16, kind="ExternalOutput").ap()
    v_o = nc.dram_tensor("Vb", [2048, D], BF16, kind="ExternalOutput").ap()
    wq_o = nc.dram_tensor("wqb", [512, 2048], BF16, kind="ExternalOutput").ap()
    S = Sched(nc)
    with ExitStack() as st:
        sb = lambda n, s, d: st.enter_context(nc.sbuf_tensor(n + "_s", s, d))
        fi = [sb("fi%d" % i, [128, 4096], F32) for i in range(3)]
        bo = [sb("bo%d" % i, [128, 4096], BF16) for i in range(3)]
        jobs = []
        for r in range(0, D, 256):
            jobs.append((ut_in[r:r + 256, :].rearrange("(a p) n -> p a n", p=128),
                         ut_o[r:r + 256, :].rearrange("(a p) n -> p a n", p=128), True))
        for r in range(0, 2048, 128):
            jobs.append((v_in[r:r + 128, :], v_o[r:r + 128, :], False))
        for r in range(0, 512, 256):
            jobs.append((wq_in[r:r + 256, :].rearrange("(a p) n -> p a n", p=128),
                         wq_o[r:r + 256, :].rearrange("(a p) n -> p a n", p=128), True))
        for i, (src, dst, two) in enumerate(jobs):
            f = fi[i % 3]; b = bo[i % 3]
            fv = f[:].rearrange("p (a n) -> p a n", a=2) if two else f[:]
            bv = b[:].rearrange("p (a n) -> p a n", a=2) if two else b[:]
            S.dma("sp", fv, src, writes=["fi%d" % (i % 3)])
            eng = ("act", "dve", "pool")[i % 3]
            if eng == "act":
                S.add("act", lambda e, f=f, b=b: e.activation(out=b[:], in_=f[:], func=AF.Copy),
                      reads=["fi%d" % (i % 3)], writes=["bo%d" % (i % 3)])
            else:
                S.add(eng, lambda e, f=f, b=b: e.tensor_copy(out=b[:], in_=f[:]),
                      reads=["fi%d" % (i % 3)], writes=["bo%d" % (i % 3)])
            S.dma("sp", dst, bv, reads=["bo%d" % (i % 3)], writes=["out"])
        _finish(S, st, ["out"])
    return nc


def p0_maps(peer_w_query, peer_u, peer_v):
    UT = peer_u[0].T
    maps = []
    for c in range(NCORES):
        es = slice(c * 2048, (c + 1) * 2048)
        maps.append({"UT": np.ascontiguousarray(UT[:, es]), "V": np.ascontiguousarray(peer_v[0][es]),
                     "wq": np.ascontiguousarray(peer_w_query[0][c * 512:(c + 1) * 512])})
    return maps


NEXP_B = 128


def build_c2(nq=4, tq=2, nb=NEXP_B, G=2):
    nc = bass.Bass("TRN2", target_bir_lowering=False)
    tokq = tq * 128
    tok = nq * tokq
    din = lambda n, s, d: nc.dram_tensor(n, s, d, kind="ExternalInput").ap()
    h_in = din("h2T", [D, tok], BF16)
    x1_in = din("x1", [tok, D], F32)
    g2_in = din("gate2", [128, D], F32)
    wq_in = din("wq", [D, 2048], BF16)
    kt_in = din("keysT", [128, 16, 128], F32)
    ut_in = din("UT", [D, nb * 128], BF16)
    v_in = din("V", [nb * 128, D], BF16)
    id_in = din("ident", [128, 128], BF16)
    y_out = nc.dram_tensor("y", [tok, D], F32, kind="ExternalOutput").ap()
    hv = h_in.rearrange("(k p) t -> p k t", p=128)
    wqv = wq_in.rearrange("(k p) n -> p k n", p=128)
    utv = ut_in.rearrange("(k p) e -> p k e", p=128)
    AX = mybir.AxisListType.X
    S = Sched(nc)
    with ExitStack() as st:
        sb = lambda n, s, d: st.enter_context(nc.sbuf_tensor(n + "_s", s, d))
        hh = sb("hh", [128, 32, tokq], BF16)
        ub = [sb("ub%d" % i, [128, 32, 128], BF16) for i in range(2)]
        qpT = sb("qpT", [128, 16, tokq], BF16)
        kTb = sb("kTb", [128, 16, 128], BF16)
        scr_x = sb("scr_x", [128, 2, 8, 128], F32)
        scr_g = sb("scr_g", [128, 2, 8, 128], F32)
        scr_e = sb("scr_e", [128, 2, 8, 128], F32)
        Ghb = [sb("Ghb%d" % i, [128, 8, 128], BF16) for i in range(2)]
        Dk = [sb("Dk%d" % i, [128, 8, 128], BF16) for i in range(tq)]
        kap = sb("kap", [128, 8], F32)
        Xt = [scr_x[:, i] for i in range(2)]; Gh = [scr_g[:, i] for i in range(2)]; Ee = [scr_e[:, i] for i in range(2)]
        ktf = scr_x[:].rearrange("p a h n -> p (a h) n")
        sc = scr_g[:].rearrange("p a h n -> p (a h) n")
        cand = scr_e[:].rearrange("p a h n -> p (a h n)").rearrange("p (h c) -> p h c", h=8)
        XT, GH, EE = ["Xt0", "Xt1"], ["Gh0", "Gh1"], ["Ee0", "Ee1"]
        mx = sb("mx", [128, 16, 16], F32); tmpv = sb("tmpv", [128, 128], F32); tmpc = sb("tmpc", [128, 256], F32)
        c16 = sb("c16", [128, 8, 16], F32)
        th = sb("th", [128, 8], F32); mm = sb("mm", [128, 8], F32); nm = sb("nm", [128, 8], F32)
        zz = sb("zz", [128, 8], F32); m2 = sb("m2", [128, 8], F32); dm = sb("dm", [128, 8], F32)
        ej = sb("ej", [128, 16], F32)
        L2 = [sb("L2_%d" % i, [128, 8, 128], F32) for i in range(tq)]
        D1 = [sb("D1_%d" % i, [128, 8, 128], F32) for i in range(tq)]
        e1 = [sb("e1_%d" % i, [128, 8], F32) for i in range(tq)]
        acc = sb("acc", [128, tq, D], F32)
        vbs = [sb("vb%d" % i, [128, D], BF16) for i in range(2 * G)]
        ga = [sb("ga%d" % i, [128, tokq], F32) for i in range(2)]
        wTs = [sb("wT%d" % i, [128, tokq], BF16) for i in range(2 * G)]
        gch = sb("gch", [128, 1024], F32); xch = [sb("xch%d" % i, [128, 1024], F32) for i in range(2)]
        ident = sb("ident", [128, 128], BF16)
        B = [st.enter_context(nc.psum_tensor("B%d" % i, [128, 512], F32)) for i in range(8)]
        Bb = [b[:].bitcast(BF16) for b in B]

        S.dma("sp", ident[:], id_in[:, :], writes=["ident"])
        S.dma("sp", ktf, kt_in[:, :, :], writes=XT)
        S.add("dve", lambda e: e.tensor_copy(out=kTb[:], in_=ktf), reads=XT, writes=["kTb"])
        for qi in range(nq):
            t0 = qi * tokq
            S.dma("sp", hh[:], hv[:, :, t0:t0 + tokq], writes=["hh"])
            for hp in range(16):
                u = ub[hp % 2]; utok = "ub%d" % (hp % 2)
                S.dma("sp", u[:], wqv[:, :, hp * 128:(hp + 1) * 128], writes=[utok])
                bk = 2 + hp % 2
                for k in range(32):
                    S.add("pe", lambda e, k=k, bk=bk, u=u: e.matmul(B[bk][:, 0:tokq], lhsT=u[:, k, :], rhs=hh[:, k, :],
                                                                   start=(k == 0), stop=(k == 31)),
                          reads=[utok, "hh"], writes=["B%d" % bk])
                S.add("act", lambda e, hp=hp, bk=bk: e.activation(out=qpT[:, hp, :], in_=B[bk][:, 0:tokq], func=AF.Copy),
                      reads=["B%d" % bk], writes=["qpT"])
            for tt in range(tq):
                tsl = slice(tt * 128, (tt + 1) * 128)
                for hp in range(16):
                    bk = 4 + hp // 4
                    S.add("pe", lambda e, hp=hp, bk=bk, tsl=tsl: e.matmul(
                        B[bk][:, (hp % 4) * 128:(hp % 4 + 1) * 128], lhsT=qpT[:, hp, tsl], rhs=kTb[:, hp, :],
                        start=True, stop=True), reads=["qpT", "kTb"], writes=["B%d" % bk])
                for g in range(4):
                    S.add("act", lambda e, g=g: e.activation(
                        out=sc[:, g * 4:(g + 1) * 4, :].rearrange("p a n -> p (a n)"), in_=B[4 + g][:, :], func=AF.Copy),
                        reads=["B%d" % (4 + g)], writes=GH)
                for hp in range(16):
                    S.add("dve", lambda e, hp=hp: e.max(out=mx[:, hp, 0:8], in_=sc[:, hp, :]), reads=GH, writes=["mx"])
                    S.add("dve", lambda e, hp=hp: e.match_replace(out=tmpv[:], in_to_replace=mx[:, hp, 0:8],
                                                                  in_values=sc[:, hp, :], imm_value=-1e30),
                          reads=GH + ["mx"], writes=["tmpv"])
                    S.add("dve", lambda e, hp=hp: e.max(out=mx[:, hp, 8:16], in_=tmpv[:]), reads=["tmpv"], writes=["mx"])
                for h in range(8):
                    S.add("dve", lambda e, h=h: e.tensor_tensor(
                        out=cand[:, h, :].rearrange("p (a b) -> p a b", a=16),
                        in0=mx[:, 2 * h, :].unsqueeze(2).to_broadcast([128, 16, 16]),
                        in1=mx[:, 2 * h + 1, :].unsqueeze(1).to_broadcast([128, 16, 16]), op=ALU.add),
                        reads=["mx"], writes=EE)
                    S.add("dve", lambda e, h=h: e.max(out=c16[:, h, 0:8], in_=cand[:, h, :]), reads=EE, writes=["c16"])
                    S.add("dve", lambda e, h=h: e.match_replace(out=tmpc[:], in_to_replace=c16[:, h, 0:8],
                                                                in_values=cand[:, h, :], imm_value=-1e30),
                          reads=EE + ["c16"], writes=["tmpc"])
                    S.add("dve", lambda e, h=h: e.max(out=c16[:, h, 8:16], in_=tmpc[:]), reads=["tmpc"], writes=["c16"])
                S.add("dve", lambda e: e.tensor_reduce(out=th[:], in_=c16[:], axis=AX, op=ALU.min), reads=["c16"], writes=["th"])
                S.add("dve", lambda e: e.tensor_reduce(out=mm[:], in_=c16[:], axis=AX, op=ALU.max), reads=["c16"], writes=["mm"])
                S.add("dve", lambda e: e.tensor_scalar(out=nm[:], in0=mm[:], scalar1=-1.0, scalar2=None, op0=ALU.mult),
                      reads=["mm"], writes=["nm"])
                for h in range(8):
                    S.add("act", lambda e, h=h: e.activation(out=ej[:], in_=c16[:, h, :], func=AF.Exp, bias=nm[:, h:h + 1],
                                                              accum_out=zz[:, h:h + 1]),
                          reads=["c16", "nm"], writes=["ej", "zz"])
                S.add("act", lambda e: e.activation(out=zz[:], in_=zz[:], func=AF.Ln), reads=["zz"], writes=["zz"])
                sc4 = sc.rearrange("p (h two) n -> p h two n", two=2)
                S.add("dve", lambda e: e.tensor_reduce(out=m2[:], in_=sc4[:, :, 1, :], axis=AX, op=ALU.max),
                      reads=GH, writes=["m2"])
                S.add("dve", lambda e, tt=tt: e.tensor_tensor(out=L2[tt][:], in0=sc4[:, :, 1, :],
                                                              in1=m2[:].unsqueeze(2).to_broadcast([128, 8, 128]),
                                                              op=ALU.subtract), reads=GH + ["m2"], writes=["L2_%d" % tt])
                S.add("dve", lambda e: e.tensor_tensor(out=dm[:], in0=m2[:], in1=th[:], op=ALU.subtract),
                      reads=["m2", "th"], writes=["dm"])
                S.add("dve", lambda e: e.tensor_scalar(out=dm[:], in0=dm[:], scalar1=1e-4, scalar2=None, op0=ALU.add),
                      reads=["dm"], writes=["dm"])
                S.add("dve", lambda e, tt=tt: e.tensor_tensor(out=D1[tt][:], in0=sc4[:, :, 0, :],
                                                              in1=dm[:].unsqueeze(2).to_broadcast([128, 8, 128]),
                                                              op=ALU.add), reads=GH + ["dm"], writes=["D1_%d" % tt])
                S.add("dve", lambda e, tt=tt: e.tensor_tensor(out=e1[tt][:], in0=th[:], in1=mm[:], op=ALU.subtract),
                      reads=["th", "mm"], writes=["e1_%d" % tt])
                S.add("dve", lambda e, tt=tt: e.tensor_tensor(out=e1[tt][:], in0=e1[tt][:], in1=zz[:], op=ALU.subtract),
                      reads=["e1_%d" % tt, "zz"], writes=["e1_%d" % tt])
                S.add("act", lambda e, tt=tt: e.activation(out=kap[:], in_=e1[tt][:], func=AF.Exp),
                      reads=["e1_%d" % tt], writes=["kap"])
                for h in range(8):
                    S.add("dve", lambda e, tt=tt, h=h: e.tensor_scalar(out=Dk[tt][:, h, :], in0=ident[:], scalar1=kap[:, h:h + 1],
                                                                       scalar2=None, op0=ALU.mult),
                          reads=["ident", "kap"], writes=["Dk%d" % tt])
            S.add("pool", lambda e: e.memset(acc[:], 0.0), writes=["acc"])
            gcnt = [0]

            def stage_a(b):
                u = ub[b % 2]; utok = "ub%d" % (b % 2)
                v = vbs[b % (2 * G)]; vtok = "vb%d" % (b % (2 * G))
                S.dma("sp", u[:], utv[:, :, b * 128:(b + 1) * 128], writes=[utok])
                S.dma("sp", v[:], v_in[b * 128:(b + 1) * 128, :], writes=[vtok])
                pb = 2 + b % 2
                for k in range(32):
                    S.add("pe", lambda e, k=k: e.matmul(B[pb][:, 0:tokq], lhsT=u[:, k, :], rhs=hh[:, k, :],
                                                        start=(k == 0), stop=(k == 31)),
                          reads=[utok, "hh"], writes=["B%d" % pb])
                tb = b % 2
                for tt in range(tq):
                    i = gcnt[0] % 2
                    gcnt[0] += 1
                    for h in range(8):
                        S.add("act", lambda e, tt=tt, i=i, h=h: e.activation(out=Ee[i][:, h, :], in_=L2[tt][:, h, :], func=AF.Exp,
                                                                             bias=D1[tt][:, h, b:b + 1]),
                              reads=["L2_%d" % tt, "D1_%d" % tt], writes=[EE[i]])
                    S.add("dve", lambda e, i=i: e.scalar_tensor_tensor(out=Ghb[i][:], in0=Ee[i], scalar=1.0, in1=Ee[i],
                                                                       op0=ALU.is_ge, op1=ALU.mult),
                          reads=[EE[i]], writes=["Ghb%d" % i])
                    for h in range(8):
                        S.add("pe", lambda e, tt=tt, i=i, h=h: e.matmul(B[tb][:, tt * 128:(tt + 1) * 128], lhsT=Ghb[i][:, h, :],
                                                                        rhs=Dk[tt][:, h, :], start=(h == 0), stop=(h == 7)),
                              reads=["Ghb%d" % i, "Dk%d" % tt], writes=["B%d" % tb])

            def stage_b(b):
                pb = 2 + b % 2
                tb = b % 2
                g_ = ga[b % 2]; gtok = "ga%d" % (b % 2)
                w = wTs[b % (2 * G)]; wtok = "wT%d" % (b % (2 * G))
                S.add("act", lambda e: e.activation(out=g_[:], in_=B[pb][:, 0:tokq], func=AF.Gelu),
                      reads=["B%d" % pb], writes=[gtok])
                S.add("dve", lambda e: e.tensor_tensor(out=w[:], in0=g_[:], in1=B[tb][:, 0:tokq], op=ALU.mult),
                      reads=[gtok, "B%d" % tb], writes=[wtok])

            pcnt = [0]

            def stage_c(b0):
                for tt in range(tq):
                    for dc in range(8):
                        bk = 4 + pcnt[0] % 4
                        pcnt[0] += 1
                        for j in range(G):
                            b = b0 + j
                            w = wTs[b % (2 * G)]; wtok = "wT%d" % (b % (2 * G))
                            v = vbs[b % (2 * G)]; vtok = "vb%d" % (b % (2 * G))
                            S.add("pe", lambda e, tt=tt, dc=dc, bk=bk, w=w, v=v, j=j: e.matmul(
                                B[bk][:, :], lhsT=w[:, tt * 128:(tt + 1) * 128], rhs=v[:, dc * 512:(dc + 1) * 512],
                                start=(j == 0), stop=(j == G - 1)), reads=[wtok, vtok], writes=["B%d" % bk])
                        S.add("dve", lambda e, tt=tt, dc=dc, bk=bk: e.tensor_tensor(
                            out=acc[:, tt, dc * 512:(dc + 1) * 512], in0=acc[:, tt, dc * 512:(dc + 1) * 512],
                            in1=B[bk][:, :], op=ALU.add), reads=["acc", "B%d" % bk], writes=["acc"])

            stage_a(0)
            for b in range(nb):
                if b + 1 < nb:
                    stage_a(b + 1)
                stage_b(b)
                if (b + 1) % G == 0:
                    stage_c(b + 1 - G)
            n = 0
            for cc in range(4):
                cs_ = slice(cc * 1024, (cc + 1) * 1024)
                S.dma("sp", gch[:], g2_in[:, cs_], writes=["gch"])
                for tt in range(tq):
                    rs = slice(t0 + tt * 128, t0 + (tt + 1) * 128)
                    xc = xch[n % 2]; xtok = "xch%d" % (n % 2)
                    n += 1
                    S.dma("sp", xc[:], x1_in[rs, cs_], writes=[xtok])
                    S.add("dve", lambda e, tt=tt, cs_=cs_: e.tensor_tensor(out=acc[:, tt, cs_], in0=acc[:, tt, cs_], in1=gch[:],
                                                                          op=ALU.mult), reads=["acc", "gch"], writes=["acc"])
                    S.add("pool", lambda e, tt=tt, cs_=cs_, xc=xc: e.tensor_tensor(out=xc[:], in0=xc[:], in1=acc[:, tt, cs_],
                                                                                 op=ALU.add), reads=["acc", xtok], writes=[xtok])
                    S.dma("sp", y_out[rs, cs_], xc[:], reads=[xtok], writes=["y"])
        _finish(S, st, ["y"])
    return nc


def c2_maps(h2T, x1, mod, wqb, peer_sub_keys, UTb, Vb, tok):
    idb, _, _ = _consts()
    gate2 = np.ascontiguousarray(np.broadcast_to(mod[5 * D:6 * D][None, :], (128, D))).astype(np.float32)
    keysT = np.ascontiguousarray(np.transpose(peer_sub_keys[0].reshape(16, 128, 128), (2, 0, 1)))
    maps = []
    for c in range(NCORES):
        ts = slice(c * tok, (c + 1) * tok)
        maps.append({"h2T": np.ascontiguousarray(h2T[:, ts]), "x1": np.ascontiguousarray(x1[ts]), "gate2": gate2,
                     "wq": wqb, "keysT": keysT, "UT": UTb, "V": Vb, "ident": idb})
    return maps


def _run(nc, maps):
    return run_bass_kernel_spmd(nc, maps, core_ids=list(range(NCORES))).results


def kernel(x, c, ada_w, ada_b, norm1_gain, norm2_gain, w_in, fox_f_bias, fox_q_gain, fox_k_gain,
           hgrn_lower_bounds, hgrn_out_gain, w_out, peer_w_query, peer_sub_keys, peer_u, peer_v):
    f = lambda a: np.asarray(a)
    x, w_in = f(x), f(w_in)
    mod = run_mod(f(c), f(ada_w), f(ada_b))
    x2 = np.ascontiguousarray(x[0])
    n1 = _run(build_n1(), n1_maps(x2, mod, f(norm1_gain)))
    hT = np.concatenate([np.asarray(r["hT"]) for r in n1], axis=1)
    del n1
    fox = _run(build_fox(), fox_maps(hT, w_in, f(fox_f_bias), f(fox_q_gain), f(fox_k_gain)))
    hg = _run(build_hgrn(), hgrn_maps(hT, w_in, f(hgrn_lower_bounds), f(hgrn_out_gain)))
    del hT
    mergedT = np.concatenate([np.asarray(r["mT"]) for r in fox] + [np.asarray(r["mT"]) for r in hg], axis=0)
    del fox, hg
    c1 = _run(build_c1(), c1_maps(x2, mergedT, mod, f(norm2_gain), f(w_out)))
    x1 = np.concatenate([np.asarray(r["x1"]) for r in c1], axis=0)
    h2T = np.concatenate([np.asarray(r["h2T"]) for r in c1], axis=1)
    del c1, mergedT
    p0 = _run(build_p0(), p0_maps(f(peer_w_query), f(peer_u), f(peer_v)))
    UTb = np.concatenate([np.asarray(r["UTb"]) for r in p0], axis=1)
    Vb = np.concatenate([np.asarray(r["Vb"]) for r in p0], axis=0)
    wqb = np.concatenate([np.asarray(r["wqb"]) for r in p0], axis=0)
    del p0
    c2 = _run(build_c2(), c2_maps(h2T, x1, mod, wqb, f(peer_sub_keys), UTb, Vb, TOK))
    y = np.concatenate([np.asarray(r["y"]) for r in c2], axis=0)
    return y.reshape(1, SEQ, D).astype(np.float32)
```

```python
from contextlib import ExitStack

import numpy as np
import ml_dtypes

import concourse.bass as bass
import concourse.mybir as mybir
from concourse.bass_utils import run_bass_kernel_spmd

F32 = mybir.dt.float32
BF16 = mybir.dt.bfloat16
AF = mybir.ActivationFunctionType
ALU = mybir.AluOpType

NCORES = 8
D = 4096
SEQ = 8192
HD = 128
EPS = 1e-6
ENGS = ("pe", "act", "dve", "pool", "sp")


class _Op:
    __slots__ = ("eng", "fn", "deps", "dma", "sig", "sem", "val", "idx")

    def __init__(self, eng, fn, dma):
        self.eng = eng; self.fn = fn; self.deps = set(); self.dma = dma
        self.sig = False; self.sem = None; self.val = 0; self.idx = 0


class Sched:
    def __init__(self, nc, n_dsem=10):
        self.nc = nc
        self.ops = {e: [] for e in ENGS}
        self.lastw = {}
        self.readers = {}
        self.n_dsem = n_dsem
        self._defer = None

    def record(self, fn):
        lst = []
        self._defer = lst
        try:
            fn()
        finally:
            self._defer = None
        return lst

    def interleave(self, lists):
        lists = [l for l in lists if l]
        if not lists:
            return
        n = max(len(l) for l in lists)
        idx = [0] * len(lists)
        for step in range(n):
            for i, l in enumerate(lists):
                tgt = ((step + 1) * len(l) + n - 1) // n
                while idx[i] < min(tgt, len(l)):
                    self.add(*l[idx[i]])
                    idx[i] += 1

    def add(self, eng, fn, reads=(), writes=(), dma=False):
        if self._defer is not None:
            self._defer.append((eng, fn, tuple(reads), tuple(writes), dma))
            return None
        op = _Op(eng, fn, dma)
        deps = set()
        for t in reads:
            w = self.lastw.get(t)
            if w is not None:
                deps.add(w)
        for t in writes:
            w = self.lastw.get(t)
            if w is not None:
                deps.add(w)
            for r in self.readers.get(t, ()):
                deps.add(r)
        for t in reads:
            self.readers.setdefault(t, []).append(op)
        for t in writes:
            self.lastw[t] = op
            self.readers[t] = []
        deps.discard(op)
        op.deps = deps
        op.idx = len(self.ops[eng])
        self.ops[eng].append(op)
        return op

    def dma(self, eng, out, in_, reads=(), writes=()):
        return self.add(eng, lambda e: e.dma_start(out=out, in_=in_), reads, writes, dma=True)

    def emit(self, stack):
        nc = self.nc

        def skip(d, op):
            return d.eng == "pe" and op.eng == "pe" and not d.dma and not op.dma

        for e in ENGS:
            for op in self.ops[e]:
                for d in op.deps:
                    if not skip(d, op):
                        d.sig = True
        csem = {e: stack.enter_context(nc.semaphore("c_" + e)) for e in ENGS}
        dsems = {e: [stack.enter_context(nc.semaphore("d_%s%d" % (e, i))) for i in range(self.n_dsem)]
                 for e in ("sp", "act", "pool")}
        for e in ENGS:
            cnt = 0
            dcnt = 0
            duse = [0] * self.n_dsem
            for op in self.ops[e]:
                if op.dma:
                    k = dcnt % self.n_dsem
                    dcnt += 1
                    duse[k] += 1
                    op.sem = dsems[e][k]
                    op.val = 16 * duse[k]
                    op.sig = True
                elif op.sig:
                    cnt += 1
                    op.sem = csem[e]
                    op.val = cnt
        blk = stack.enter_context(nc.Block())

        def run(e):
            def body(eng):
                waited = {}
                dprev = {}

                def wait(d):
                    key = id(d.sem)
                    if waited.get(key, 0) >= d.val:
                        return
                    eng.wait_ge(d.sem, d.val)
                    waited[key] = d.val

                for op in self.ops[e]:
                    for d in sorted(op.deps, key=lambda o: (o.eng, o.idx)):
                        if not skip(d, op):
                            wait(d)
                    if op.dma:
                        p = dprev.get(id(op.sem))
                        if p is not None:
                            wait(p)
                        dprev[id(op.sem)] = op
                    ins = op.fn(eng)
                    if op.sig:
                        ins.then_inc(op.sem, 16 if op.dma else 1)
            return body

        blk.tensor(run("pe"))
        blk.scalar(run("act"))
        blk.vector(run("dve"))
        blk.gpsimd(run("pool"))
        blk.sync(run("sp"))


def _finish(S, st, out_tokens):
    S.add("sp", lambda e: e.nop(), reads=list(out_tokens))
    S.emit(st)


MODC = 6 * D // NCORES


def build_mod():
    nc = bass.Bass("TRN2", target_bir_lowering=False)
    c_in = nc.dram_tensor("c", [128, 32], F32, kind="ExternalInput").ap()
    w_in = nc.dram_tensor("w", [D, MODC], F32, kind="ExternalInput").ap()
    b_in = nc.dram_tensor("b", [1, MODC], F32, kind="ExternalInput").ap()
    out = nc.dram_tensor("mod", [1, MODC], F32, kind="ExternalOutput").ap()
    wv = w_in.rearrange("(p k) n -> k p n", k=32)
    S = Sched(nc)
    with ExitStack() as st:
        ct = st.enter_context(nc.sbuf_tensor("ct", [128, 32], F32))
        cs = st.enter_context(nc.sbuf_tensor("cs", [128, 32], F32))
        acc = st.enter_context(nc.sbuf_tensor("acc", [128, MODC], F32))
        wts = [st.enter_context(nc.sbuf_tensor("wt%d" % i, [128, MODC], F32)) for i in range(3)]
        ones = st.enter_context(nc.sbuf_tensor("ones", [128, 1], F32))
        bt = st.enter_context(nc.sbuf_tensor("bt", [1, MODC], F32))
        res = st.enter_context(nc.sbuf_tensor("res", [1, MODC], F32))
        pss = [st.enter_context(nc.psum_tensor("ps%d" % i, [128, 512], F32)) for i in range(6)]
        S.dma("sp", ct[:], c_in[:, :], writes=["ct"])
        S.dma("sp", bt[:], b_in[:, :], writes=["bt"])
        S.add("act", lambda e: e.activation(out=cs[:], in_=ct[:], func=AF.Silu), reads=["ct"], writes=["cs"])
        S.add("pool", lambda e: e.memset(ones[:], 1.0), writes=["ones"])
        for k in range(32):
            wt = wts[k % 3]
            tok = "wt%d" % (k % 3)
            S.dma("sp", wt[:], wv[k], writes=[tok])
            if k == 0:
                S.add("dve", lambda e, wt=wt: e.tensor_scalar(out=acc[:], in0=wt[:], scalar1=cs[:, 0:1], scalar2=None,
                                                               op0=ALU.mult), reads=[tok, "cs"], writes=["acc"])
            else:
                S.add("dve", lambda e, wt=wt, k=k: e.scalar_tensor_tensor(out=acc[:], in0=wt[:], scalar=cs[:, k:k + 1],
                                                                           in1=acc[:], op0=ALU.mult, op1=ALU.add),
                      reads=[tok, "cs", "acc"], writes=["acc"])
        for j in range(6):
            S.add("pe", lambda e, j=j: e.matmul(pss[j][0:1, :], lhsT=ones[:, 0:1], rhs=acc[:, j * 512:(j + 1) * 512],
                                                start=True, stop=True), reads=["ones", "acc"], writes=["ps%d" % j])
            S.add("dve", lambda e, j=j: e.tensor_tensor(out=res[0:1, j * 512:(j + 1) * 512], in0=pss[j][0:1, :],
                                                        in1=bt[0:1, j * 512:(j + 1) * 512], op=ALU.add),
                  reads=["ps%d" % j, "bt"], writes=["res%d" % j])
        S.dma("sp", out[:, :], res[:], reads=["res%d" % j for j in range(6)], writes=["out"])
        _finish(S, st, ["out"])
    return nc


def run_mod(c, ada_w, ada_b):
    nc = build_mod()
    c2 = np.ascontiguousarray(c.reshape(128, 32))
    maps = []
    for i in range(NCORES):
        sl = slice(i * MODC, (i + 1) * MODC)
        maps.append({"c": c2, "w": np.ascontiguousarray(ada_w[0][:, sl]),
                     "b": np.ascontiguousarray(ada_b[0][sl].reshape(1, MODC))})
    res = run_bass_kernel_spmd(nc, maps, core_ids=list(range(NCORES)))
    return np.concatenate([np.asarray(r["mod"]).reshape(-1) for r in res.results])


def build_n1(ntl=SEQ // NCORES // 128):
    nc = bass.Bass("TRN2", target_bir_lowering=False)
    tok = ntl * 128
    din = lambda n, s, d: nc.dram_tensor(n, s, d, kind="ExternalInput").ap()
    x_in = din("x", [tok, D], F32)
    g_in = din("g1", [128, 32], F32); sc_in = din("sc1", [128, 32], F32); sh_in = din("sh1", [128, 32], F32)
    id_in = din("ident", [128, 128], BF16)
    h_out = nc.dram_tensor("hT", [D, tok], BF16, kind="ExternalOutput").ap()
    hv = h_out.rearrange("(k p) t -> p k t", p=128)
    S = Sched(nc)
    with ExitStack() as st:
        sb = lambda n, s, d: st.enter_context(nc.sbuf_tensor(n + "_s", s, d))
        a1 = sb("a1", [128, 32], F32); sh1 = sb("sh1", [128, 32], F32); g1 = sb("g1", [128, 32], F32)
        ident = sb("ident", [128, 128], BF16)
        xb = [sb("xb%d" % i, [128, D], F32) for i in range(2)]
        xs = [sb("xs%d" % i, [128, D], BF16) for i in range(2)]
        ss = sb("ss", [128, ntl], F32)
        hT = [sb("hT%d" % i, [128, 32, 128], BF16) for i in range(2)]
        B = [st.enter_context(nc.psum_tensor("B%d" % i, [128, 512], F32)) for i in range(4)]
        Bb = [b[:].bitcast(BF16) for b in B]
        for (t, src, name) in ((g1, g_in, "g1"), (a1, sc_in, "a1"), (sh1, sh_in, "sh1"), (ident, id_in, "ident")):
            S.dma("sp", t[:], src[:, :], writes=[name])
        S.add("dve", lambda e: e.scalar_tensor_tensor(out=a1[:], in0=a1[:], scalar=1.0, in1=g1[:], op0=ALU.add,
                                                      op1=ALU.mult), reads=["a1", "g1"], writes=["a1"])
        for tt in range(ntl):
            ts = slice(tt * 128, (tt + 1) * 128)
            x_ = xb[tt % 2]; xtok = "xb%d" % (tt % 2); s_ = xs[tt % 2]; stok = "xs%d" % (tt % 2)
            S.dma("sp", x_[:], x_in[ts, :], writes=[xtok])
            S.add("act", lambda e, tt=tt, x_=x_, s_=s_: e.activation(out=s_[:], in_=x_[:], func=AF.Square,
                                                                    accum_out=ss[:, tt:tt + 1]),
                  reads=[xtok], writes=[stok, "ss"])
            S.add("act", lambda e, tt=tt: e.activation(out=ss[:, tt:tt + 1], in_=ss[:, tt:tt + 1], func=AF.Sqrt,
                                                        scale=1.0 / D, bias=EPS), reads=["ss"], writes=["ss"])
            S.add("dve", lambda e, tt=tt: e.reciprocal(out=ss[:, tt:tt + 1], in_=ss[:, tt:tt + 1]),
                  reads=["ss"], writes=["ss"])
            S.add("dve", lambda e, tt=tt, x_=x_, s_=s_: e.tensor_scalar(out=s_[:], in0=x_[:], scalar1=ss[:, tt:tt + 1],
                                                                       scalar2=None, op0=ALU.mult),
                  reads=[xtok, "ss"], writes=[stok])
            hb = hT[tt % 2]
            htok = "hT%d" % (tt % 2)
            for k4 in range(8):
                bk = k4 % 4
                for j in range(4):
                    k = k4 * 4 + j
                    S.add("pe", lambda e, k=k, j=j, bk=bk, s_=s_: e.transpose(Bb[bk][:, j * 128:(j + 1) * 128],
                                                                             s_[:, k * 128:(k + 1) * 128], ident[:]),
                          reads=[stok, "ident"], writes=["B%d" % bk])
                for j in range(4):
                    k = k4 * 4 + j
                    S.add("act", lambda e, k=k, j=j, bk=bk, hb=hb: e.activation(
                        out=hb[:, k, :], in_=Bb[bk][:, j * 128:(j + 1) * 128], func=AF.Identity,
                        scale=a1[:, k:k + 1], bias=sh1[:, k:k + 1]),
                        reads=["B%d" % bk, "a1", "sh1"], writes=[htok])
            S.dma("sp", hv[:, :, ts], hb[:], reads=[htok], writes=["h"])
        _finish(S, st, ["h"])
    return nc


def n1_maps(x2, mod, norm1_gain):
    idb, _, _ = _consts()
    tok = SEQ // NCORES
    return [{"x": np.ascontiguousarray(x2[c * tok:(c + 1) * tok]), "g1": _pk(norm1_gain[0]), "sc1": _pk(mod[D:2 * D]),
             "sh1": _pk(mod[0:D]), "ident": idb} for c in range(NCORES)]


NT = SEQ // 128
FC = 770


def _consts():
    ident = np.eye(128, dtype=np.float32)
    ut = np.triu(np.ones((128, 128), np.float32))
    return ident.astype(ml_dtypes.bfloat16), ut, ut.astype(ml_dtypes.bfloat16)


def build_fox(nt=NT):
    nc = bass.Bass("TRN2", target_bir_lowering=False)
    seq = nt * 128
    h_in = nc.dram_tensor("hT", [D, seq], BF16, kind="ExternalInput").ap()
    hTv = h_in.rearrange("(k p) t -> p k t", p=128)
    w_in = nc.dram_tensor("w", [D, FC], F32, kind="ExternalInput").ap()
    fb_in = nc.dram_tensor("fb", [128, 2], F32, kind="ExternalInput").ap()
    gq_in = nc.dram_tensor("gq", [128, 2], F32, kind="ExternalInput").ap()
    gk_in = nc.dram_tensor("gk", [128, 2], F32, kind="ExternalInput").ap()
    id_in = nc.dram_tensor("ident", [128, 128], BF16, kind="ExternalInput").ap()
    ut_in = nc.dram_tensor("ut", [128, 128], F32, kind="ExternalInput").ap()
    utb_in = nc.dram_tensor("utb", [128, 128], BF16, kind="ExternalInput").ap()
    neg_in = nc.dram_tensor("negm", [128, 128], BF16, kind="ExternalInput").ap()
    idf_in = nc.dram_tensor("identf", [128, 128], F32, kind="ExternalInput").ap()
    out = nc.dram_tensor("mT", [256, seq], BF16, kind="ExternalOutput").ap()
    wv = w_in.rearrange("(k p) n -> k p n", p=128)
    S = Sched(nc)
    with ExitStack() as st:
        sb = lambda n, s, d: st.enter_context(nc.sbuf_tensor(n, s, d))
        wF = sb("wF", [128, 32, FC], BF16)
        wst = [sb("wst%d" % i, [128, FC], F32) for i in range(2)]
        qT = [sb("qT%d" % h, [128, seq], BF16) for h in range(2)]
        kT = [sb("kT%d" % h, [128, seq], BF16) for h in range(2)]
        Vx = [sb("Vx%d" % h, [128, nt, 129], BF16) for h in range(2)]
        fl = sb("fl", [128, 2, nt], F32)
        hT = [sb("hT%d" % i, [128, 32, 128], BF16) for i in range(3)]
        ss = sb("ss", [128, nt], F32)
        sq4 = sb("sq4", [128, 4], F32)
        qn = [sb("qn%d" % i, [128, 128], BF16) for i in range(4)]
        fb = sb("fb_s", [128, 2], F32); gq = sb("gq_s", [128, 2], F32); gk = sb("gk_s", [128, 2], F32)
        ident = sb("ident_s", [128, 128], BF16); ut = sb("ut_s", [128, 128], F32); utb = sb("utb_s", [128, 128], BF16)
        onesf = sb("onesf", [128, 128], F32)
        negm = sb("negm_s", [128, 128], BF16); identf = sb("identf_s", [128, 128], F32)
        cum2 = sb("cum2", [128, 128], F32); off2 = sb("off2", [128, 128], F32)
        ctf = sb("ctf", [128, 128], F32); cthi = sb("cthi", [128, 128], BF16); ctr = sb("ctr", [128, 128], F32)
        ctlo = sb("ctlo", [128, 128], BF16)
        CT2 = [sb("CT2_%d" % h, [128, 128], BF16) for h in range(2)]
        E2 = [sb("E2_%d" % i, [128, 128], BF16) for i in range(2)]
        junk = sb("junk", [128, 128], BF16)
        cum = sb("cum", [128, 2, nt], F32); off = sb("off", [128, 2, nt], F32); tot = sb("tot", [128, 2, nt], F32)
        lf = sb("lf", [128, 2, nt], F32)
        negb2 = [sb("negb%d" % i, [128, nt], F32) for i in range(2)]
        rden2 = [sb("rden%d" % i, [128, 1], F32) for i in range(2)]
        on2 = [sb("on%d" % i, [128, 128], BF16) for i in range(2)]
        pT = [sb("pT%d" % i, [128, 128], BF16) for i in range(3)]
        rden = sb("rden", [128, 1], F32)
        on = sb("on", [128, 128], BF16)
        ost = [sb("ost%d" % i, [128, 128], BF16) for i in range(2)]
        B = [st.enter_context(nc.psum_tensor("B%d" % i, [128, 512], F32)) for i in range(8)]
        Bb = [b[:].bitcast(BF16) for b in B]

        for (t, src, name) in ((fb, fb_in, "fb"),
                               (gq, gq_in, "gq"), (gk, gk_in, "gk"), (ident, id_in, "ident"), (ut, ut_in, "ut"),
                               (utb, utb_in, "utb"), (negm, neg_in, "negm"), (identf, idf_in, "identf")):
            S.dma("sp", t[:], src[:, :], writes=[name])
        S.add("dve", lambda e: e.tensor_scalar(out=fb[:], in0=fb[:], scalar1=-1.0, scalar2=None, op0=ALU.mult),
              reads=["fb"], writes=["fb"])
        S.add("pool", lambda e: e.memset(onesf[:], 1.0), writes=["onesf"])
        for h in range(2):
            S.add("pool", lambda e, h=h: e.memset(Vx[h][:, :, 128:129], 1.0), writes=["Vx%d" % h])
        for k in range(32):
            S.dma("sp", wst[k % 2][:], wv[k], writes=["wst%d" % (k % 2)])
            eng = ("act", "dve", "pool")[k % 3]
            if eng == "act":
                S.add("act", lambda e, k=k: e.activation(out=wF[:, k, :], in_=wst[k % 2][:], func=AF.Copy),
                      reads=["wst%d" % (k % 2)], writes=["wF"])
            else:
                S.add(eng, lambda e, k=k: e.tensor_copy(out=wF[:, k, :], in_=wst[k % 2][:]),
                      reads=["wst%d" % (k % 2)], writes=["wF"])

        for tt in range(nt):
            ts = slice(tt * 128, (tt + 1) * 128)
            pb0 = 2 if tt % 2 == 0 else 0
            hb = hT[tt % 3]
            htok = "hT%d" % (tt % 3)
            S.dma("sp", hb[:], hTv[:, :, ts], writes=[htok])
            for cg, (c0, c1) in enumerate(((0, 512), (512, FC))):
                for k in range(32):
                    S.add("pe", lambda e, k=k, cg=cg, c0=c0, c1=c1, hb=hb, pb0=pb0: e.matmul(
                        B[pb0 + cg][:, 0:c1 - c0], lhsT=hb[:, k, :], rhs=wF[:, k, c0:c1], start=(k == 0), stop=(k == 31)),
                        reads=[htok, "wF"], writes=["B%d" % (pb0 + cg)])
            for i in range(4):
                S.add("act", lambda e, i=i, pb0=pb0: e.activation(out=junk[:, 0:128], in_=B[pb0][:, i * 128:(i + 1) * 128],
                                                          func=AF.Square, accum_out=sq4[:, i:i + 1]),
                      reads=["B%d" % pb0], writes=["junk", "sq4"])
            S.add("act", lambda e: e.activation(out=sq4[:], in_=sq4[:], func=AF.Sqrt, scale=1.0 / HD, bias=EPS),
                  reads=["sq4"], writes=["sq4"])
            S.add("dve", lambda e: e.reciprocal(out=sq4[:], in_=sq4[:]), reads=["sq4"], writes=["sq4"])
            for i in range(4):
                S.add("dve", lambda e, i=i, pb0=pb0: e.tensor_scalar(out=qn[i][:], in0=B[pb0][:, i * 128:(i + 1) * 128],
                                                             scalar1=sq4[:, i:i + 1], scalar2=None, op0=ALU.mult),
                      reads=["B%d" % pb0, "sq4"], writes=["qn%d" % i])
                S.add("pe", lambda e, i=i: e.transpose(Bb[4][:, i * 128:(i + 1) * 128], qn[i][:], ident[:]),
                      reads=["qn%d" % i, "ident"], writes=["B4"])
                h = i % 2
                dst, gg, gname = (qT[h], gq, "gq") if i < 2 else (kT[h], gk, "gk")
                S.add("act", lambda e, i=i, dst=dst, gg=gg, h=h, ts=ts: e.activation(
                    out=dst[:, ts], in_=Bb[4][:, i * 128:(i + 1) * 128], func=AF.Copy, scale=gg[:, h:h + 1]),
                    reads=["B4", gname], writes=["qk%d" % i])
            for h in range(2):
                S.add("dve", lambda e, h=h, tt=tt, pb0=pb0: e.tensor_copy(out=Vx[h][:, tt, 0:128], in_=B[pb0 + 1][:, h * 128:(h + 1) * 128]),
                      reads=["B%d" % (pb0 + 1)], writes=["Vx%d" % h])
            S.add("dve", lambda e, tt=tt, pb0=pb0: e.tensor_copy(out=fl[:, :, tt], in_=B[pb0 + 1][:, 256:258]),
                  reads=["B%d" % (pb0 + 1)], writes=["fl"])

        for h in range(2):
            S.add("act", lambda e, h=h: e.activation(out=lf[:, h, :], in_=fl[:, h, :], func=AF.Exp, scale=-1.0,
                                                      bias=fb[:, h:h + 1]), reads=["fl", "fb"], writes=["lf"])
        S.add("act", lambda e: e.activation(out=lf[:], in_=lf[:], func=AF.Ln, scale=1.0, bias=1.0),
              reads=["lf"], writes=["lf"])
        S.add("dve", lambda e: e.tensor_scalar(out=lf[:], in0=lf[:], scalar1=-1.0, scalar2=None, op0=ALU.mult),
              reads=["lf"], writes=["lf"])
        lf2 = lf[:].rearrange("p h t -> p (h t)")
        n2 = 2 * nt
        S.add("pe", lambda e: e.matmul(B[0][:, 0:n2], lhsT=ut[:], rhs=lf2, start=True, stop=True),
              reads=["ut", "lf"], writes=["B0"])
        S.add("pe", lambda e: e.matmul(B[1][:, 0:n2], lhsT=onesf[:], rhs=lf2, start=True, stop=True),
              reads=["onesf", "lf"], writes=["B1"])
        S.add("dve", lambda e: e.tensor_copy(out=tot[:].rearrange("p h t -> p (h t)"), in_=B[1][:, 0:n2]),
              reads=["B1"], writes=["tot"])
        S.add("pool", lambda e: e.memset(off[:], 0.0), writes=["off"])
        for j in range(1, nt):
            S.add("dve", lambda e, j=j: e.tensor_tensor(out=off[:, :, j], in0=off[:, :, j - 1], in1=tot[:, :, j - 1],
                                                        op=ALU.add), reads=["off", "tot"], writes=["off"])
        S.add("dve", lambda e: e.tensor_tensor(out=cum[:].rearrange("p h t -> p (h t)"), in0=B[0][:, 0:n2],
                                               in1=off[:].rearrange("p h t -> p (h t)"), op=ALU.add),
              reads=["B0", "off"], writes=["cum"])

        scale = float(HD) ** -0.5
        for h in range(2):
            S.add("pool", lambda e: e.memset(cum2[:], 0.0), writes=["cum2"])
            S.add("pool", lambda e: e.memset(off2[:], 0.0), writes=["off2"])
            for r in range(2):
                S.add("dve", lambda e, h=h, r=r: e.tensor_copy(out=cum2[:, r * 64:r * 64 + nt], in_=cum[:, h, :]),
                      reads=["cum"], writes=["cum2"])
                S.add("dve", lambda e, h=h, r=r: e.tensor_copy(out=off2[:, r * 64:r * 64 + nt], in_=off[:, h, :]),
                      reads=["off"], writes=["off2"])
            S.add("pe", lambda e: e.transpose(B[0][:, 0:128], cum2[:], identf[:]), reads=["cum2", "identf"], writes=["B0"])
            S.add("pe", lambda e: e.transpose(B[1][:, 0:128], off2[:], identf[:]), reads=["off2", "identf"], writes=["B1"])
            S.add("dve", lambda e: e.tensor_copy(out=ctr[:], in_=B[1][:, 0:128]), reads=["B1"], writes=["ctr"])
            S.add("dve", lambda e: e.tensor_scalar(out=ctf[:], in0=B[0][:, 0:128], scalar1=ctr[:, 0:1], scalar2=1.0 / scale,
                                                   op0=ALU.subtract, op1=ALU.mult), reads=["B0", "ctr"], writes=["ctf"])
            S.add("dve", lambda e: e.tensor_copy(out=cthi[:], in_=ctf[:]), reads=["ctf"], writes=["cthi"])
            S.add("dve", lambda e: e.tensor_tensor(out=ctr[:], in0=ctf[:], in1=cthi[:], op=ALU.subtract),
                  reads=["ctf", "cthi"], writes=["ctr"])
            S.add("dve", lambda e: e.tensor_copy(out=ctlo[:], in_=ctr[:]), reads=["ctr"], writes=["ctlo"])
            S.add("dve", lambda e, h=h: e.tensor_copy(out=CT2[h][0:64, :], in_=cthi[0:64, :]), reads=["cthi"],
                  writes=["CT2_%d" % h])
            S.add("dve", lambda e, h=h: e.tensor_copy(out=CT2[h][64:128, :], in_=ctlo[64:128, :]), reads=["ctlo"],
                  writes=["CT2_%d" % h])
        for h in range(2):
            pairs = [(qb, kb) for qb in range(nt) for kb in range(qb + 1)]

            def emit_s(i, h=h):
                qb, kb = pairs[i]
                qs_ = slice(qb * 128, (qb + 1) * 128)
                sbk = 4 + (i % 2)
                e2 = E2[qb % 2]; e2tok = "E2_%d" % (qb % 2)
                if kb == 0:
                    S.add("dve", lambda e, qb=qb, e2=e2: e.tensor_tensor(
                        out=e2[:], in0=ident[:, qb:qb + 1].to_broadcast([128, 128]),
                        in1=ident[:, 64 + qb:65 + qb].to_broadcast([128, 128]), op=ALU.add),
                        reads=["ident"], writes=[e2tok])
                S.add("pe", lambda e: e.matmul(B[sbk][:, 0:128], lhsT=kT[h][:, kb * 128:(kb + 1) * 128], rhs=qT[h][:, qs_],
                                               start=True, stop=False),
                      reads=["qk%d" % h, "qk%d" % (2 + h)], writes=["B%d" % sbk])
                S.add("pe", lambda e: e.matmul(B[sbk][:, 0:128], lhsT=e2[:], rhs=CT2[h][:], start=False, stop=(kb != qb)),
                      reads=[e2tok, "CT2_%d" % h], writes=["B%d" % sbk])
                if kb == qb:
                    S.add("pe", lambda e: e.matmul(B[sbk][:, 0:128], lhsT=ident[:], rhs=negm[:], start=False, stop=True),
                          reads=["ident", "negm"], writes=["B%d" % sbk])

            def emit_rest(i, h=h):
                qb, kb = pairs[i]
                qs_ = slice(qb * 128, (qb + 1) * 128)
                sbk = 4 + (i % 2)
                pt = pT[i % 3]; ptok = "pT%d" % (i % 3)
                ab = 6 + (qb % 2)
                nb_ = negb2[qb % 2]; nbtok = "negb%d" % (qb % 2)
                if kb == 0:
                    S.add("dve", lambda e: e.tensor_scalar(
                        out=nb_[:, 0:qb + 1], in0=cum[:, h, 0:qb + 1], scalar1=-1.0, scalar2=off[:, h, qb:qb + 1],
                        op0=ALU.mult, op1=ALU.add), reads=["cum", "off"], writes=[nbtok])
                S.add("act", lambda e: e.activation(out=pt[:], in_=B[sbk][:, 0:128], func=AF.Exp, scale=scale,
                                                    bias=nb_[:, kb:kb + 1]),
                      reads=["B%d" % sbk, nbtok], writes=[ptok])
                S.add("pe", lambda e: e.matmul(B[ab][:, 0:129], lhsT=pt[:], rhs=Vx[h][:, kb, :], start=(kb == 0),
                                               stop=(kb == qb)), reads=[ptok, "Vx%d" % h], writes=["B%d" % ab])
                if kb == qb:
                    rd = rden2[qb % 2]; rdtok = "rden%d" % (qb % 2)
                    onb = on2[qb % 2]; ontok = "on%d" % (qb % 2)
                    S.add("dve", lambda e: e.reciprocal(out=rd[:], in_=B[ab][:, 128:129]), reads=["B%d" % ab], writes=[rdtok])
                    S.add("dve", lambda e: e.tensor_scalar(out=onb[:], in0=B[ab][:, 0:128], scalar1=rd[:, 0:1],
                                                           scalar2=None, op0=ALU.mult),
                          reads=["B%d" % ab, rdtok], writes=[ontok])
                    tb = qb % 2
                    S.add("pe", lambda e: e.transpose(Bb[tb][:, 0:128], onb[:], ident[:]), reads=[ontok, "ident"],
                          writes=["B%d" % tb])
                    ob = ost[qb % 2]
                    S.add("act", lambda e: e.activation(out=ob[:], in_=Bb[tb][:, 0:128], func=AF.Copy),
                          reads=["B%d" % tb], writes=["ost%d" % (qb % 2)])
                    S.dma("sp", out[h * 128:(h + 1) * 128, qs_], ob[:], reads=["ost%d" % (qb % 2)], writes=["out"])

            emit_s(0)
            for i in range(len(pairs)):
                if i + 1 < len(pairs):
                    emit_s(i + 1)
                emit_rest(i)
        _finish(S, st, ["out"])
    return nc


def _pk(v):
    return np.ascontiguousarray(np.asarray(v, np.float32).reshape(32, 128).T)


def fox_maps(hT, w_in, fox_f_bias, fox_q_gain, fox_k_gain):
    idb, ut, utb = _consts()
    maps = []
    for c in range(NCORES):
        hs = [2 * c, 2 * c + 1]
        cols = np.concatenate([np.arange(256 * c, 256 * c + 256), 2048 + np.arange(256 * c, 256 * c + 256),
                               4096 + np.arange(256 * c, 256 * c + 256), 6144 + np.array(hs)])
        maps.append({
            "hT": hT,
            "w": np.ascontiguousarray(w_in[0][:, cols]),
            "fb": np.ascontiguousarray(np.broadcast_to(fox_f_bias[0][hs][None, :], (128, 2))).astype(np.float32),
            "gq": np.ascontiguousarray(fox_q_gain[0][hs].T), "gk": np.ascontiguousarray(fox_k_gain[0][hs].T),
            "ident": idb, "ut": ut, "utb": utb, "identf": np.eye(128, dtype=np.float32),
            "negm": (np.tril(np.ones((128, 128), np.float32), -1) * -30000.0).astype(ml_dtypes.bfloat16),
        })
    return maps


HC = 1024


def build_hgrn(nt=NT):
    nc = bass.Bass("TRN2", target_bir_lowering=False)
    seq = nt * 128
    din = lambda n, s, d: nc.dram_tensor(n, s, d, kind="ExternalInput").ap()
    h_in = din("hT", [D, seq], BF16)
    hTv = h_in.rearrange("(k p) t -> p k t", p=128)
    w_in = din("w", [D, HC], F32)
    lb0_in = din("lb0", [128, 256], F32); lb1_in = din("lb1", [128, 256], F32); og_in = din("og", [128, 256], F32)
    id_in = din("ident", [128, 128], BF16)
    tri_in = din("tri", [128, 128], BF16); stri_in = din("stri", [128, 128], BF16); m_in = din("m128", [128, 128], F32)
    c01_in = din("c01", [128, 2], F32)
    out = nc.dram_tensor("mT", [256, seq], BF16, kind="ExternalOutput").ap()
    wv = w_in.rearrange("(k p) n -> k p n", p=128)
    S = Sched(nc)
    with ExitStack() as st:
        sb = lambda n, s, d: st.enter_context(nc.sbuf_tensor(n + "_s", s, d))
        wH = sb("wH", [128, 32, HC], BF16)
        wst = [sb("wst%d" % i, [128, HC], F32) for i in range(2)]
        hT = [sb("hT%d" % i, [128, 32, 128], BF16) for i in range(3)]
        ss = sb("ss", [128, nt], F32)
        lbb = sb("lbb", [128, 256], F32); omlb = sb("omlb", [128, 256], F32); ogb = sb("ogb", [128, 256], F32)
        ident = sb("ident", [128, 128], BF16)
        tri = sb("tri", [128, 128], BF16); stri = sb("stri", [128, 128], BF16); m128 = sb("m128", [128, 128], F32)
        c01 = sb("c01", [128, 2], F32)
        junk = sb("junk", [128, D], BF16)
        sg2 = [sb("sg%d" % i, [128, 256], F32) for i in range(2)]; gl2 = [sb("gl%d" % i, [128, 256], F32) for i in range(2)]
        ghi2 = [sb("ghi%d" % i, [128, 256], BF16) for i in range(2)]; glo2 = [sb("glo%d" % i, [128, 256], BF16) for i in range(2)]
        gr2 = [sb("gr%d" % i, [128, 256], F32) for i in range(2)]
        omfb2 = [sb("omfb%d" % i, [128, 256], BF16) for i in range(2)]; qsb2 = [sb("qsb%d" % i, [128, 256], BF16) for i in range(2)]
        gs2 = [sb("gs%d" % i, [128, 256], F32) for i in range(2)]; vHb2 = [sb("vHb%d" % i, [128, 256], BF16) for i in range(2)]
        ebs = [sb("eb%d" % h, [128, 128], F32) for h in range(2)]
        enbs = [sb("enb%d" % h, [128, 128], F32) for h in range(2)]
        ers = [sb("er%d" % h, [128, 128], F32) for h in range(2)]
        QtTs = [sb("QtT%d" % h, [128, 128], BF16) for h in range(2)]
        Qt0s = [sb("Qt0%d" % h, [128, 128], BF16) for h in range(2)]
        Qt1s = [sb("Qt1%d" % h, [128, 128], BF16) for h in range(2)]
        KtTs = [sb("KtT%d" % h, [128, 128], BF16) for h in range(2)]
        Khs = [sb("Kh%d" % h, [128, 128], BF16) for h in range(2)]
        Kh0s = [sb("Kh0%d" % h, [128, 128], BF16) for h in range(2)]
        Kh1s = [sb("Kh1%d" % h, [128, 128], BF16) for h in range(2)]
        scms = [sb("scm%d" % h, [128, 128], BF16) for h in range(2)]
        junks = [sb("junkh%d" % h, [128, 128], BF16) for h in range(2)]
        Sst = [sb("S%d" % h, [128, 128], F32) for h in range(2)]
        Sbf = [sb("Sbf%d" % h, [128, 128], BF16) for h in range(2)]
        sos = [sb("so%d" % h, [128, 1], F32) for h in range(2)]
        on1s = [sb("on1%d" % h, [128, 128], F32) for h in range(2)]
        on3s = [sb("on3%d" % h, [128, 128], BF16) for h in range(2)]
        ost = [sb("ost%d" % i, [128, 128], BF16) for i in range(4)]
        B = [st.enter_context(nc.psum_tensor("B%d" % i, [128, 512], F32)) for i in range(8)]
        Bb = [b[:].bitcast(BF16) for b in B]

        for (t, src, name) in ((lbb, lb0_in, "lbb"),
                               (omlb, lb1_in, "omlb"), (ogb, og_in, "ogb"), (ident, id_in, "ident"),
                               (tri, tri_in, "tri"), (stri, stri_in, "stri"), (m128, m_in, "m128"), (c01, c01_in, "c01")):
            S.dma("sp", t[:], src[:, :], writes=[name])
        S.add("dve", lambda e: e.tensor_tensor(out=lbb[:], in0=lbb[:], in1=omlb[:], op=ALU.subtract),
              reads=["lbb", "omlb"], writes=["lbb"])
        S.add("act", lambda e: e.activation(out=lbb[:], in_=lbb[:], func=AF.Sigmoid), reads=["lbb"], writes=["lbb"])
        S.add("dve", lambda e: e.tensor_scalar(out=omlb[:], in0=lbb[:], scalar1=-1.0, scalar2=1.0, op0=ALU.mult,
                                               op1=ALU.add), reads=["lbb"], writes=["omlb"])
        for h in range(2):
            S.add("pool", lambda e, h=h: e.memset(Sst[h][:], 0.0), writes=["S%d" % h])
            S.add("pool", lambda e, h=h: e.memset(Sbf[h][:], 0.0), writes=["Sbf%d" % h])
        for h in range(2):
            S.add("pool", lambda e, h=h: e.memset(Qt0s[h][:], 0.0), writes=["Qt0_h%d" % h])
            S.add("pool", lambda e, h=h: e.memset(Qt1s[h][:], 0.0), writes=["Qt1_h%d" % h])
        for k in range(32):
            S.dma("sp", wst[k % 2][:], wv[k], writes=["wst%d" % (k % 2)])
            eng = ("act", "dve", "pool")[k % 3]
            if eng == "act":
                S.add("act", lambda e, k=k: e.activation(out=wH[:, k, :], in_=wst[k % 2][:], func=AF.Copy),
                      reads=["wst%d" % (k % 2)], writes=["wH"])
            else:
                S.add(eng, lambda e, k=k: e.tensor_copy(out=wH[:, k, :], in_=wst[k % 2][:]),
                      reads=["wst%d" % (k % 2)], writes=["wH"])

        def pre_ops(tt):
            pp = tt % 2
            pb0 = 2 if pp == 0 else 0
            ts = slice(tt * 128, (tt + 1) * 128)
            ts = slice(tt * 128, (tt + 1) * 128)
            hb = hT[tt % 3]
            htok = "hT%d" % (tt % 3)
            S.dma("sp", hb[:], hTv[:, :, ts], writes=[htok])
            for cg in range(2):
                for k in range(32):
                    S.add("pe", lambda e, k=k, cg=cg, hb=hb: e.matmul(
                        B[pb0 + cg][:, :], lhsT=hb[:, k, :], rhs=wH[:, k, cg * 512:(cg + 1) * 512],
                        start=(k == 0), stop=(k == 31)), reads=[htok, "wH"], writes=["B%d" % (pb0 + cg)])
            S.add("act", lambda e: e.activation(out=sg2[pp][:], in_=B[pb0][:, 256:512], func=AF.Sigmoid),
                  reads=["B%d" % pb0], writes=["sg%d" % pp])
            S.add("dve", lambda e: e.tensor_tensor(out=sg2[pp][:], in0=sg2[pp][:], in1=omlb[:], op=ALU.mult),
                  reads=["sg%d" % pp, "omlb"], writes=["sg%d" % pp])
            S.add("dve", lambda e: e.tensor_tensor(out=sg2[pp][:], in0=sg2[pp][:], in1=lbb[:], op=ALU.add),
                  reads=["sg%d" % pp, "lbb"], writes=["sg%d" % pp])
            S.add("act", lambda e: e.activation(out=gl2[pp][:], in_=sg2[pp][:], func=AF.Ln), reads=["sg%d" % pp], writes=["gl%d" % pp])
            S.add("dve", lambda e: e.tensor_copy(out=ghi2[pp][:], in_=gl2[pp][:]), reads=["gl%d" % pp], writes=["ghi%d" % pp])
            S.add("dve", lambda e: e.tensor_tensor(out=gr2[pp][:], in0=gl2[pp][:], in1=ghi2[pp][:], op=ALU.subtract),
                  reads=["gl%d" % pp, "ghi%d" % pp], writes=["gr%d" % pp])
            S.add("dve", lambda e: e.tensor_copy(out=glo2[pp][:], in_=gr2[pp][:]), reads=["gr%d" % pp], writes=["glo%d" % pp])
            S.add("dve", lambda e: e.tensor_scalar(out=omfb2[pp][:], in0=sg2[pp][:], scalar1=-1.0, scalar2=1.0, op0=ALU.mult,
                                                   op1=ALU.add), reads=["sg%d" % pp], writes=["omfb%d" % pp])
            S.add("act", lambda e: e.activation(out=qsb2[pp][:], in_=B[pb0][:, 0:256], func=AF.Silu),
                  reads=["B%d" % pb0], writes=["qsb%d" % pp])
            S.add("act", lambda e: e.activation(out=gs2[pp][:], in_=B[pb0 + 1][:, 256:512], func=AF.Silu),
                  reads=["B%d" % (pb0 + 1)], writes=["gs%d" % pp])
            S.add("act", lambda e: e.activation(out=vHb2[pp][:], in_=B[pb0 + 1][:, 0:256], func=AF.Copy),
                  reads=["B%d" % (pb0 + 1)], writes=["vHb%d" % pp])

        def all_heads(tt):
            pp = tt % 2
            ts = slice(tt * 128, (tt + 1) * 128)
            def head_ops(h):
                hc = slice(h * 128, (h + 1) * 128)
                X, Y = 4 + h, 6 + h
                BX, BY, BXb, BYb = B[X], B[Y], Bb[X], Bb[Y]
                tX, tY = "B%d" % X, "B%d" % Y
                tk = lambda n: "%s_h%d" % (n, h)
                eb, enb, er = ebs[h], enbs[h], ers[h]
                QtT, Qt0, Qt1, KtT = QtTs[h], Qt0s[h], Qt1s[h], KtTs[h]
                Kh, Kh0, Kh1, scm = Khs[h], Kh0s[h], Kh1s[h], scms[h]
                so, on1, on3 = sos[h], on1s[h], on3s[h]
                S.add("pe", lambda e: e.matmul(BX[:, 0:128], lhsT=ghi2[pp][:, hc], rhs=tri[:], start=True, stop=False),
                      reads=["ghi%d" % pp, "tri"], writes=[tX])
                S.add("pe", lambda e: e.matmul(BX[:, 0:128], lhsT=glo2[pp][:, hc], rhs=tri[:], start=False, stop=True),
                      reads=["glo%d" % pp, "tri"], writes=[tX])
                S.add("pe", lambda e: e.matmul(BX[:, 128:256], lhsT=stri[:], rhs=ghi2[pp][:, hc], start=True, stop=False),
                      reads=["ghi%d" % pp, "stri"], writes=[tX])
                S.add("pe", lambda e: e.matmul(BX[:, 128:256], lhsT=stri[:], rhs=glo2[pp][:, hc], start=False, stop=True),
                      reads=["glo%d" % pp, "stri"], writes=[tX])
                S.add("act", lambda e: e.activation(out=eb[:], in_=BX[:, 0:128], func=AF.Exp), reads=[tX],
                      writes=[tk("eb")])
                S.add("act", lambda e: e.activation(out=enb[:], in_=BX[:, 0:128], func=AF.Exp, scale=-1.0),
                      reads=[tX], writes=[tk("enb")])
                S.add("act", lambda e: e.activation(out=er[:], in_=BX[:, 128:256], func=AF.Exp), reads=[tX],
                      writes=[tk("er")])
                S.add("pe", lambda e: e.transpose(BXb[:, 512:640], qsb2[pp][:, hc], ident[:]), reads=["qsb%d" % pp, "ident"],
                      writes=[tX])
                S.add("pe", lambda e: e.transpose(BXb[:, 640:768], omfb2[pp][:, hc], ident[:]), reads=["omfb%d" % pp, "ident"],
                      writes=[tX])
                S.add("dve", lambda e: e.tensor_tensor(out=QtT[:], in0=BXb[:, 512:640], in1=eb[:], op=ALU.mult),
                      reads=[tX, tk("eb")], writes=[tk("QtT")])
                S.add("pool", lambda e: e.tensor_copy(out=Qt0[:, 0:64], in_=QtT[:, 0:64]), reads=[tk("QtT")],
                      writes=[tk("Qt0")])
                S.add("pool", lambda e: e.tensor_copy(out=Qt1[:, 64:128], in_=QtT[:, 64:128]), reads=[tk("QtT")],
                      writes=[tk("Qt1")])
                S.add("dve", lambda e: e.tensor_tensor(out=KtT[:], in0=BXb[:, 640:768], in1=enb[:], op=ALU.mult),
                      reads=[tX, tk("enb")], writes=[tk("KtT")])
                S.add("dve", lambda e: e.tensor_tensor(out=Kh[:], in0=omfb2[pp][:, hc], in1=er[:], op=ALU.mult),
                      reads=["omfb%d" % pp, tk("er")], writes=[tk("Kh")])
                S.add("pool", lambda e: e.tensor_scalar(out=Kh0[:], in0=Kh[:], scalar1=c01[:, 0:1], scalar2=None,
                                                        op0=ALU.mult), reads=[tk("Kh"), "c01"], writes=[tk("Kh0")])
                S.add("pool", lambda e: e.tensor_scalar(out=Kh1[:], in0=Kh[:], scalar1=c01[:, 1:2], scalar2=None,
                                                        op0=ALU.mult), reads=[tk("Kh"), "c01"], writes=[tk("Kh1")])
                S.add("pe", lambda e: e.matmul(BY[:, 0:128], lhsT=KtT[:], rhs=QtT[:], start=True, stop=True),
                      reads=[tk("KtT"), tk("QtT")], writes=[tY])
                S.add("dve", lambda e: e.tensor_tensor(out=scm[:], in0=BY[:, 0:128], in1=m128[:], op=ALU.mult),
                      reads=[tY, "m128"], writes=[tk("scm")])
                S.add("pe", lambda e: e.matmul(BY[:, 128:256], lhsT=scm[:], rhs=vHb2[pp][:, hc], start=True, stop=False),
                      reads=[tk("scm"), "vHb%d" % pp], writes=[tY])
                S.add("pe", lambda e: e.matmul(BY[:, 128:256], lhsT=Qt0[:], rhs=Sbf[h][:], start=False, stop=False),
                      reads=[tk("Qt0"), "Sbf%d" % h], writes=[tY])
                sr = slice(384, 512)
                S.add("pe", lambda e: e.matmul(BX[:, sr], lhsT=Kh0[:], rhs=vHb2[pp][:, hc], start=True, stop=True),
                      reads=[tk("Kh0"), "vHb%d" % pp], writes=[tX])
                S.add("dve", lambda e: e.scalar_tensor_tensor(out=Sst[h][:], in0=Sst[h][:], scalar=eb[:, 63:64],
                                                              in1=BX[:, sr], op0=ALU.mult, op1=ALU.add),
                      reads=["S%d" % h, tk("eb"), tX], writes=["S%d" % h])
                S.add("act", lambda e: e.activation(out=Sbf[h][:], in_=Sst[h][:], func=AF.Copy),
                      reads=["S%d" % h], writes=["Sbf%d" % h])
                S.add("pe", lambda e: e.matmul(BY[:, 128:256], lhsT=Qt1[:], rhs=Sbf[h][:], start=False, stop=True),
                      reads=[tk("Qt1"), "Sbf%d" % h], writes=[tY])
                S.add("pe", lambda e: e.matmul(BX[:, sr], lhsT=Kh1[:], rhs=vHb2[pp][:, hc], start=True, stop=True),
                      reads=[tk("Kh1"), "vHb%d" % pp], writes=[tX])
                S.add("dve", lambda e: e.scalar_tensor_tensor(out=Sst[h][:], in0=Sst[h][:], scalar=eb[:, 127:128],
                                                              in1=BX[:, sr], op0=ALU.mult, op1=ALU.add),
                      reads=["S%d" % h, tk("eb"), tX], writes=["S%d" % h])
                S.add("act", lambda e: e.activation(out=Sbf[h][:], in_=Sst[h][:], func=AF.Copy),
                      reads=["S%d" % h], writes=["Sbf%d" % h])
                S.add("act", lambda e: e.activation(out=junks[h][:], in_=BY[:, 128:256], func=AF.Square,
                                                    accum_out=so[:, 0:1]), reads=[tY], writes=[tk("junk"), tk("so")])
                S.add("act", lambda e: e.activation(out=so[:], in_=so[:], func=AF.Sqrt, scale=1.0 / HD, bias=EPS * HD),
                      reads=[tk("so")], writes=[tk("so")])
                S.add("dve", lambda e: e.reciprocal(out=so[:], in_=so[:]), reads=[tk("so")], writes=[tk("so")])
                S.add("dve", lambda e: e.scalar_tensor_tensor(out=on1[:], in0=BY[:, 128:256], scalar=so[:, 0:1],
                                                              in1=ogb[:, hc], op0=ALU.mult, op1=ALU.mult),
                      reads=[tY, tk("so"), "ogb"], writes=[tk("on1")])
                S.add("pool", lambda e: e.tensor_tensor(out=on3[:], in0=on1[:], in1=gs2[pp][:, hc], op=ALU.mult),
                      reads=[tk("on1"), "gs%d" % pp], writes=[tk("on3")])
                S.add("pe", lambda e: e.transpose(BYb[:, 768:896], on3[:], ident[:]), reads=[tk("on3"), "ident"],
                      writes=[tY])
                ob = ost[(2 * tt + h) % 4]
                otok = "ost%d" % ((2 * tt + h) % 4)
                S.add("act", lambda e: e.activation(out=ob[:], in_=BYb[:, 768:896], func=AF.Copy), reads=[tY],
                      writes=[otok])
                S.dma("sp", out[h * 128:(h + 1) * 128, ts], ob[:], reads=[otok], writes=["out"])

            return [S.record(lambda: head_ops(0)), S.record(lambda: head_ops(1))]

        prev = []
        for tt in range(nt):
            cur_pre = S.record(lambda: pre_ops(tt))
            S.interleave([cur_pre] + prev)
            prev = all_heads(tt)
        S.interleave(prev)
        _finish(S, st, ["out"])
    return nc


def hgrn_maps(hT, w_in, hgrn_lower_bounds, hgrn_out_gain):
    idb, _, _ = _consts()
    t64 = np.triu(np.ones((64, 64), np.float32))
    s64 = np.tril(np.ones((64, 64), np.float32), -1)
    z = np.zeros((64, 64), np.float32)
    tri = np.block([[t64, z], [z, t64]]); stri = np.block([[s64, z], [z, s64]])
    c01 = np.zeros((128, 2), np.float32); c01[:64, 0] = 1.0; c01[64:, 1] = 1.0
    maps = []
    o4 = 3 * 2048 + 16
    for c in range(NCORES):
        cr = np.arange(256 * c, 256 * c + 256)
        cols = np.concatenate([o4 + cr, o4 + 2048 + cr, o4 + 4096 + cr, o4 + 6144 + cr])
        bc = lambda v: np.ascontiguousarray(np.broadcast_to(np.asarray(v, np.float32)[None, :], (128, 256)))
        maps.append({
            "hT": hT,
            "w": np.ascontiguousarray(w_in[0][:, cols]),
            "lb0": bc(hgrn_lower_bounds[0][cr]), "lb1": bc(hgrn_lower_bounds[1][cr]),
            "og": bc(hgrn_out_gain[0].reshape(-1)[cr]),
            "ident": idb, "tri": tri.astype(ml_dtypes.bfloat16), "stri": stri.astype(ml_dtypes.bfloat16),
            "m128": tri, "c01": c01,
        })
    return maps


TOK = SEQ // NCORES


def build_c1(ntl=TOK // 128):
    nc = bass.Bass("TRN2", target_bir_lowering=False)
    tok = ntl * 128
    din = lambda n, s, d: nc.dram_tensor(n, s, d, kind="ExternalInput").ap()
    x_in = din("x", [tok, D], F32)
    m_in = din("mT", [D, tok], BF16)
    w_in = din("w", [D, D], F32)
    gt_in = din("gate1", [128, D], F32)
    g2_in = din("g2", [128, 32], F32); sc_in = din("sc2", [128, 32], F32); sh_in = din("sh2", [128, 32], F32)
    id_in = din("ident", [128, 128], BF16)
    x1_out = nc.dram_tensor("x1", [tok, D], F32, kind="ExternalOutput").ap()
    h2_out = nc.dram_tensor("h2T", [D, tok], BF16, kind="ExternalOutput").ap()
    wv = w_in.rearrange("(k p) n -> k p n", p=128)
    mv = m_in.rearrange("(k p) t -> p k t", p=128)
    h2v = h2_out.rearrange("(k p) t -> p k t", p=128)
    S = Sched(nc)
    with ExitStack() as st:
        sb = lambda n, s, d: st.enter_context(nc.sbuf_tensor(n + "_s", s, d))
        wob = sb("wob", [128, 32, 512], BF16)
        wst = [sb("wst%d" % i, [128, 512], F32) for i in range(3)]
        mt = [sb("mt%d" % i, [128, 32, 128], BF16) for i in range(2)]
        xc = [sb("xc%d" % i, [128, 512], F32) for i in range(2)]
        oc_ = [sb("oc%d" % i, [128, 512], F32) for i in range(2)]
        gt = sb("gt", [128, D], F32)
        a2 = sb("a2", [128, 32], F32); sh2 = sb("sh2", [128, 32], F32); g2 = sb("g2", [128, 32], F32)
        ident = sb("ident", [128, 128], BF16)
        xb = sb("xb", [128, D], F32); xs = sb("xs", [128, D], BF16); junk = sb("junk", [128, D], BF16)
        ss = sb("ss", [128, ntl], F32)
        hT = [sb("hT%d" % i, [128, 32, 128], BF16) for i in range(2)]
        B = [st.enter_context(nc.psum_tensor("B%d" % i, [128, 512], F32)) for i in range(8)]
        Bb = [b[:].bitcast(BF16) for b in B]
        for (t, src, name) in ((g2, g2_in, "g2"), (a2, sc_in, "a2"), (sh2, sh_in, "sh2"), (ident, id_in, "ident"),
                               (gt, gt_in, "gt")):
            S.dma("sp", t[:], src[:, :], writes=[name])
        S.add("dve", lambda e: e.scalar_tensor_tensor(out=a2[:], in0=a2[:], scalar=1.0, in1=g2[:], op0=ALU.add,
                                                      op1=ALU.mult), reads=["a2", "g2"], writes=["a2"])
        n = 0
        for cg in range(8):
            cs = slice(cg * 512, (cg + 1) * 512)
            for k in range(32):
                S.dma("sp", wst[k % 3][:], wv[k][:, cs], writes=["wst%d" % (k % 3)])
                eng = ("act", "dve", "pool")[k % 3]
                if eng == "act":
                    S.add("act", lambda e, k=k: e.activation(out=wob[:, k, :], in_=wst[k % 3][:], func=AF.Copy),
                          reads=["wst%d" % (k % 3)], writes=["wob"])
                else:
                    S.add(eng, lambda e, k=k: e.tensor_copy(out=wob[:, k, :], in_=wst[k % 3][:]),
                          reads=["wst%d" % (k % 3)], writes=["wob"])
            for tt in range(ntl):
                ts = slice(tt * 128, (tt + 1) * 128)
                mb = mt[n % 2]; mtok = "mt%d" % (n % 2)
                xcb = xc[n % 2]; xtok = "xc%d" % (n % 2)
                ob = oc_[n % 2]; otok = "oc%d" % (n % 2)
                bk = 2 + (n % 2)
                n += 1
                S.dma("sp", mb[:], mv[:, :, ts], writes=[mtok])
                S.dma("sp", xcb[:], x_in[ts, cs], writes=[xtok])
                for k in range(32):
                    S.add("pe", lambda e, k=k, mb=mb, bk=bk: e.matmul(B[bk][:, :], lhsT=mb[:, k, :], rhs=wob[:, k, :],
                                                                     start=(k == 0), stop=(k == 31)),
                          reads=[mtok, "wob"], writes=["B%d" % bk])
                S.add("dve", lambda e, ob=ob, bk=bk, cs=cs: e.tensor_tensor(out=ob[:], in0=B[bk][:, :], in1=gt[:, cs],
                                                                           op=ALU.mult),
                      reads=["B%d" % bk, "gt"], writes=[otok])
                S.add("pool", lambda e, ob=ob, xcb=xcb: e.tensor_tensor(out=ob[:], in0=ob[:], in1=xcb[:], op=ALU.add),
                      reads=[otok, xtok], writes=[otok])
                S.dma("sp", x1_out[ts, cs], ob[:], reads=[otok], writes=["x1"])
        for tt in range(ntl):
            ts = slice(tt * 128, (tt + 1) * 128)
            S.dma("sp", xb[:], x1_out[ts, :], reads=["x1"], writes=["xb"])
            S.add("act", lambda e, tt=tt: e.activation(out=junk[:], in_=xb[:], func=AF.Square,
                                                        accum_out=ss[:, tt:tt + 1]), reads=["xb"], writes=["junk", "ss"])
            S.add("act", lambda e, tt=tt: e.activation(out=ss[:, tt:tt + 1], in_=ss[:, tt:tt + 1], func=AF.Sqrt,
                                                        scale=1.0 / D, bias=EPS), reads=["ss"], writes=["ss"])
            S.add("dve", lambda e, tt=tt: e.reciprocal(out=ss[:, tt:tt + 1], in_=ss[:, tt:tt + 1]),
                  reads=["ss"], writes=["ss"])
            S.add("dve", lambda e, tt=tt: e.tensor_scalar(out=xs[:], in0=xb[:], scalar1=ss[:, tt:tt + 1], scalar2=None,
                                                           op0=ALU.mult), reads=["xb", "ss"], writes=["xs"])
            hb = hT[tt % 2]
            htok = "hT%d" % (tt % 2)
            for k4 in range(8):
                bk = k4 % 2
                for j in range(4):
                    k = k4 * 4 + j
                    S.add("pe", lambda e, k=k, j=j, bk=bk: e.transpose(Bb[bk][:, j * 128:(j + 1) * 128],
                                                                       xs[:, k * 128:(k + 1) * 128], ident[:]),
                          reads=["xs", "ident"], writes=["B%d" % bk])
                for j in range(4):
                    k = k4 * 4 + j
                    S.add("act", lambda e, k=k, j=j, bk=bk, hb=hb: e.activation(
                        out=hb[:, k, :], in_=Bb[bk][:, j * 128:(j + 1) * 128], func=AF.Identity,
                        scale=a2[:, k:k + 1], bias=sh2[:, k:k + 1]),
                        reads=["B%d" % bk, "a2", "sh2"], writes=[htok])
            S.dma("sp", h2v[:, :, ts], hb[:], reads=[htok], writes=["h2"])
        _finish(S, st, ["x1", "h2"])
    return nc


def c1_maps(x2, mergedT, mod, norm2_gain, w_out):
    idb, _, _ = _consts()
    maps = []
    gate1 = np.ascontiguousarray(np.broadcast_to(mod[2 * D:3 * D][None, :], (128, D))).astype(np.float32)
    wo = np.ascontiguousarray(w_out[0])
    for c in range(NCORES):
        ts = slice(c * TOK, (c + 1) * TOK)
        maps.append({"x": np.ascontiguousarray(x2[ts]), "mT": np.ascontiguousarray(mergedT[:, ts]), "w": wo,
                     "gate1": gate1, "g2": _pk(norm2_gain[0]), "sc2": _pk(mod[4 * D:5 * D]), "sh2": _pk(mod[3 * D:4 * D]),
                     "ident": idb})
    return maps


def build_p0():
    nc = bass.Bass("TRN2", target_bir_lowering=False)
    din = lambda n, s, d: nc.dram_tensor(n, s, d, kind="ExternalInput").ap()
    ut_in = din("UT", [D, 2048], F32); v_in = din("V", [2048, D], F32); wq_in = din("wq", [512, 2048], F32)
    ut_o = nc.dram_tensor("UTb", [D, 2048], BF16, kind="ExternalOutput").ap()
    v_o = nc.dram_tensor("Vb", [2048, D], BF16, kind="ExternalOutput").ap()
    wq_o = nc.dram_tensor("wqb", [512, 2048], BF16, kind="ExternalOutput").ap()
    S = Sched(nc)
    with ExitStack() as st:
        sb = lambda n, s, d: st.enter_context(nc.sbuf_tensor(n + "_s", s, d))
        fi = [sb("fi%d" % i, [128, 4096], F32) for i in range(3)]
        bo = [sb("bo%d" % i, [128, 4096], BF16) for i in range(3)]
        jobs = []
        for r in range(0, D, 256):
            jobs.append((ut_in[r:r + 256, :].rearrange("(a p) n -> p a n", p=128),
                         ut_o[r:r + 256, :].rearrange("(a p) n -> p a n", p=128), True))
        for r in range(0, 2048, 128):
            jobs.append((v_in[r:r + 128, :], v_o[r:r + 128, :], False))
        for r in range(0, 512, 256):
            jobs.append((wq_in[r:r + 256, :].rearrange("(a p) n -> p a n", p=128),
                         wq_o[r:r + 256, :].rearrange("(a p) n -> p a n", p=128), True))
        for i, (src, dst, two) in enumerate(jobs):
            f = fi[i % 3]; b = bo[i % 3]
            fv = f[:].rearrange("p (a n) -> p a n", a=2) if two else f[:]
            bv = b[:].rearrange("p (a n) -> p a n", a=2) if two else b[:]
            S.dma("sp", fv, src, writes=["fi%d" % (i % 3)])
            eng = ("act", "dve", "pool")[i % 3]
            if eng == "act":
                S.add("act", lambda e, f=f, b=b: e.activation(out=b[:], in_=f[:], func=AF.Copy),
                      reads=["fi%d" % (i % 3)], writes=["bo%d" % (i % 3)])
            else:
                S.add(eng, lambda e, f=f, b=b: e.tensor_copy(out=b[:], in_=f[:]),
                      reads=["fi%d" % (i % 3)], writes=["bo%d" % (i % 3)])
            S.dma("sp", dst, bv, reads=["bo%d" % (i % 3)], writes=["out"])
        _finish(S, st, ["out"])
    return nc


def p0_maps(peer_w_query, peer_u, peer_v):
    UT = peer_u[0].T
    maps = []
    for c in range(NCORES):
        es = slice(c * 2048, (c + 1) * 2048)
        maps.append({"UT": np.ascontiguousarray(UT[:, es]), "V": np.ascontiguousarray(peer_v[0][es]),
                     "wq": np.ascontiguousarray(peer_w_query[0][c * 512:(c + 1) * 512])})
    return maps


NEXP_B = 128


def build_c2(nq=4, tq=2, nb=NEXP_B, G=4):
    nc = bass.Bass("TRN2", target_bir_lowering=False)
    tokq = tq * 128
    tok = nq * tokq
    din = lambda n, s, d: nc.dram_tensor(n, s, d, kind="ExternalInput").ap()
    h_in = din("h2T", [D, tok], BF16)
    x1_in = din("x1", [tok, D], F32)
    g2_in = din("gate2", [128, D], F32)
    wq_in = din("wq", [D, 2048], BF16)
    kt_in = din("keysT", [128, 16, 128], F32)
    ut_in = din("UT", [D, nb * 128], BF16)
    v_in = din("V", [nb * 128, D], BF16)
    id_in = din("ident", [128, 128], BF16)
    y_out = nc.dram_tensor("y", [tok, D], F32, kind="ExternalOutput").ap()
    hv = h_in.rearrange("(k p) t -> p k t", p=128)
    wqv = wq_in.rearrange("(k p) n -> p k n", p=128)
    utv = ut_in.rearrange("(k p) e -> p k e", p=128)
    AX = mybir.AxisListType.X
    S = Sched(nc)
    with ExitStack() as st:
        sb = lambda n, s, d: st.enter_context(nc.sbuf_tensor(n + "_s", s, d))
        hh = sb("hh", [128, 32, tokq], BF16)
        ub = [sb("ub%d" % i, [128, 32, 128], BF16) for i in range(2)]
        qpT = sb("qpT", [128, 16, tokq], BF16)
        kTb = sb("kTb", [128, 16, 128], BF16)
        scr_g = sb("scr_g", [128, 2, 8, 128], F32)
        scr_e = sb("scr_e", [128, 2, 8, 128], F32)
        Ghb = [sb("Ghb%d" % i, [128, 8, 128], BF16) for i in range(2)]
        Dk = [sb("Dk%d" % i, [128, 8, 128], BF16) for i in range(tq)]
        kap = sb("kap", [128, 8], F32)
        Gh = [scr_g[:, i] for i in range(2)]; Ee = [scr_e[:, i] for i in range(2)]
        ktf = scr_g[:].rearrange("p a h n -> p (a h) n")
        sc = scr_g[:].rearrange("p a h n -> p (a h) n")
        cand = scr_e[:].rearrange("p a h n -> p (a h n)").rearrange("p (h c) -> p h c", h=8)
        XT, GH, EE = ["Xt0", "Xt1"], ["Gh0", "Gh1"], ["Ee0", "Ee1"]
        EEall = ["%s_h%d" % (t_, h_) for t_ in EE for h_ in range(8)]
        mx = sb("mx", [128, 16, 16], F32); tmpv = sb("tmpv", [128, 128], F32); tmpc = sb("tmpc", [128, 256], F32)
        c16 = sb("c16", [128, 8, 16], F32)
        th = sb("th", [128, 8], F32); mm = sb("mm", [128, 8], F32); nm = sb("nm", [128, 8], F32)
        zz = sb("zz", [128, 8], F32); m2 = sb("m2", [128, 8], F32); dm = sb("dm", [128, 8], F32)
        ej = sb("ej", [128, 16], F32)
        L2 = [sb("L2_%d" % i, [128, 8, 128], F32) for i in range(tq)]
        D1 = [sb("D1_%d" % i, [128, 8, 128], F32) for i in range(tq)]
        e1 = [sb("e1_%d" % i, [128, 8], F32) for i in range(tq)]
        acc = sb("acc", [128, tq, D], F32)
        vbs = [sb("vb%d" % i, [128, D], BF16) for i in range(2 * G)]
        ga = [sb("ga%d" % i, [128, tokq], F32) for i in range(2)]
        wTs = [sb("wT%d" % i, [128, tokq], BF16) for i in range(2 * G)]
        gch = sb("gch", [128, 1024], F32); xch = [sb("xch%d" % i, [128, 1024], F32) for i in range(2)]
        ident = sb("ident", [128, 128], BF16)
        B = [st.enter_context(nc.psum_tensor("B%d" % i, [128, 512], F32)) for i in range(8)]
        Bb = [b[:].bitcast(BF16) for b in B]

        S.dma("sp", ident[:], id_in[:, :], writes=["ident"])
        S.dma("sp", ktf, kt_in[:, :, :], writes=GH)
        S.add("dve", lambda e: e.tensor_copy(out=kTb[:], in_=ktf), reads=GH, writes=["kTb"])
        for qi in range(nq):
            t0 = qi * tokq
            S.dma("sp", hh[:], hv[:, :, t0:t0 + tokq], writes=["hh"])
            for hp in range(16):
                u = ub[hp % 2]; utok = "ub%d" % (hp % 2)
                S.dma("sp", u[:], wqv[:, :, hp * 128:(hp + 1) * 128], writes=[utok])
                bk = 2 + hp % 2
                for k in range(32):
                    S.add("pe", lambda e, k=k, bk=bk, u=u: e.matmul(B[bk][:, 0:tokq], lhsT=u[:, k, :], rhs=hh[:, k, :],
                                                                   start=(k == 0), stop=(k == 31)),
                          reads=[utok, "hh"], writes=["B%d" % bk])
                S.add("act", lambda e, hp=hp, bk=bk: e.activation(out=qpT[:, hp, :], in_=B[bk][:, 0:tokq], func=AF.Copy),
                      reads=["B%d" % bk], writes=["qpT"])
            for tt in range(tq):
                tsl = slice(tt * 128, (tt + 1) * 128)
                for hp in range(16):
                    bk = 4 + hp // 4
                    S.add("pe", lambda e, hp=hp, bk=bk, tsl=tsl: e.matmul(
                        B[bk][:, (hp % 4) * 128:(hp % 4 + 1) * 128], lhsT=qpT[:, hp, tsl], rhs=kTb[:, hp, :],
                        start=True, stop=True), reads=["qpT", "kTb"], writes=["B%d" % bk])
                for g in range(4):
                    S.add("act", lambda e, g=g: e.activation(
                        out=sc[:, g * 4:(g + 1) * 4, :].rearrange("p a n -> p (a n)"), in_=B[4 + g][:, :], func=AF.Copy),
                        reads=["B%d" % (4 + g)], writes=GH)
                for hp in range(16):
                    S.add("dve", lambda e, hp=hp: e.max(out=mx[:, hp, 0:8], in_=sc[:, hp, :]), reads=GH, writes=["mx"])
                    S.add("dve", lambda e, hp=hp: e.match_replace(out=tmpv[:], in_to_replace=mx[:, hp, 0:8],
                                                                  in_values=sc[:, hp, :], imm_value=-1e30),
                          reads=GH + ["mx"], writes=["tmpv"])
                    S.add("dve", lambda e, hp=hp: e.max(out=mx[:, hp, 8:16], in_=tmpv[:]), reads=["tmpv"], writes=["mx"])
                for h in range(8):
                    S.add("dve", lambda e, h=h: e.tensor_tensor(
                        out=cand[:, h, :].rearrange("p (a b) -> p a b", a=16),
                        in0=mx[:, 2 * h, :].unsqueeze(2).to_broadcast([128, 16, 16]),
                        in1=mx[:, 2 * h + 1, :].unsqueeze(1).to_broadcast([128, 16, 16]), op=ALU.add),
                        reads=["mx"], writes=EEall)
                    S.add("dve", lambda e, h=h: e.max(out=c16[:, h, 0:8], in_=cand[:, h, :]), reads=EEall, writes=["c16"])
                    S.add("dve", lambda e, h=h: e.match_replace(out=tmpc[:], in_to_replace=c16[:, h, 0:8],
                                                                in_values=cand[:, h, :], imm_value=-1e30),
                          reads=EEall + ["c16"], writes=["tmpc"])
                    S.add("dve", lambda e, h=h: e.max(out=c16[:, h, 8:16], in_=tmpc[:]), reads=["tmpc"], writes=["c16"])
                S.add("dve", lambda e: e.tensor_reduce(out=th[:], in_=c16[:], axis=AX, op=ALU.min), reads=["c16"], writes=["th"])
                S.add("dve", lambda e: e.tensor_reduce(out=mm[:], in_=c16[:], axis=AX, op=ALU.max), reads=["c16"], writes=["mm"])
                S.add("dve", lambda e: e.tensor_scalar(out=nm[:], in0=mm[:], scalar1=-1.0, scalar2=None, op0=ALU.mult),
                      reads=["mm"], writes=["nm"])
                for h in range(8):
                    S.add("act", lambda e, h=h: e.activation(out=ej[:], in_=c16[:, h, :], func=AF.Exp, bias=nm[:, h:h + 1],
                                                              accum_out=zz[:, h:h + 1]),
                          reads=["c16", "nm"], writes=["ej", "zz"])
                S.add("act", lambda e: e.activation(out=zz[:], in_=zz[:], func=AF.Ln), reads=["zz"], writes=["zz"])
                sc4 = sc.rearrange("p (h two) n -> p h two n", two=2)
                S.add("dve", lambda e: e.tensor_reduce(out=m2[:], in_=sc4[:, :, 1, :], axis=AX, op=ALU.max),
                      reads=GH, writes=["m2"])
                S.add("dve", lambda e, tt=tt: e.tensor_tensor(out=L2[tt][:], in0=sc4[:, :, 1, :],
                                                              in1=m2[:].unsqueeze(2).to_broadcast([128, 8, 128]),
                                                              op=ALU.subtract), reads=GH + ["m2"], writes=["L2_%d" % tt])
                S.add("dve", lambda e: e.tensor_tensor(out=dm[:], in0=m2[:], in1=th[:], op=ALU.subtract),
                      reads=["m2", "th"], writes=["dm"])
                S.add("dve", lambda e: e.tensor_scalar(out=dm[:], in0=dm[:], scalar1=1e-4, scalar2=None, op0=ALU.add),
                      reads=["dm"], writes=["dm"])
                S.add("dve", lambda e, tt=tt: e.tensor_tensor(out=D1[tt][:], in0=sc4[:, :, 0, :],
                                                              in1=dm[:].unsqueeze(2).to_broadcast([128, 8, 128]),
                                                              op=ALU.add), reads=GH + ["dm"], writes=["D1_%d" % tt])
                S.add("dve", lambda e, tt=tt: e.tensor_tensor(out=e1[tt][:], in0=th[:], in1=mm[:], op=ALU.subtract),
                      reads=["th", "mm"], writes=["e1_%d" % tt])
                S.add("dve", lambda e, tt=tt: e.tensor_tensor(out=e1[tt][:], in0=e1[tt][:], in1=zz[:], op=ALU.subtract),
                      reads=["e1_%d" % tt, "zz"], writes=["e1_%d" % tt])
                S.add("act", lambda e, tt=tt: e.activation(out=kap[:], in_=e1[tt][:], func=AF.Exp),
                      reads=["e1_%d" % tt], writes=["kap"])
                for h in range(8):
                    S.add("dve", lambda e, tt=tt, h=h: e.tensor_scalar(out=Dk[tt][:, h, :], in0=ident[:], scalar1=kap[:, h:h + 1],
                                                                       scalar2=None, op0=ALU.mult),
                          reads=["ident", "kap"], writes=["Dk%d_h%d" % (tt, h)])
            S.add("pool", lambda e: e.memset(acc[:], 0.0), writes=["acc"])
            gcnt = [0]

            def stage_a(b):
                u = ub[b % 2]; utok = "ub%d" % (b % 2)
                v = vbs[b % (2 * G)]; vtok = "vb%d" % (b % (2 * G))
                S.dma("sp", u[:], utv[:, :, b * 128:(b + 1) * 128], writes=[utok])
                S.dma("sp", v[:], v_in[b * 128:(b + 1) * 128, :], writes=[vtok])
                pb = 2 + b % 2
                for k in range(32):
                    S.add("pe", lambda e, k=k: e.matmul(B[pb][:, 0:tokq], lhsT=u[:, k, :], rhs=hh[:, k, :],
                                                        start=(k == 0), stop=(k == 31)),
                          reads=[utok, "hh"], writes=["B%d" % pb])
                tb = b % 2
                for tt in range(tq):
                    i = gcnt[0] % 2
                    gcnt[0] += 1
                    for h in range(8):
                        S.add("act", lambda e, tt=tt, i=i, h=h: e.activation(out=Ee[i][:, h, :], in_=L2[tt][:, h, :], func=AF.Exp,
                                                                             bias=D1[tt][:, h, b:b + 1]),
                              reads=["L2_%d" % tt, "D1_%d" % tt], writes=["%s_h%d" % (EE[i], h)])
                    S.add("dve", lambda e, i=i: e.scalar_tensor_tensor(out=Ghb[i][:], in0=Ee[i], scalar=1.0, in1=Ee[i],
                                                                       op0=ALU.is_ge, op1=ALU.mult),
                          reads=["%s_h%d" % (EE[i], h) for h in range(8)], writes=["Ghb%d" % i])
                    for h in range(8):
                        S.add("pe", lambda e, tt=tt, i=i, h=h: e.matmul(B[tb][:, tt * 128:(tt + 1) * 128], lhsT=Ghb[i][:, h, :],
                                                                        rhs=Dk[tt][:, h, :], start=(h == 0), stop=(h == 7)),
                              reads=["Ghb%d" % i, "Dk%d_h%d" % (tt, h)], writes=["B%d" % tb])

            def stage_b(b):
                pb = 2 + b % 2
                tb = b % 2
                g_ = ga[b % 2]; gtok = "ga%d" % (b % 2)
                w = wTs[b % (2 * G)]; wtok = "wT%d" % (b % (2 * G))
                S.add("act", lambda e: e.activation(out=g_[:], in_=B[pb][:, 0:tokq], func=AF.Gelu),
                      reads=["B%d" % pb], writes=[gtok])
                S.add("dve", lambda e: e.tensor_tensor(out=w[:], in0=g_[:], in1=B[tb][:, 0:tokq], op=ALU.mult),
                      reads=[gtok, "B%d" % tb], writes=[wtok])

            pcnt = [0]

            def stage_c(b0):
                for tt in range(tq):
                    for dc in range(8):
                        bk = 4 + pcnt[0] % 4
                        pcnt[0] += 1
                        for j in range(G):
                            b = b0 + j
                            w = wTs[b % (2 * G)]; wtok = "wT%d" % (b % (2 * G))
                            v = vbs[b % (2 * G)]; vtok = "vb%d" % (b % (2 * G))
                            S.add("pe", lambda e, tt=tt, dc=dc, bk=bk, w=w, v=v, j=j: e.matmul(
                                B[bk][:, :], lhsT=w[:, tt * 128:(tt + 1) * 128], rhs=v[:, dc * 512:(dc + 1) * 512],
                                start=(j == 0), stop=(j == G - 1)), reads=[wtok, vtok], writes=["B%d" % bk])
                        S.add("dve", lambda e, tt=tt, dc=dc, bk=bk: e.tensor_tensor(
                            out=acc[:, tt, dc * 512:(dc + 1) * 512], in0=acc[:, tt, dc * 512:(dc + 1) * 512],
                            in1=B[bk][:, :], op=ALU.add), reads=["acc", "B%d" % bk], writes=["acc"])

            stage_a(0)
            for b in range(nb):
                if b + 1 < nb:
                    stage_a(b + 1)
                stage_b(b)
                if (b + 1) % G == 0:
                    stage_c(b + 1 - G)
            n = 0
            for cc in range(4):
                cs_ = slice(cc * 1024, (cc + 1) * 1024)
                S.dma("sp", gch[:], g2_in[:, cs_], writes=["gch"])
                for tt in range(tq):
                    rs = slice(t0 + tt * 128, t0 + (tt + 1) * 128)
                    xc = xch[n % 2]; xtok = "xch%d" % (n % 2)
                    n += 1
                    S.dma("sp", xc[:], x1_in[rs, cs_], writes=[xtok])
                    S.add("dve", lambda e, tt=tt, cs_=cs_: e.tensor_tensor(out=acc[:, tt, cs_], in0=acc[:, tt, cs_], in1=gch[:],
                                                                          op=ALU.mult), reads=["acc", "gch"], writes=["acc"])
                    S.add("pool", lambda e, tt=tt, cs_=cs_, xc=xc: e.tensor_tensor(out=xc[:], in0=xc[:], in1=acc[:, tt, cs_],
                                                                                 op=ALU.add), reads=["acc", xtok], writes=[xtok])
                    S.dma("sp", y_out[rs, cs_], xc[:], reads=[xtok], writes=["y"])
        _finish(S, st, ["y"])
    return nc


def c2_maps(h2T, x1, mod, wqb, peer_sub_keys, UTb, Vb, tok):
    idb, _, _ = _consts()
    gate2 = np.ascontiguousarray(np.broadcast_to(mod[5 * D:6 * D][None, :], (128, D))).astype(np.float32)
    keysT = np.ascontiguousarray(np.transpose(peer_sub_keys[0].reshape(16, 128, 128), (2, 0, 1)))
    maps = []
    for c in range(NCORES):
        ts = slice(c * tok, (c + 1) * tok)
        maps.append({"h2T": np.ascontiguousarray(h2T[:, ts]), "x1": np.ascontiguousarray(x1[ts]), "gate2": gate2,
                     "wq": wqb, "keysT": keysT, "UT": UTb, "V": Vb, "ident": idb})
    return maps


def _run(nc, maps):
    return run_bass_kernel_spmd(nc, maps, core_ids=list(range(NCORES))).results


def kernel(x, c, ada_w, ada_b, norm1_gain, norm2_gain, w_in, fox_f_bias, fox_q_gain, fox_k_gain,
           hgrn_lower_bounds, hgrn_out_gain, w_out, peer_w_query, peer_sub_keys, peer_u, peer_v):
    f = lambda a: np.asarray(a)
    x, w_in = f(x), f(w_in)
    mod = run_mod(f(c), f(ada_w), f(ada_b))
    x2 = np.ascontiguousarray(x[0])
    n1 = _run(build_n1(), n1_maps(x2, mod, f(norm1_gain)))
    hT = np.concatenate([np.asarray(r["hT"]) for r in n1], axis=1)
    del n1
    fox = _run(build_fox(), fox_maps(hT, w_in, f(fox_f_bias), f(fox_q_gain), f(fox_k_gain)))
    hg = _run(build_hgrn(), hgrn_maps(hT, w_in, f(hgrn_lower_bounds), f(hgrn_out_gain)))
    del hT
    mergedT = np.concatenate([np.asarray(r["mT"]) for r in fox] + [np.asarray(r["mT"]) for r in hg], axis=0)
    del fox, hg
    c1 = _run(build_c1(), c1_maps(x2, mergedT, mod, f(norm2_gain), f(w_out)))
    x1 = np.concatenate([np.asarray(r["x1"]) for r in c1], axis=0)
    h2T = np.concatenate([np.asarray(r["h2T"]) for r in c1], axis=1)
    del c1, mergedT
    p0 = _run(build_p0(), p0_maps(f(peer_w_query), f(peer_u), f(peer_v)))
    UTb = np.concatenate([np.asarray(r["UTb"]) for r in p0], axis=1)
    Vb = np.concatenate([np.asarray(r["Vb"]) for r in p0], axis=0)
    wqb = np.concatenate([np.asarray(r["wqb"]) for r in p0], axis=0)
    del p0
    c2 = _run(build_c2(), c2_maps(h2T, x1, mod, wqb, f(peer_sub_keys), UTb, Vb, TOK))
    y = np.concatenate([np.asarray(r["y"]) for r in c2], axis=0)
    return y.reshape(1, SEQ, D).astype(np.float32)
```
